# Optimizing a Trainium2 kernel written in Bass

```python
import jax, jax.numpy as jnp
from jax import lax
import numpy as np

D_MODEL = 1024
BATCH = 32
SEQ = 2048
DEPTH = 1

HEAD_DIM = 64
DIL_CONFIGS = ((128, 1), (512, 4), (2048, 16))
N_DIL_GROUPS = 3
DIL_HEADS = 4
DIL_WIDTH = N_DIL_GROUPS * DIL_HEADS * HEAD_DIM
DIL_OUT_WIDTH = DIL_HEADS * HEAD_DIM
GQA_Q_HEADS = 16
GQA_KV_HEADS = 4
GQA_REP = GQA_Q_HEADS // GQA_KV_HEADS
GQA_Q_WIDTH = GQA_Q_HEADS * HEAD_DIM
GQA_KV_WIDTH = GQA_KV_HEADS * HEAD_DIM
N_BRANCHES = 2
IN_WIDTH = 3 * DIL_WIDTH + GQA_Q_WIDTH + 2 * GQA_KV_WIDTH + N_BRANCHES * D_MODEL
QBLK = 128
GRID_W = 64
ROPE_THETA = 10000.0
N_EXPERTS = 256
TOP_K = 8
N_EXPERT_GROUPS = 8
TOPK_GROUPS = 4
EXPERT_FF = 256
SHARED_FF = 256
ROUTED_SCALE = 2.5
MOE_BLOCK = 128
NORM_EPS = 1e-6

kernel_name = "hybrid_dilated_gqa_moe_adaln_block"


def rms_norm(x, g):
    xf = x.astype(jnp.float32)
    y = xf * lax.rsqrt(jnp.mean(xf * xf, axis=-1, keepdims=True) + NORM_EPS)
    return (y * g.astype(jnp.float32)).astype(x.dtype)


def rope_cos_sin(pos, dim):
    inv = ROPE_THETA ** (-jnp.arange(0, dim, 2, dtype=jnp.float32) / dim)
    ang = pos.astype(jnp.float32)[:, None] * inv[None, :]
    ang = jnp.concatenate([ang, ang], axis=-1)
    return jnp.cos(ang), jnp.sin(ang)


def apply_rope(x, cos, sin):
    xf = x.astype(jnp.float32)
    x1, x2 = jnp.split(xf, 2, axis=-1)
    rot = jnp.concatenate([-x2, x1], axis=-1)
    extra = x.ndim - 3
    shp = (1, cos.shape[0]) + (1,) * extra + (cos.shape[1],)
    return (xf * cos.reshape(shp) + rot * sin.reshape(shp)).astype(x.dtype)


def dilated_window_attention(q, k, v, dil, n):
    B, S, H, hd = q.shape
    L = S // dil
    nb = -(-L // n)
    Lp = nb * n

    def to_res(a):
        a = a.reshape(B, L, dil, H, hd).transpose(0, 2, 1, 3, 4)
        return jnp.pad(a, ((0, 0), (0, 0), (0, Lp - L), (0, 0), (0, 0)))

    def band(a):
        ap = jnp.pad(a, ((0, 0), (0, 0), (n, n), (0, 0), (0, 0)))
        parts = [ap[:, :, i * n: i * n + Lp].reshape(B, dil, nb, n, H, hd) for i in range(3)]
        return jnp.concatenate(parts, axis=3)

    qb = to_res(q).reshape(B, dil, nb, n, H, hd)
    kb = band(to_res(k))
    vb = band(to_res(v))
    s = jnp.einsum('brnqhd,brnkhd->brnhqk', qb, kb).astype(jnp.float32) * (hd ** -0.5)
    blk = jnp.arange(nb)[:, None]
    q_pos = blk * n + jnp.arange(n)[None, :]
    k_pos = blk * n - n + jnp.arange(3 * n)[None, :]
    diff = k_pos[:, None, :] - q_pos[:, :, None]
    valid = (jnp.abs(diff) <= n) & (k_pos[:, None, :] >= 0) & (k_pos[:, None, :] < L)
    s = jnp.where(valid[None, None, :, None, :, :], s, -jnp.inf)
    lse = jax.nn.logsumexp(s, axis=-1)
    p = jnp.exp(s - lse[..., None])
    o = jnp.einsum('brnhqk,brnkhd->brnqhd', p.astype(v.dtype), vb)
    o = o.reshape(B, dil, Lp, H, hd)[:, :, :L].transpose(0, 2, 1, 3, 4).reshape(B, S, H, hd)
    lse = lse.transpose(0, 1, 2, 4, 3).reshape(B, dil, Lp, H)[:, :, :L]
    lse = lse.transpose(0, 2, 1, 3).reshape(B, S, H)
    return o, lse


def dilated_mixture(q, k, v):
    outs, lses = [], []
    for g, (window, dil) in enumerate(DIL_CONFIGS):
        o, lse = dilated_window_attention(q[:, :, g], k[:, :, g], v[:, :, g], dil, window // (2 * dil))
        outs.append(o)
        lses.append(lse)
    w = jax.nn.softmax(jnp.stack(lses, axis=0), axis=0)
    o = jnp.sum(w[..., None] * jnp.stack(outs, axis=0).astype(jnp.float32), axis=0)
    B, S = q.shape[:2]
    return o.astype(q.dtype).reshape(B, S, DIL_OUT_WIDTH)


def gqa_blocked(q, k, v):
    B, S, _, hd = q.shape
    nqb = S // QBLK
    qb = q.reshape(B, nqb, QBLK, GQA_KV_HEADS, GQA_REP, hd).transpose(1, 0, 2, 3, 4, 5)

    def one_block(qblk):
        s = jnp.einsum('bqgrd,bkgd->bgrqk', qblk, k).astype(jnp.float32) * (hd ** -0.5)
        p = jax.nn.softmax(s, axis=-1)
        return jnp.einsum('bgrqk,bkgd->bqgrd', p.astype(v.dtype), v)

    o = lax.map(one_block, qb)
    return o.transpose(1, 0, 2, 3, 4, 5).reshape(B, S, GQA_Q_WIDTH)


def swiglu(h, w_gate, w_up, w_down):
    return (jax.nn.silu(h @ w_gate) * (h @ w_up)) @ w_down


def routed_moe(h, router_w, router_bias, w_exp_gate, w_exp_up, w_exp_down):
    T, D = h.shape
    E = N_EXPERTS
    scores = jax.nn.sigmoid((h @ router_w).astype(jnp.float32))
    biased = scores + router_bias.astype(jnp.float32)[None, :]
    grp = biased.reshape(T, N_EXPERT_GROUPS, E // N_EXPERT_GROUPS)
    grp_score = lax.top_k(grp, 2)[0].sum(-1)
    _, grp_idx = lax.top_k(grp_score, TOPK_GROUPS)
    grp_mask = jax.nn.one_hot(grp_idx, N_EXPERT_GROUPS, dtype=jnp.float32).sum(1)
    exp_mask = jnp.repeat(grp_mask, E // N_EXPERT_GROUPS, axis=1)
    _, idx = lax.top_k(jnp.where(exp_mask > 0, biased, -jnp.inf), TOP_K)
    gate = jnp.take_along_axis(scores, idx, axis=1)
    gate = gate / jnp.sum(gate, axis=-1, keepdims=True) * ROUTED_SCALE

    A = T * TOP_K
    flat_e = idx.reshape(-1).astype(jnp.int32)
    order = jnp.argsort(flat_e)
    sorted_e = flat_e[order]
    tok_sorted = (order // TOP_K).astype(jnp.int32)
    gate_sorted = gate.reshape(-1)[order].astype(h.dtype)
    counts = jnp.bincount(flat_e, length=E).astype(jnp.int32)
    start = jnp.cumsum(counts) - counts
    padded = ((counts + MOE_BLOCK - 1) // MOE_BLOCK) * MOE_BLOCK
    pend = jnp.cumsum(padded)
    pstart = pend - padded
    dest = pstart[sorted_e] + (jnp.arange(A, dtype=jnp.int32) - start[sorted_e])
    NB = -(-A // MOE_BLOCK) + E
    buf_tok = jnp.full((NB * MOE_BLOCK,), T, dtype=jnp.int32).at[dest].set(tok_sorted)
    buf_gate = jnp.zeros((NB * MOE_BLOCK,), dtype=h.dtype).at[dest].set(gate_sorted)
    block_expert = jnp.searchsorted(pend, jnp.arange(NB, dtype=jnp.int32) * MOE_BLOCK, side='right')
    block_expert = jnp.minimum(block_expert, E - 1).astype(jnp.int32)
    h_pad = jnp.concatenate([h, jnp.zeros((1, D), h.dtype)], axis=0)

    def step(acc, blk):
        e, tok, g = blk
        rows = h_pad[tok]
        y = swiglu(rows, w_exp_gate[e], w_exp_up[e], w_exp_down[e])
        return acc.at[tok].add(y * g[:, None]), None

    acc0 = jnp.zeros((T + 1, D), h.dtype)
    acc, _ = lax.scan(step, acc0, (block_expert,
                                   buf_tok.reshape(NB, MOE_BLOCK),
                                   buf_gate.reshape(NB, MOE_BLOCK)))
    return acc[:T]


def setup_inputs(seed: int = 0) -> dict:
    key = jax.random.key(seed)
    ks = jax.random.split(key, 22)
    D = D_MODEL
    nrm = lambda k, shp, s: jax.random.normal(k, shp, jnp.float32) * s
    return {
        "x": nrm(ks[0], (BATCH, SEQ, D), 1.0),
        "c": nrm(ks[1], (BATCH, D), 1.0),
        "w_ada": nrm(ks[2], (D, 6 * D), 0.5 * D ** -0.5),
        "b_ada": nrm(ks[3], (6 * D,), 0.01),
        "norm1_g": 1.0 + nrm(ks[4], (D,), 0.02),
        "w_in": nrm(ks[5], (D, IN_WIDTH), D ** -0.5),
        "b_gate": nrm(ks[6], (N_BRANCHES * D,), 0.01),
        "qn_g": 1.0 + nrm(ks[7], (HEAD_DIM,), 0.02),
        "kn_g": 1.0 + nrm(ks[8], (HEAD_DIM,), 0.02),
        "w_o_dil": nrm(ks[9], (DIL_OUT_WIDTH, D), DIL_OUT_WIDTH ** -0.5),
        "w_o_gqa": nrm(ks[10], (GQA_Q_WIDTH, D), GQA_Q_WIDTH ** -0.5),
        "w_out": nrm(ks[11], (D, D), D ** -0.5),
        "norm2_g": 1.0 + nrm(ks[12], (D,), 0.02),
        "router_w": nrm(ks[13], (D, N_EXPERTS), D ** -0.5),
        "router_bias": nrm(ks[14], (N_EXPERTS,), 0.01),
        "w_exp_gate": nrm(ks[15], (N_EXPERTS, D, EXPERT_FF), D ** -0.5),
        "w_exp_up": nrm(ks[16], (N_EXPERTS, D, EXPERT_FF), D ** -0.5),
        "w_exp_down": nrm(ks[17], (N_EXPERTS, EXPERT_FF, D), EXPERT_FF ** -0.5),
        "w_sh_gate": nrm(ks[18], (D, SHARED_FF), D ** -0.5),
        "w_sh_up": nrm(ks[19], (D, SHARED_FF), D ** -0.5),
        "w_sh_down": nrm(ks[20], (SHARED_FF, D), SHARED_FF ** -0.5),
        "final_g": 1.0 + nrm(ks[21], (D,), 0.02),
    }


def reference(x, c, w_ada, b_ada, norm1_g, w_in, b_gate, qn_g, kn_g, w_o_dil, w_o_gqa,
              w_out, norm2_g, router_w, router_bias, w_exp_gate, w_exp_up, w_exp_down,
              w_sh_gate, w_sh_up, w_sh_down, final_g):
    B, S, D = x.shape
    hd = HEAD_DIM
    rows = S // GRID_W
    t = jnp.arange(S, dtype=jnp.int32)
    cos1, sin1 = rope_cos_sin(t, hd)
    row_pos = jnp.repeat(jnp.arange(rows, dtype=jnp.int32), GRID_W)
    col_pos = jnp.tile(jnp.arange(GRID_W, dtype=jnp.int32), rows)
    cos_r, sin_r = rope_cos_sin(row_pos, hd // 2)
    cos_c, sin_c = rope_cos_sin(col_pos, hd // 2)

    for _ in range(DEPTH):
        mod = (jax.nn.silu(c) @ w_ada + b_ada).reshape(B, 6, D)
        shift1, scale1, gate1, shift2, scale2, gate2 = [mod[:, i][:, None, :] for i in range(6)]

        h = rms_norm(x, norm1_g) * (1.0 + scale1) + shift1
        proj = h @ w_in
        o0 = 0
        qa = proj[..., o0:o0 + DIL_WIDTH]; o0 += DIL_WIDTH
        ka = proj[..., o0:o0 + DIL_WIDTH]; o0 += DIL_WIDTH
        va = proj[..., o0:o0 + DIL_WIDTH]; o0 += DIL_WIDTH
        qb = proj[..., o0:o0 + GQA_Q_WIDTH]; o0 += GQA_Q_WIDTH
        kb = proj[..., o0:o0 + GQA_KV_WIDTH]; o0 += GQA_KV_WIDTH
        vb = proj[..., o0:o0 + GQA_KV_WIDTH]; o0 += GQA_KV_WIDTH
        gates = jax.nn.sigmoid(proj[..., o0:o0 + N_BRANCHES * D] + b_gate)
        g_a, g_b = gates[..., :D], gates[..., D:]

        dshape = (B, S, N_DIL_GROUPS, DIL_HEADS, hd)
        qa = apply_rope(qa.reshape(dshape), cos1, sin1)
        ka = apply_rope(ka.reshape(dshape), cos1, sin1)
        va = va.reshape(dshape)
        y_a = dilated_mixture(qa, ka, va) @ w_o_dil

        qb = rms_norm(qb.reshape(B, S, GQA_Q_HEADS, hd), qn_g)
        kb = rms_norm(kb.reshape(B, S, GQA_KV_HEADS, hd), kn_g)
        vb = vb.reshape(B, S, GQA_KV_HEADS, hd)
        half = hd // 2
        qb = jnp.concatenate([apply_rope(qb[..., :half], cos_r, sin_r),
                              apply_rope(qb[..., half:], cos_c, sin_c)], axis=-1)
        kb = jnp.concatenate([apply_rope(kb[..., :half], cos_r, sin_r),
                              apply_rope(kb[..., half:], cos_c, sin_c)], axis=-1)
        y_b = gqa_blocked(qb, kb, vb) @ w_o_gqa

        mixed = (g_a * y_a + g_b * y_b) @ w_out
        x = x + gate1 * mixed

        h2 = (rms_norm(x, norm2_g) * (1.0 + scale2) + shift2).reshape(B * S, D)
        ffn = swiglu(h2, w_sh_gate, w_sh_up, w_sh_down) + routed_moe(
            h2, router_w, router_bias, w_exp_gate, w_exp_up, w_exp_down)
        x = x + gate2 * ffn.reshape(B, S, D)

    return rms_norm(x, final_g)
```

```python
import contextlib
import numpy as np
import ml_dtypes
import concourse.bass as bass
import concourse.mybir as mybir
from concourse.bass_utils import run_bass_kernel_spmd

F32 = mybir.dt.float32
BF16 = mybir.dt.bfloat16
I32 = mybir.dt.int32
AF = mybir.ActivationFunctionType
ALU = mybir.AluOpType

_DSIZE = {F32: 4, BF16: 2, I32: 4}
GRAN = 256
SEM_LIM = 30000

D = 1024
S = 2048
NT = S // 128
NE = 256
ENC = 8192
MAXB = 64
EPS = 1e-6


class Op:
    __slots__ = ("eng", "fn", "waits", "venue", "idx", "clock", "inc")


def _region(ap):
    dsz = _DSIZE[ap.dtype]
    pairs = list(ap.ap)
    pstep = pairs[0][0]
    off = ap.offset
    if pstep > 0:
        off = off % pstep
    lo = off
    hi = off
    for (st, cnt) in pairs[1:]:
        if st >= 0:
            hi += st * (cnt - 1)
        else:
            lo += st * (cnt - 1)
    return (ap.tensor.name, lo * dsz, (hi + 1) * dsz)


class Prog:
    ENGS = ["pe", "act", "dve", "pool", "sp"]

    def __init__(self, nc):
        self.nc = nc
        self.ops = {e: [] for e in self.ENGS}
        self.know = {e: {} for e in self.ENGS}
        self.last_w = {}
        self.readers = {}
        self.dram_w = {}
        self.dram_r = {}
        self.venue_cnt = {}
        self.venue_last = {}
        self.streams = {}

    def _grans(self, ap):
        name, lo, hi = _region(ap)
        if name.startswith("ps"):
            return [(name, 0)]
        return [(name, g) for g in range(lo // GRAN, (hi - 1) // GRAN + 1)]

    def _dep(self, eng, d, waits):
        if d is None:
            return
        k = self.know[eng]
        if k.get(d.venue, 0) >= d.idx:
            return
        if d.venue == eng and eng == "pe":
            return
        waits.append(d)
        d.inc = True
        for v, n in d.clock.items():
            if k.get(v, 0) < n:
                k[v] = n

    def add(self, eng, fn, reads=(), writes=(), dr=(), dw=(), dma=None):
        op = Op()
        op.eng = eng
        op.fn = fn
        op.inc = False
        waits = []
        rg = []
        for ap in reads:
            rg += self._grans(ap)
        wg = []
        for ap in writes:
            wg += self._grans(ap)
        for g in rg:
            self._dep(eng, self.last_w.get(g), waits)
            if g[0].startswith("ps"):
                for r in self.readers.get(g, ()):
                    if r.eng != eng:
                        self._dep(eng, r, waits)
        for g in wg:
            self._dep(eng, self.last_w.get(g), waits)
            for r in self.readers.get(g, ()):
                self._dep(eng, r, waits)
        for key in dr:
            for w in self.dram_w.get(key, {}).values():
                self._dep(eng, w, waits)
        for key in dw:
            for r in self.dram_r.get(key, {}).values():
                self._dep(eng, r, waits)
        if dma is not None:
            st = self.streams[dma]
            assert st["eng"] == eng
            k = st["n"] % st["nsem"]
            st["n"] += 1
            venue = "dma:%s:%d" % (dma, k)
            self._dep(eng, self.venue_last.get(venue), waits)
            op.inc = True
        else:
            venue = eng
        op.venue = venue
        op.idx = self.venue_cnt.get(venue, 0) + 1
        self.venue_cnt[venue] = op.idx
        self.venue_last[venue] = op
        op.waits = waits
        ck = dict(self.know[eng])
        ck[venue] = op.idx
        op.clock = ck
        for g in rg:
            self.readers.setdefault(g, []).append(op)
        for g in wg:
            self.last_w[g] = op
            self.readers[g] = []
        for key in dr:
            self.dram_r.setdefault(key, {})[venue] = op
        for key in dw:
            self.dram_w.setdefault(key, {})[venue] = op
        self.ops[eng].append(op)
        return op

    def stream(self, name, eng, nsem):
        self.streams[name] = {"eng": eng, "nsem": nsem, "n": 0}

    def emit(self):
        nc = self.nc
        semval = {}
        counts = {}
        for eng in self.ENGS:
            for op in self.ops[eng]:
                if op.inc:
                    c = counts.get(op.venue, 0) + 1
                    counts[op.venue] = c
                    semval[id(op)] = c
        step = {}
        nsems = {}
        for v, c in counts.items():
            s = 16 if v.startswith("dma:") else 1
            step[v] = s
            lim = SEM_LIM // s
            nsems[v] = (c - 1) // lim + 1
        self.total_sems = sum(nsems.values())
        with contextlib.ExitStack() as es:
            sems = {}
            for v, n in nsems.items():
                sems[v] = [es.enter_context(nc.semaphore(("s_%s_%d" % (v, i)).replace(":", "_")))
                           for i in range(n)]

            def semof(op):
                c = semval[id(op)]
                s = step[op.venue]
                lim = SEM_LIM // s
                return sems[op.venue][(c - 1) // lim], ((c - 1) % lim + 1) * s

            def run(eng, e):
                for op in self.ops[eng]:
                    for d in op.waits:
                        sm, val = semof(d)
                        e.wait_ge(sm, val)
                    if op.fn is None:
                        continue
                    ins = op.fn(e)
                    if op.inc:
                        sm, val = semof(op)
                        ins.then_inc(sm, step[op.venue])

            with nc.Block() as block:
                @block.tensor
                def _(e):
                    run("pe", e)

                @block.scalar
                def _(e):
                    run("act", e)

                @block.vector
                def _(e):
                    run("dve", e)

                @block.gpsimd
                def _(e):
                    run("pool", e)

                @block.sync
                def _(e):
                    run("sp", e)


DIL = (1, 4, 16)
REACH = (64, 256, 1024)
MC0 = tuple(r + 511 for r in REACH)
MW = tuple(2 * r + 1150 for r in REACH)
MOFF = (0, MW[0], MW[0] + MW[1])
MTOT = sum(MW)


def _host_consts():
    bf = ml_dtypes.bfloat16
    c = {}
    c["identb"] = np.eye(128, dtype=np.float32).astype(bf)
    c["identf"] = np.eye(128, dtype=np.float32)
    r1 = np.zeros((128, 128), np.float32)
    ra = np.zeros((128, 128), np.float32)
    for m in range(128):
        dm = m % 64
        base = m - dm
        if dm < 32:
            r1[base + dm + 32, m] = -1.0
        else:
            r1[base + dm - 32, m] = 1.0
        sub = dm % 32
        blk = dm - sub
        if sub < 16:
            ra[base + blk + sub + 16, m] = -1.0
        else:
            ra[base + blk + sub - 16, m] = 1.0
    c["rm1"] = r1.astype(bf)
    c["rma"] = ra.astype(bf)
    bo = np.zeros((128, 128), np.float32)
    bo[:64, :64] = 1.0
    bo[64:, 64:] = 1.0
    c["bones"] = bo.astype(bf)
    c["utri"] = np.triu(np.ones((128, 128), np.float32), 1).astype(bf)
    c["ones"] = np.ones((128, 128), np.float32).astype(bf)
    t = np.arange(S, dtype=np.float32)
    inv1 = (np.float32(10000.0) ** (-np.arange(0, 64, 2, dtype=np.float32) / np.float32(64))).astype(np.float32)
    inva = (np.float32(10000.0) ** (-np.arange(0, 32, 2, dtype=np.float32) / np.float32(32))).astype(np.float32)
    row = (np.arange(S) // 64).astype(np.float32)
    col = (np.arange(S) % 64).astype(np.float32)
    cos1 = np.zeros((128, S), np.float32)
    sin1 = np.zeros((128, S), np.float32)
    cosa = np.zeros((128, S), np.float32)
    sina = np.zeros((128, S), np.float32)
    for p in range(128):
        dm = p % 64
        ang = (t * inv1[dm % 32]).astype(np.float32)
        cos1[p] = np.cos(ang)
        sin1[p] = np.sin(ang)
        if dm < 32:
            ang = (row * inva[dm % 16]).astype(np.float32)
        else:
            ang = (col * inva[(dm - 32) % 16]).astype(np.float32)
        cosa[p] = np.cos(ang)
        sina[p] = np.sin(ang)
    c["rope"] = np.stack([cos1, sin1, cosa, sina], axis=1).astype(bf)
    strips = []
    for g in range(3):
        i = np.arange(128)[:, None]
        cc = np.arange(MW[g])[None, :]
        dlt = i - cc + MC0[g]
        ok = (np.abs(dlt) <= REACH[g]) & (dlt % DIL[g] == 0)
        strips.append(ok.astype(np.float32))
    c["mstrip"] = np.concatenate(strips, axis=1).astype(bf)
    c["eidx"] = np.tile((np.arange(NE, dtype=np.float32) * ENC + 1.0)[None, :], (128, 1)).astype(np.float32)
    c["eio"] = np.tile(np.arange(NE, dtype=np.float32)[None, :], (128, 1)).astype(np.float32)
    return c


def build(nseq=4, upto="all", dbg=()):
    nc = bass.Bass("TRN2", target_bir_lowering=False)
    NTOK = nseq * S
    NTT = nseq * NT

    def din(name, shape, dt=F32):
        return nc.dram_tensor(name, list(shape), dt, kind="ExternalInput").ap()

    x_d = din("x", [NTOK, D])
    cT_d = din("cT", [128, 8, nseq])
    w_ada = din("w_ada", [D, 6 * D])
    b_ada = din("b_ada", [1, 6 * D])
    n1g_d = din("n1g", [128, 8])
    n2g_d = din("norm2_g", [1, D])
    fg_d = din("final_g", [1, D])
    w_in = din("w_in", [D, 5888])
    bgc_d = din("bgc", [128, 16])
    qng_d = din("qng2", [128, 1])
    kng_d = din("kng2", [128, 1])
    w_o_dil = din("w_o_dil", [256, D])
    w_o_gqa = din("w_o_gqa", [D, D])
    w_out = din("w_out", [D, D])
    router_w = din("router_w", [D, NE])
    rbias_d = din("router_bias", [1, NE])
    w_eg_h = nc.dram_tensor("w_exp_gate", [NE, D, 256], F32, kind="ExternalInput")
    w_eu_h = nc.dram_tensor("w_exp_up", [NE, D, 256], F32, kind="ExternalInput")
    w_ed_h = nc.dram_tensor("w_exp_down", [NE, 256, D], F32, kind="ExternalInput")
    w_sg = din("w_sh_gate", [D, 256])
    w_su = din("w_sh_up", [D, 256])
    w_sd = din("w_sh_down", [256, D])
    identb_d = din("identb", [128, 128], BF16)
    identf_d = din("identf", [128, 128])
    rm1_d = din("rm1", [128, 128], BF16)
    rma_d = din("rma", [128, 128], BF16)
    bones_d = din("bones", [128, 128], BF16)
    utri_d = din("utri", [128, 128], BF16)
    ones_d = din("ones", [128, 128], BF16)
    rope_d = din("rope", [128, 4, S], BF16)
    mstrip_d = din("mstrip", [128, MTOT], BF16)
    eidx_d = din("eidx", [128, NE])
    out_d = nc.dram_tensor("out", [NTOK, D], F32, kind="ExternalOutput").ap()
    modd = nc.dram_tensor("modd", [nseq, 6 * D], F32, kind="Internal").ap()
    NB = NTOK * 8 // 128 + NE
    NBJ = NB // 128
    XS = nc.dram_tensor("xs", [NB * 128, D], BF16, kind="Internal").ap()
    H2S = nc.dram_tensor("h2s", [NTOK, D], BF16, kind="Internal").ap()
    eio_d = din("eio", [128, NE])
    bidx_d = din("bidx", [128, NBJ])
    PART = nc.dram_tensor("part", [NTOK, D], F32, kind="Internal").ap()

    P = Prog(nc)
    P.stream("sp_c", "sp", 4)
    P.stream("sp_x", "sp", 2)
    P.stream("sp_w", "sp", 4)
    P.stream("sp_st", "sp", 4)
    P.stream("sp_z", "sp", 4)
    P.stream("pw", "pool", 6)
    P.stream("pwg", "pool", 12)
    P.stream("psc", "pool", 8)
    P.stream("pg", "pool", 8)
    dbg_outs = {}

    with contextlib.ExitStack() as es:
        ARENA_EL = 103 * 1024 + 512
        A = es.enter_context(nc.sbuf_tensor("arena", [128, ARENA_EL], BF16))
        PS = [es.enter_context(nc.psum_tensor("ps%d" % i, [128, 512], F32)) for i in range(8)]
        cur = [0]

        def alloc(nbytes):
            o = cur[0]
            cur[0] = (o + nbytes + 63) // 64 * 64
            assert cur[0] <= ARENA_EL * 2, ("SBUF overflow", cur[0])
            return o

        def view(off, shape, dt=BF16):
            n = 1
            for s_ in shape:
                n *= s_
            nb = n * _DSIZE[dt]
            assert off % 4 == 0
            ap = A[:, off // 2: off // 2 + nb // 2]
            if dt != BF16:
                ap = ap.bitcast(dt)
            if len(shape) == 2:
                ap = ap.rearrange("p (a b) -> p a b", a=shape[0])
            elif len(shape) == 3:
                ap = ap.rearrange("p (a b c) -> p a b c", a=shape[0], b=shape[1])
            return ap

        def new(shape, dt=BF16):
            n = 1
            for s_ in shape:
                n *= s_
            return view(alloc(n * _DSIZE[dt]), shape, dt)

        def aps(*xs):
            return [a for a in xs if a is not None and not isinstance(a, (int, float))]

        def MM(out, lhsT, rhs, start, stop):
            P.add("pe", lambda e: e.matmul(out, lhsT=lhsT, rhs=rhs, start=start, stop=stop),
                  reads=[lhsT, rhs], writes=[out])

        def TR(out, in_, ident):
            P.add("pe", lambda e: e.transpose(out=out, in_=in_, identity=ident), reads=[in_, ident], writes=[out])

        def ACT(out, in_, func, bias=None, scale=None, accum=None):
            kw = {}
            if bias is not None:
                kw["bias"] = bias
            if scale is not None:
                kw["scale"] = scale
            if accum is not None:
                kw["accum_out"] = accum
            P.add("act", lambda e: e.activation(out=out, in_=in_, func=func, **kw),
                  reads=aps(in_, bias, scale), writes=aps(out, accum))

        def TT(eng, out, in0, in1, op):
            P.add(eng, lambda e: e.tensor_tensor(out=out, in0=in0, in1=in1, op=op), reads=[in0, in1], writes=[out])

        def TS(eng, out, in0, s1, s2, op0, op1=None, accum=None):
            if accum is not None:
                P.add(eng, lambda e: e.tensor_scalar(out=out, in0=in0, scalar1=s1, scalar2=s2, op0=op0, op1=op1, accum_out=accum),
                      reads=aps(in0, s1, s2), writes=[out, accum])
            elif op1 is None:
                P.add(eng, lambda e: e.tensor_scalar(out=out, in0=in0, scalar1=s1, scalar2=None, op0=op0),
                      reads=aps(in0, s1), writes=[out])
            else:
                P.add(eng, lambda e: e.tensor_scalar(out=out, in0=in0, scalar1=s1, scalar2=s2, op0=op0, op1=op1),
                      reads=aps(in0, s1, s2), writes=[out])

        def STT(out, in0, scalar, in1, op0, op1, accum=None):
            kw = {}
            if accum is not None:
                kw["accum_out"] = accum
            P.add("dve", lambda e: e.scalar_tensor_tensor(out=out, in0=in0, scalar=scalar, in1=in1, op0=op0, op1=op1, **kw),
                  reads=aps(in0, scalar, in1), writes=aps(out, accum))

        def CP(eng, out, in_):
            if eng == "act":
                P.add("act", lambda e: e.copy(out=out, in_=in_), reads=[in_], writes=[out])
            else:
                P.add(eng, lambda e: e.tensor_copy(out=out, in_=in_), reads=[in_], writes=[out])

        def MAX8(out, in_):
            P.add("dve", lambda e: e.max(out=out, in_=in_), reads=[in_], writes=[out])

        def RECIP(out, in_):
            P.add("dve", lambda e: e.reciprocal(out=out, in_=in_), reads=[in_], writes=[out])

        def MEMSET(eng, ap, val):
            P.add(eng, lambda e: e.memset(ap, val), writes=[ap])

        def LD(stream, out, in_, dr=(), **kw):
            eng = P.streams[stream]["eng"]
            P.add(eng, lambda e: e.dma_start(out=out, in_=in_, **kw), writes=[out], dr=dr, dma=stream)

        def ST(stream, out, in_, dw=(), **kw):
            eng = P.streams[stream]["eng"]
            P.add(eng, lambda e: e.dma_start(out=out, in_=in_, **kw), reads=[in_], dw=dw, dma=stream)

        def DUMP(name, ap, dt=None):
            if name not in dbg:
                return
            shp = list(ap.shape)
            d_ = nc.dram_tensor("dbg_" + name, shp, ap.dtype, kind="ExternalOutput").ap()
            dbg_outs[name] = d_
            ST("sp_st", d_, ap, dw=["dbg"])

        def rstd_from_ssq(out, ssq, n, tmp):
            ACT(tmp, ssq, AF.Ln, bias=epsc[:, 0:1], scale=1.0 / n)
            ACT(out, tmp, AF.Exp, scale=-0.5)

        identb = new([128]); rm1 = new([128]); rma = new([128]); bones = new([128])
        utri = new([128]); ones = new([128])
        identf = new([128], F32); onesf = new([128], F32)
        rope = new([4, S])
        mstrip = new([MTOT])
        n1g = new([8], F32); bgc = new([16], F32); qng = new([1], F32); kng = new([1], F32)
        epsc = new([1], F32)
        eidx = new([NE], F32); rbias = new([NE], F32); eio = new([NE], F32)
        gts = new([NTT, 8], F32); cnt = new([NE], F32)
        ef8 = new([NTT, 8], F32); pf8 = new([NTT, 8], F32)
        gs1c = new([8], F32); sh1c = new([8], F32); sc1c = new([8], F32)
        small = new([64], F32)

        o_hT = alloc(8 * S * 2)
        o_QK = alloc(12 * S * 2)
        o_U1 = alloc(16384 * 2)
        o_vb = alloc(NT * 4 * 65 * 2)
        o_oA = alloc(2 * S * 2)
        hT = view(o_hT, [8, S])
        QK = view(o_QK, [12, S])
        va = view(o_U1, [NT, 12, 65])
        oB = view(o_U1, [8, S])
        vb = view(o_vb, [NT, 4, 65])
        oA = view(o_oA, [2, S])

        xt = new([D], F32)
        xn = new([D])
        wch = [new([8, 256]) for _ in range(4)]
        PT = [new([512]) for _ in range(2)]
        tb1 = [new([512]) for _ in range(2)]
        tsq = new([512])
        o_tf = cur[0]
        tf = [new([512], F32) for _ in range(3)]
        junk = view(o_tf, [D], F32)
        zt = new([D])
        SB_END = cur[0]

        psT3 = PS[7][:, :].bitcast(BF16).rearrange("p (a b) -> p a b", a=8)
        psT3b = PS[6][:, :].bitcast(BF16).rearrange("p (a b) -> p a b", a=8)

        def stat(i):
            return small[:, (i % 4) * 16:(i % 4) * 16 + 16]

        LD("sp_c", identb, identb_d); LD("sp_c", rm1, rm1_d); LD("sp_c", rma, rma_d)
        LD("sp_c", bones, bones_d); LD("sp_c", utri, utri_d); LD("sp_c", ones, ones_d)
        LD("sp_c", identf, identf_d); LD("sp_c", rope, rope_d); LD("sp_c", mstrip, mstrip_d)
        LD("sp_c", n1g, n1g_d); LD("sp_c", bgc, bgc_d); LD("sp_c", qng, qng_d); LD("sp_c", kng, kng_d)
        LD("sp_c", eidx, eidx_d)
        LD("sp_c", eio, eio_d)
        LD("sp_c", rbias, rbias_d.partition_broadcast(128))
        MEMSET("dve", epsc, EPS)
        MEMSET("dve", cnt, 0.0)
        MEMSET("dve", onesf, 1.0)
        MEMSET("pool", zt, 0.0)
        zfill = [0]
        NZ = NB
        MEMSET("pool", vb[:, :, :, 64:65], 1.0)

        sct = view(o_QK, [8, nseq], F32)
        LD("sp_c", sct, cT_d)
        ACT(sct, sct, AF.Silu)
        wst = [view(o_hT, [8, 512], F32), view(o_hT + 16384, [8, 512], F32)]
        mrow = [view(o_U1, [512], F32), view(o_U1 + 2048, [512], F32)]
        brow = view(o_U1 + 4096, [6 * D], F32)
        LD("sp_c", brow[0:nseq, :], b_ada.partition_broadcast(nseq))
        for blk in range(12):
            wb_ = wst[blk % 2]
            LD("sp_w", wb_, w_ada[:, blk * 512:(blk + 1) * 512].rearrange("(kc p) n -> p kc n", p=128))
            pm = PS[blk % 2]
            for kc in range(8):
                MM(pm[0:nseq, :], sct[:, kc, :], wb_[:, kc, :], kc == 0, kc == 7)
            mr = mrow[blk % 2]
            TT("dve", mr[0:nseq, :], pm[0:nseq, :], brow[0:nseq, blk * 512:(blk + 1) * 512], ALU.add)
            ST("sp_st", modd[:, blk * 512:(blk + 1) * 512], mr[0:nseq, :], dw=["modd"])

        def load_wblock(buf, col0, ncols, dst0=0, src=None, kcn=8):
            src = w_in if src is None else src
            LD("pw", buf[:, 0:kcn, dst0:dst0 + ncols],
               src[:, col0:col0 + ncols].rearrange("(kc p) n -> p kc n", p=128))

        pacc_i = [0]

        def proj_fm(wbuf, c0, tb):
            pm = PS[pacc_i[0] % 2]
            pacc_i[0] += 1
            for kc in range(8):
                MM(pm[:, :], wbuf[:, kc, c0:c0 + 128], hT[:, kc, tb * 512:(tb + 1) * 512], kc == 0, kc == 7)
            return pm

        def rope_plain(pm, dst, tb, ti):
            tok = slice(tb * 512, (tb + 1) * 512)
            qg = tb1[ti % 2]
            CP("act", qg, pm[:, :])
            pr = PS[2 + ti % 2]
            MM(pr[:, :], rm1, qg, True, True)
            TT("dve", tf[0], pm[:, :], rope[:, 0, tok], ALU.mult)
            TT("dve", tf[1], pr[:, :], rope[:, 1, tok], ALU.mult)
            TT("dve", dst, tf[0], tf[1], ALU.add)

        def rope_norm(pm, dst, tb, ti, gcol):
            tok = slice(tb * 512, (tb + 1) * 512)
            ACT(tsq, pm[:, :], AF.Square)
            qg = tb1[ti % 2]
            ACT(qg, pm[:, :], AF.Copy, scale=gcol[:, 0:1])
            pq = PS[2]
            pr = PS[3]
            MM(pq[:, :], bones, tsq, True, True)
            MM(pr[:, :], rma, qg, True, True)
            ACT(tf[2], pq[:, :], AF.Ln, bias=epsc[:, 0:1], scale=1.0 / 64.0)
            ACT(tf[2], tf[2], AF.Exp, scale=-0.5)
            TT("dve", tf[0], qg, rope[:, 2, tok], ALU.mult)
            TT("dve", tf[1], pr[:, :], rope[:, 3, tok], ALU.mult)
            TT("dve", tf[0], tf[0], tf[1], ALU.add)
            TT("dve", dst, tf[0], tf[2], ALU.mult)

        def attention(branches, dst_chunk, dst_half):
            for qc in range(4):
                po = PS[4 + (qc % 2)]
                blocks = []
                for (q_fn, k_fn, v_fn, g) in branches:
                    for kb in range(NT):
                        if g is not None:
                            delta = kb * 128 - qc * 512
                            if delta + 127 < -REACH[g] or delta - 511 > REACH[g]:
                                continue
                        blocks.append((q_fn, k_fn, v_fn, g, kb))
                nb = len(blocks)
                for i, (q_fn, k_fn, v_fn, g, kb) in enumerate(blocks):
                    psc = PS[i % 3]
                    MM(psc[:, :], k_fn(kb), q_fn(qc), True, True)
                    pt = PT[i % 2]
                    ACT(pt, psc[:, :], AF.Exp, scale=0.125)
                    if g is not None:
                        c0 = MOFF[g] + MC0[g] - (kb * 128 - qc * 512)
                        TT("dve", pt, pt, mstrip[:, c0:c0 + 512], ALU.mult)
                    MM(po[0:65, :], v_fn(kb), pt, i == 0, i == nb - 1)
                rr = tf[2]
                RECIP(rr[64:65, :], po[64:65, :])
                pb_ = PS[6]
                MM(pb_[0:64, :], onesf[64:65, 0:64], rr[64:65, :], True, True)
                CP("act", tf[0][0:64, :], pb_[0:64, :])
                TT("dve", dst_chunk[dst_half * 64:(dst_half + 1) * 64, qc * 512:(qc + 1) * 512],
                   po[0:64, :], tf[0][0:64, :], ALU.mult)

        g1bc = view(o_hT, [D], F32); gs2bc = view(o_hT + 4096, [D], F32); sh2bc = view(o_hT + 8192, [D], F32)
        g2bc_t = view(o_hT + 12288, [D], F32); n2gbc = view(o_hT + 16384, [D], F32)
        x1 = view(o_hT + 20480, [D], F32); h2 = view(o_hT + 24576, [D], F32)
        h2b = view(o_hT + 28672, [D]); h2Tb = view(o_hT + 30720, [8, 128])
        wshgu = view(o_U1, [8, 512]); wshd = view(o_U1 + 8192, [2, 1024])
        routw = view(o_U1 + 12288, [8, NE], F32); h2T = view(o_U1 + 20480, [8, 128], F32)
        r_sc = view(o_U1 + 24576, [NE], F32); r_bi = view(o_U1 + 25600, [NE], F32)
        r_ma = view(o_U1 + 26624, [NE], F32); r_se = view(o_U1 + 27648, [NE], F32)
        r_ga = view(o_U1 + 28672, [NE], F32); r_D = view(o_U1 + 29696, [NE], F32)
        r_selb = view(o_U1 + 30720, [NE]); r_m8 = view(o_U1 + 31232, [8, 8], F32)
        r_gs = view(o_U1 + 31488, [8], F32); r_gs8 = view(o_U1 + 31520, [8], F32); r_gm = view(o_U1 + 31552, [8], F32)
        r_pen = view(o_U1 + 31584, [8], F32); r_t8 = view(o_U1 + 31616, [8], F32); r_s8 = view(o_U1 + 31648, [8], F32)
        r_HT = view(o_U1 + 31744, [2, 128])
        wout = view(o_QK + 8 * S * 2, [8, D])
        sgs = tf[0]

        def do_seq(s):
            tok0 = s * S
            LD("sp_c", sh1c, modd[s:s + 1, 0:D].rearrange("o (j p) -> p (o j)", p=128), dr=["modd"],
               allow_slow_non_contiguous=True)
            LD("sp_c", sc1c, modd[s:s + 1, D:2 * D].rearrange("o (j p) -> p (o j)", p=128), dr=["modd"],
               allow_slow_non_contiguous=True)
            STT(gs1c, sc1c, 1.0, n1g, ALU.add, ALU.mult)
            MEMSET("pool", va[:, :, :, 64:65], 1.0)
            for t in range(NT):
                st_ = stat(t)
                LD("sp_x", xt, x_d[tok0 + t * 128: tok0 + (t + 1) * 128, :])
                for _z in range(-(-NZ // NTT)):
                    if zfill[0] < NZ:
                        z0 = zfill[0] * 128
                        ST("sp_z", XS[z0:z0 + 128, :], zt, dw=["xsz"])
                        zfill[0] += 1
                STT(junk, xt, 1.0, xt, ALU.mult, ALU.mult, accum=st_[:, 0:1])
                ACT(st_[:, 1:2], st_[:, 0:1], AF.Ln, bias=epsc[:, 0:1], scale=1.0 / D)
                ACT(st_[:, 2:3], st_[:, 1:2], AF.Exp, scale=-0.5)
                ACT(xn, xt, AF.Copy, scale=st_[:, 2:3])
                for c in range(8):
                    TR(psT3[:, c, :], xn[:, c * 128:(c + 1) * 128], identb)
                for c in range(8):
                    TS("dve", hT[:, c, t * 128:(t + 1) * 128], psT3[:, c, :],
                       gs1c[:, c:c + 1], sh1c[:, c:c + 1], ALU.mult, ALU.add)
            DUMP("hT", hT)
            if upto == "A1":
                return
            ti = 0
            for bi in range(6):
                wb_ = wch[bi % 4]
                load_wblock(wb_, bi * 256, 256)
                for c2 in range(2):
                    ch = bi * 2 + c2
                    for tb in range(4):
                        pm = proj_fm(wb_, c2 * 128, tb)
                        rope_plain(pm, QK[:, ch, tb * 512:(tb + 1) * 512], tb, ti)
                        ti += 1
            DUMP("qaka", QK)
            if upto == "A2q":
                return
            for vbi in range(4):
                wb_ = wch[(2 + vbi) % 4]
                load_wblock(wb_, (1536 + vbi * 256) if vbi < 3 else 3584, 256)
                for t in range(NT):
                    pm = PS[pacc_i[0] % 2]
                    pacc_i[0] += 1
                    for kc in range(8):
                        MM(pm[:, 0:256], hT[:, kc, t * 128:(t + 1) * 128], wb_[:, kc, :], kc == 0, kc == 7)
                    src = pm[:, 0:256].rearrange("p (h d) -> p h d", h=4)
                    if vbi < 3:
                        CP("act" if t % 2 == 0 else "dve", va[:, t, vbi * 4:(vbi + 1) * 4, 0:64], src)
                    else:
                        CP("act" if t % 2 == 0 else "dve", vb[:, t, :, 0:64], src)
            DUMP("va", va); DUMP("vb", vb)
            if upto == "A2":
                return
            for h in range(4):
                br = []
                for g in range(3):
                    hh = g * 4 + h
                    chk, hf = hh // 2, hh % 2
                    br.append((
                        (lambda qc, chk=chk, hf=hf: QK[hf * 64:(hf + 1) * 64, chk, qc * 512:(qc + 1) * 512]),
                        (lambda kb, chk=chk, hf=hf: QK[hf * 64:(hf + 1) * 64, 6 + chk, kb * 128:(kb + 1) * 128]),
                        (lambda kb, hh=hh: va[:, kb, hh, :]),
                        g))
                attention(br, oA[:, h // 2, :], h % 2)
            DUMP("oA", oA)
            if upto == "dil":
                return
            ti = 0
            for bi in range(4):
                wb_ = wch[bi % 4]
                load_wblock(wb_, 2304 + bi * 256, 256)
                for c2 in range(2):
                    ch = bi * 2 + c2
                    for tb in range(4):
                        pm = proj_fm(wb_, c2 * 128, tb)
                        rope_norm(pm, QK[:, ch, tb * 512:(tb + 1) * 512], tb, ti, qng)
                        ti += 1
            for kv in range(4):
                wb_ = wch[kv % 4]
                load_wblock(wb_, 3328 + kv * 64, 64, dst0=0)
                load_wblock(wb_, 3328 + kv * 64, 64, dst0=64)
                for tb in range(4):
                    pm = proj_fm(wb_, 0, tb)
                    rope_norm(pm, QK[:, 8 + kv, tb * 512:(tb + 1) * 512], tb, ti, kng)
                    ti += 1
            DUMP("qbkb", QK)
            for hq in range(16):
                kv, chk, hf = hq // 4, hq // 2, hq % 2
                br = [(
                    (lambda qc, chk=chk, hf=hf: QK[hf * 64:(hf + 1) * 64, chk, qc * 512:(qc + 1) * 512]),
                    (lambda kb, kv=kv, hf=hf: QK[hf * 64:(hf + 1) * 64, 8 + kv, kb * 128:(kb + 1) * 128]),
                    (lambda kb, kv=kv: vb[:, kb, kv, :]),
                    None)]
                attention(br, oB[:, chk, :], hf)
            DUMP("oB", oB)
            if upto == "gqa":
                return
            for m in range(8):
                wg_ = wch[(m % 2) * 2]
                wo_ = wch[(m % 2) * 2 + 1]
                load_wblock(wg_, 3840 + m * 128, 128, dst0=0)
                load_wblock(wg_, 4864 + m * 128, 128, dst0=128)
                load_wblock(wo_, m * 128, 128, dst0=0, src=w_o_gqa)
                load_wblock(wo_, m * 128, 128, dst0=128, src=w_o_dil, kcn=2)
                for tb in range(4):
                    tok = slice(tb * 512, (tb + 1) * 512)
                    pa, pg, pb_, pg2 = PS[0], PS[1], PS[2], PS[3]
                    for c in range(2):
                        MM(pa[:, :], wo_[:, c, 128:256], oA[:, c, tok], c == 0, c == 1)
                    for kc in range(8):
                        MM(pg[:, :], wg_[:, kc, 0:128], hT[:, kc, tok], kc == 0, kc == 7)
                    ACT(tb1[0], pg[:, :], AF.Sigmoid, bias=bgc[:, m:m + 1])
                    TT("dve", tf[0], tb1[0], pa[:, :], ALU.mult)
                    for kc in range(8):
                        MM(pb_[:, :], wo_[:, kc, 0:128], oB[:, kc, tok], kc == 0, kc == 7)
                    for kc in range(8):
                        MM(pg2[:, :], wg_[:, kc, 128:256], hT[:, kc, tok], kc == 0, kc == 7)
                    ACT(tb1[1], pg2[:, :], AF.Sigmoid, bias=bgc[:, 8 + m:9 + m])
                    TT("dve", tf[1], tb1[1], pb_[:, :], ALU.mult)
                    TT("dve", QK[:, m, tok], tf[0], tf[1], ALU.add)
            DUMP("zT", QK)
            if upto == "A4":
                return
            LD("pw", wout, w_out.rearrange("(kc p) n -> p kc n", p=128))
            LD("pw", wshgu[:, :, 0:256], w_sg.rearrange("(kc p) n -> p kc n", p=128))
            LD("pw", wshgu[:, :, 256:512], w_su.rearrange("(kc p) n -> p kc n", p=128))
            LD("pw", wshd, w_sd.rearrange("(j p) n -> p j n", p=128))
            LD("sp_w", routw, router_w.rearrange("(kc p) n -> p kc n", p=128))
            LD("sp_c", g1bc, modd[s:s + 1, 2 * D:3 * D].partition_broadcast(128), dr=["modd"])
            LD("sp_c", sh2bc, modd[s:s + 1, 3 * D:4 * D].partition_broadcast(128), dr=["modd"])
            LD("sp_c", gs2bc, modd[s:s + 1, 4 * D:5 * D].partition_broadcast(128), dr=["modd"])
            LD("sp_c", g2bc_t, modd[s:s + 1, 5 * D:6 * D].partition_broadcast(128), dr=["modd"])
            LD("sp_c", n2gbc, n2g_d.partition_broadcast(128))
            STT(gs2bc, gs2bc, 1.0, n2gbc, ALU.add, ALU.mult)
            for t in range(NT):
                T = s * NT + t
                st_ = stat(t)
                for n in range(2):
                    for kc in range(8):
                        MM(PS[n][:, :], QK[:, kc, t * 128:(t + 1) * 128], wout[:, kc, n * 512:(n + 1) * 512], kc == 0, kc == 7)
                LD("sp_x", xt, x_d[tok0 + t * 128: tok0 + (t + 1) * 128, :])
                for n in range(2):
                    TT("dve", x1[:, n * 512:(n + 1) * 512], PS[n][:, :], g1bc[:, n * 512:(n + 1) * 512], ALU.mult)
                TT("pool", x1, x1, xt, ALU.add)
                STT(junk, x1, 1.0, x1, ALU.mult, ALU.mult, accum=st_[:, 0:1])
                ACT(st_[:, 1:2], st_[:, 0:1], AF.Ln, bias=epsc[:, 0:1], scale=1.0 / D)
                ACT(st_[:, 2:3], st_[:, 1:2], AF.Exp, scale=-0.5)
                STT(h2, x1, st_[:, 2:3], gs2bc, ALU.mult, ALU.mult)
                TT("pool", h2, h2, sh2bc, ALU.add)
                CP("act", h2b, h2)
                for c in range(8):
                    pX = PS[2 + c // 4][:, :].rearrange("p (a b) -> p a b", a=4)
                    TR(pX[:, c % 4, :], h2[:, c * 128:(c + 1) * 128], identf)
                for hf in range(2):
                    pX = PS[2 + hf][:, :].rearrange("p (a b) -> p a b", a=4)
                    CP("act", h2T[:, hf * 4:(hf + 1) * 4, :], pX)
                    CP("dve", h2Tb[:, hf * 4:(hf + 1) * 4, :], pX)
                pr = PS[4]
                for kc in range(8):
                    MM(pr[:, 0:NE], h2T[:, kc, :], routw[:, kc, :], kc == 0, kc == 7)
                ACT(r_sc, pr[:, 0:NE], AF.Sigmoid)
                TT("dve", r_bi, r_sc, rbias, ALU.add)
                for g in range(8):
                    MAX8(r_m8[:, g, :], r_bi[:, g * 32:(g + 1) * 32])
                TT("dve", r_gs, r_m8[:, :, 0], r_m8[:, :, 1], ALU.add)
                MAX8(r_gs8, r_gs)
                TS("dve", r_gm, r_gs, r_gs8[:, 3:4], None, ALU.is_ge)
                TS("dve", r_pen, r_gm, -1.0, 10.0, ALU.add, ALU.mult)
                for g in range(8):
                    TS("dve", r_ma[:, g * 32:(g + 1) * 32], r_bi[:, g * 32:(g + 1) * 32],
                       r_gm[:, g:g + 1], r_pen[:, g:g + 1], ALU.mult, ALU.add)
                MAX8(r_t8, r_ma)
                TS("dve", r_se, r_ma, r_t8[:, 7:8], None, ALU.is_ge)
                STT(r_ga, r_sc, 1.0, r_se, ALU.mult, ALU.mult, accum=st_[:, 4:5])
                RECIP(st_[:, 5:6], st_[:, 4:5])
                TS("dve", r_ga, r_ga, st_[:, 5:6], 2.5, ALU.mult, ALU.mult)
                CP("act", r_selb, r_se)
                pc = PS[5]
                MM(pc[:, 0:NE], utri, r_selb, True, True)
                MM(pc[:, NE:2 * NE], ones, r_selb, True, True)
                TT("dve", r_D, pc[:, 0:NE], cnt, ALU.add)
                TT("dve", r_D, r_D, eidx, ALU.add)
                TT("dve", r_D, r_D, r_se, ALU.mult)
                TT("dve", cnt, cnt, pc[:, NE:2 * NE], ALU.add)
                MAX8(r_s8, r_D)
                STT(r_ma, eio, 1.0, r_se, ALU.add, ALU.mult)
                MAX8(r_t8, r_ma)
                TS("dve", ef8[:, T, :], r_t8, -1.0, None, ALU.add)
                STT(pf8[:, T, :], ef8[:, T, :], -float(ENC), r_s8, ALU.mult, ALU.add)
                TS("dve", pf8[:, T, :], pf8[:, T, :], -1.0, None, ALU.add)
                for k in range(8):
                    STT(junk[:, 0:NE], r_D, r_s8[:, k:k + 1], r_ga, ALU.is_equal, ALU.mult, accum=gts[:, T, k:k + 1])
                ST("sp_st", H2S[T * 128:(T + 1) * 128, :], h2b, dw=["h2s"])
                pgu = PS[6]
                for j in range(4):
                    for kc in range(8):
                        MM(pgu[:, j * 128:(j + 1) * 128], wshgu[:, kc, j * 128:(j + 1) * 128], h2Tb[:, kc, :], kc == 0, kc == 7)
                ACT(sgs[:, 0:256], pgu[:, 0:256], AF.Silu)
                TT("dve", r_HT.rearrange("p a b -> p (a b)"), sgs[:, 0:256], pgu[:, 256:512], ALU.mult)
                for n in range(2):
                    for j in range(2):
                        MM(PS[n][:, :], r_HT[:, j, :], wshd[:, j, n * 512:(n + 1) * 512], j == 0, j == 1)
                for n in range(2):
                    TT("dve", h2[:, n * 512:(n + 1) * 512], PS[n][:, :], g2bc_t[:, n * 512:(n + 1) * 512], ALU.mult)
                TT("pool", h2, h2, x1, ALU.add)
                ST("sp_st", PART[T * 128:(T + 1) * 128, :], h2, dw=["part"])
                if t == 0:
                    DUMP("x1", x1); DUMP("rsc", r_sc); DUMP("rse", r_se); DUMP("rga", r_ga); DUMP("rD", r_D)

        for s in range(nseq):
            do_seq(s)
        DUMP("gts", gts)

        if upto in ("all", "C0", "C1"):
            oc = [o_hT]

            def cnew(shape, dt=BF16):
                n = 1
                for s_ in shape:
                    n *= s_
                o = oc[0]
                oc[0] = (o + n * _DSIZE[dt] + 63) // 64 * 64
                assert oc[0] <= SB_END
                return view(o, shape, dt)
            slots = cnew([NTT, 8], I32)
            o_after_slots = oc[0]
            nblk = cnew([NE], F32); pendb = cnew([NE], F32); onesr = cnew([NE], F32); bidx = cnew([NBJ], F32)
            pstart = cnew([NE], F32)
            t_e = cnew([NBJ], F32)
            ps8 = cnew([8], F32)
            h2r = [cnew([D]) for _ in range(2)]
            LD("sp_c", bidx, bidx_d)
            MEMSET("dve", onesr, 1.0)
            TS("dve", nblk, cnt, 0.0, None, ALU.is_gt)
            for j in range(1, MAXB):
                STT(nblk, cnt, float(128 * j), nblk, ALU.is_gt, ALU.add)
            P.add("dve", lambda e: e.tensor_tensor_scan(out=pendb, data0=onesr, data1=nblk, initial=0.0,
                                                          op0=ALU.mult, op1=ALU.add),
                  reads=[onesr, nblk], writes=[pendb])
            TT("dve", pstart, pendb, nblk, ALU.subtract)
            TS("dve", pstart, pstart, 128.0, None, ALU.mult)
            for j in range(NBJ):
                TS("dve", junk[:, 0:NE], pendb, bidx[:, j:j + 1], 0.0, ALU.is_le, ALU.add, accum=t_e[:, j:j + 1])
            TS("dve", t_e, t_e, float(NE - 1), None, ALU.min)
            idxg = [cnew([NB], I32) for _ in range(8)]
            idxd = [cnew([NB], I32) for _ in range(2)]
            pcol = cnew([10], F32)
            dg = cnew([128], F32)
            for kc in range(8):
                TS("dve", pcol[:, kc:kc + 1], bidx[:, 0:1], float(kc * 128), None, ALU.add)
            for j in range(2):
                TS("dve", pcol[:, 8 + j:9 + j], bidx[:, 0:1], float(j * 128), None, ALU.add)
            for j in range(NBJ):
                TS("dve", dg, identf, t_e[:, j:j + 1], None, ALU.mult)
                MM(PS[j % 2][:, 0:128], onesf, dg, True, True)
                for kc in range(8):
                    TS("dve", idxg[kc][:, j * 128:(j + 1) * 128], PS[j % 2][:, 0:128], 1024.0, pcol[:, kc:kc + 1], ALU.mult, ALU.add)
                for j2 in range(2):
                    TS("dve", idxd[j2][:, j * 128:(j + 1) * 128], PS[j % 2][:, 0:128], 256.0, pcol[:, 8 + j2:9 + j2], ALU.mult, ALU.add)
            DUMP("t_e", t_e); DUMP("cnt", cnt); DUMP("pstart", pstart)
            for T in range(NTT):
                b_ = T % 2
                LD("sp_x", h2r[b_], H2S[T * 128:(T + 1) * 128, :], dr=["h2s"])
                for k in range(8):
                    STT(junk[:, 0:NE], eio, ef8[:, T, k:k + 1], pstart, ALU.is_equal, ALU.mult, accum=ps8[:, k:k + 1])
                TT("dve", ps8, ps8, pf8[:, T, :], ALU.add)
                CP("dve", slots[:, T, :], ps8)
                for k in range(8):
                    P.add("pool", (lambda e, T=T, k=k, b_=b_: e.indirect_dma_start(
                        out=XS, out_offset=bass.IndirectOffsetOnAxis(ap=slots[:, T, k:k + 1], axis=0),
                        in_=h2r[b_], in_offset=None)),
                        reads=[slots[:, T, k:k + 1], h2r[b_]], dr=["xsz"], dw=["xs"], dma="psc")
            DUMP("slots", slots)

            NWB = 2
            wg32 = [cnew([8, 256], F32) for _ in range(NWB)]
            wu32 = [cnew([8, 256], F32) for _ in range(NWB)]
            wd32 = [cnew([2, D], F32) for _ in range(NWB)]
            wg = [cnew([8, 256]) for _ in range(NWB)]
            wu = [cnew([8, 256]) for _ in range(NWB)]
            wd = [cnew([2, D]) for _ in range(NWB)]
            xr = [cnew([D]) for _ in range(2)]
            XT = [cnew([8, 128]) for _ in range(2)]
            HT = [cnew([2, 128]) for _ in range(2)]
            sgt = [cnew([256], F32) for _ in range(2)]
            Yb = [cnew([D]) for _ in range(2)]
            wg_v = w_eg_h.ap().rearrange("e r n -> (e r) n")
            wu_v = w_eu_h.ap().rearrange("e r n -> (e r) n")
            wd_v = w_ed_h.ap().rearrange("e r n -> (e r) n")

            def gather_w(b, dst, srcv, idx):
                P.add("pool", (lambda e: e.indirect_dma_start(
                    out=dst, out_offset=None, in_=srcv,
                    in_offset=bass.IndirectOffsetOnAxis(ap=idx[:, b:b + 1], axis=0))),
                    reads=[idx[:, b:b + 1]], writes=[dst], dma="pwg")

            for b in range(NB if upto == "all" else (4 if upto == "C1" else 0)):
                wb_ = b % NWB
                b_ = b % 2
                for kc in range(8):
                    gather_w(b, wg32[wb_][:, kc, :], wg_v, idxg[kc])
                    gather_w(b, wu32[wb_][:, kc, :], wu_v, idxg[kc])
                for j2 in range(2):
                    gather_w(b, wd32[wb_][:, j2, :], wd_v, idxd[j2])
                CP("act", wg[wb_], wg32[wb_])
                CP("dve", wu[wb_], wu32[wb_])
                CP("act", wd[wb_][:, 0, :], wd32[wb_][:, 0, :])
                CP("dve", wd[wb_][:, 1, :], wd32[wb_][:, 1, :])
                LD("sp_w", xr[b_], XS[b * 128:(b + 1) * 128, :], dr=["xs"])
                pT = psT3 if b % 2 == 0 else psT3b
                for c in range(8):
                    TR(pT[:, c, :], xr[b_][:, c * 128:(c + 1) * 128], identb)
                CP("act" if b % 2 == 0 else "dve", XT[b_], pT)
                pgu = PS[b % 2]
                for j in range(4):
                    wsrc = wg[wb_] if j < 2 else wu[wb_]
                    for kc in range(8):
                        MM(pgu[:, j * 128:(j + 1) * 128], wsrc[:, kc, (j % 2) * 128:(j % 2 + 1) * 128], XT[b_][:, kc, :], kc == 0, kc == 7)
                ACT(sgt[b_], pgu[:, 0:256], AF.Silu)
                TT("dve", HT[b_].rearrange("p a b -> p (a b)"), sgt[b_], pgu[:, 256:512], ALU.mult)
                for n in range(2):
                    py = PS[2 + 2 * (b % 2) + n]
                    for j2 in range(2):
                        MM(py[:, :], HT[b_][:, j2, :], wd[wb_][:, j2, n * 512:(n + 1) * 512], j2 == 0, j2 == 1)
                    CP("act" if n == 0 else "dve", Yb[b_][:, n * 512:(n + 1) * 512], py[:, :])
                ST("sp_st", XS[b * 128:(b + 1) * 128, :], Yb[b_], dw=["ys"])
            oc[0] = o_after_slots
            ptl = [cnew([D], F32) for _ in range(2)]
            Yg = [cnew([8, D]) for _ in range(2)]
            acc = [cnew([D], F32) for _ in range(2)]
            g2all = cnew([nseq, D], F32)
            fgbc = cnew([D], F32)
            for s in range(nseq):
                LD("sp_c", g2all[:, s, :], modd[s:s + 1, 5 * D:6 * D].partition_broadcast(128), dr=["modd"])
            LD("sp_c", fgbc, fg_d.partition_broadcast(128))
            for T in range(NTT if upto == "all" else 0):
                s = T // NT
                b_ = T % 2
                st_ = stat(T)
                LD("sp_x", ptl[b_], PART[T * 128:(T + 1) * 128, :], dr=["part"])
                for k in range(8):
                    P.add("pool", (lambda e, T=T, k=k, b_=b_: e.indirect_dma_start(
                        out=Yg[b_][:, k, :], out_offset=None, in_=XS,
                        in_offset=bass.IndirectOffsetOnAxis(ap=slots[:, T, k:k + 1], axis=0))),
                        reads=[slots[:, T, k:k + 1]], writes=[Yg[b_][:, k, :]], dr=["ys"], dma="pg")
                a_ = acc[b_]
                TS("dve", a_, Yg[b_][:, 0, :], gts[:, T, 0:1], None, ALU.mult)
                for k in range(1, 8):
                    STT(a_, Yg[b_][:, k, :], gts[:, T, k:k + 1], a_, ALU.mult, ALU.add)
                TT("dve", a_, a_, g2all[:, s, :], ALU.mult)
                TT("pool", a_, a_, ptl[b_], ALU.add)
                STT(junk, a_, 1.0, a_, ALU.mult, ALU.mult, accum=st_[:, 0:1])
                ACT(st_[:, 1:2], st_[:, 0:1], AF.Ln, bias=epsc[:, 0:1], scale=1.0 / D)
                ACT(st_[:, 2:3], st_[:, 1:2], AF.Exp, scale=-0.5)
                STT(ptl[b_], a_, st_[:, 2:3], fgbc, ALU.mult, ALU.mult)
                ST("sp_st", out_d[T * 128:(T + 1) * 128, :], ptl[b_], dw=["out"])
        P.add("sp", None, dr=["out", "dbg", "part", "xs", "ys", "modd", "h2s", "xsz"])
        P.emit()
    nc._prog = P
    nc._dbg = list(dbg_outs.keys())
    return nc


def _prep_inputs(inputs, ncores, nseq):
    f = lambda a: np.ascontiguousarray(np.asarray(a, dtype=np.float32))
    cst = _host_consts()
    shared = {
        "w_ada": f(inputs["w_ada"]), "b_ada": f(inputs["b_ada"]).reshape(1, -1),
        "n1g": f(f(inputs["norm1_g"]).reshape(8, 128).T),
        "norm2_g": f(inputs["norm2_g"]).reshape(1, -1), "final_g": f(inputs["final_g"]).reshape(1, -1),
        "w_in": f(inputs["w_in"]),
        "bgc": f(f(inputs["b_gate"]).reshape(16, 128).T),
        "qng2": f(np.tile(f(inputs["qn_g"]), 2).reshape(128, 1)),
        "kng2": f(np.tile(f(inputs["kn_g"]), 2).reshape(128, 1)),
        "w_o_dil": f(inputs["w_o_dil"]), "w_o_gqa": f(inputs["w_o_gqa"]), "w_out": f(inputs["w_out"]),
        "router_w": f(inputs["router_w"]), "router_bias": f(inputs["router_bias"]).reshape(1, -1),
        "bidx": f((np.arange(((nseq * S * 8) // 128 + NE) // 128)[None, :] * 128 + np.arange(128)[:, None]).astype(np.float32)),
        "w_exp_gate": f(inputs["w_exp_gate"]), "w_exp_up": f(inputs["w_exp_up"]), "w_exp_down": f(inputs["w_exp_down"]),
        "w_sh_gate": f(inputs["w_sh_gate"]), "w_sh_up": f(inputs["w_sh_up"]), "w_sh_down": f(inputs["w_sh_down"]),
    }
    shared.update(cst)
    x = f(inputs["x"])
    c = f(inputs["c"])
    maps = []
    for i in range(ncores):
        m = dict(shared)
        m["x"] = x[i * nseq:(i + 1) * nseq].reshape(nseq * S, D)
        ci = c[i * nseq:(i + 1) * nseq]
        m["cT"] = f(ci.reshape(nseq, 8, 128).transpose(2, 1, 0))
        maps.append(m)
    return maps


def kernel(**inputs):
    ncores, nseq = 8, 4
    nc = build(nseq=nseq)
    maps = _prep_inputs(inputs, ncores, nseq)
    res = run_bass_kernel_spmd(nc, maps, core_ids=list(range(ncores)))
    out = np.concatenate([r["out"] for r in res.results], axis=0)
    return out.reshape(ncores * nseq, S, D).astype(np.float32)
```

```python
import contextlib
import numpy as np
import ml_dtypes
import concourse.bass as bass
import concourse.mybir as mybir
from concourse.bass_utils import run_bass_kernel_spmd

F32 = mybir.dt.float32
BF16 = mybir.dt.bfloat16
I32 = mybir.dt.int32
AF = mybir.ActivationFunctionType
ALU = mybir.AluOpType

_DSIZE = {F32: 4, BF16: 2, I32: 4}
GRAN = 256
SEM_LIM = 30000

D = 1024
S = 2048
NT = S // 128
NE = 256
ENC = 8192
MAXB = 64
EPS = 1e-6


class Op:
    __slots__ = ("eng", "fn", "waits", "venue", "idx", "clock", "inc")


def _region(ap):
    dsz = _DSIZE[ap.dtype]
    pairs = list(ap.ap)
    pstep = pairs[0][0]
    off = ap.offset
    if pstep > 0:
        off = off % pstep
    lo = off
    hi = off
    for (st, cnt) in pairs[1:]:
        if st >= 0:
            hi += st * (cnt - 1)
        else:
            lo += st * (cnt - 1)
    return (ap.tensor.name, lo * dsz, (hi + 1) * dsz)


class Prog:
    ENGS = ["pe", "act", "dve", "pool", "sp"]

    def __init__(self, nc):
        self.nc = nc
        self.ops = {e: [] for e in self.ENGS}
        self.know = {e: {} for e in self.ENGS}
        self.last_w = {}
        self.readers = {}
        self.dram_w = {}
        self.dram_r = {}
        self.venue_cnt = {}
        self.venue_last = {}
        self.streams = {}

    def _grans(self, ap):
        name, lo, hi = _region(ap)
        if name.startswith("ps"):
            return [(name, 0)]
        return [(name, g) for g in range(lo // GRAN, (hi - 1) // GRAN + 1)]

    def _dep(self, eng, d, waits):
        if d is None:
            return
        k = self.know[eng]
        if k.get(d.venue, 0) >= d.idx:
            return
        if d.venue == eng and eng == "pe":
            return
        waits.append(d)
        d.inc = True
        for v, n in d.clock.items():
            if k.get(v, 0) < n:
                k[v] = n

    def add(self, eng, fn, reads=(), writes=(), dr=(), dw=(), dma=None):
        op = Op()
        op.eng = eng
        op.fn = fn
        op.inc = False
        waits = []
        rg = []
        for ap in reads:
            rg += self._grans(ap)
        wg = []
        for ap in writes:
            wg += self._grans(ap)
        for g in rg:
            self._dep(eng, self.last_w.get(g), waits)
            if g[0].startswith("ps"):
                for r in self.readers.get(g, ()):
                    if r.eng != eng:
                        self._dep(eng, r, waits)
        for g in wg:
            self._dep(eng, self.last_w.get(g), waits)
            for r in self.readers.get(g, ()):
                self._dep(eng, r, waits)
        for key in dr:
            for w in self.dram_w.get(key, {}).values():
                self._dep(eng, w, waits)
        for key in dw:
            for r in self.dram_r.get(key, {}).values():
                self._dep(eng, r, waits)
        if dma is not None:
            st = self.streams[dma]
            assert st["eng"] == eng
            k = st["n"] % st["nsem"]
            st["n"] += 1
            venue = "dma:%s:%d" % (dma, k)
            self._dep(eng, self.venue_last.get(venue), waits)
            op.inc = True
        else:
            venue = eng
        op.venue = venue
        op.idx = self.venue_cnt.get(venue, 0) + 1
        self.venue_cnt[venue] = op.idx
        self.venue_last[venue] = op
        op.waits = waits
        ck = dict(self.know[eng])
        ck[venue] = op.idx
        op.clock = ck
        for g in rg:
            self.readers.setdefault(g, []).append(op)
        for g in wg:
            self.last_w[g] = op
            self.readers[g] = []
        for key in dr:
            self.dram_r.setdefault(key, {})[venue] = op
        for key in dw:
            self.dram_w.setdefault(key, {})[venue] = op
        self.ops[eng].append(op)
        return op

    def stream(self, name, eng, nsem):
        self.streams[name] = {"eng": eng, "nsem": nsem, "n": 0}

    def emit(self):
        nc = self.nc
        semval = {}
        counts = {}
        for eng in self.ENGS:
            for op in self.ops[eng]:
                if op.inc:
                    c = counts.get(op.venue, 0) + 1
                    counts[op.venue] = c
                    semval[id(op)] = c
        step = {}
        nsems = {}
        for v, c in counts.items():
            s = 16 if v.startswith("dma:") else 1
            step[v] = s
            lim = SEM_LIM // s
            nsems[v] = (c - 1) // lim + 1
        self.total_sems = sum(nsems.values())
        with contextlib.ExitStack() as es:
            sems = {}
            for v, n in nsems.items():
                sems[v] = [es.enter_context(nc.semaphore(("s_%s_%d" % (v, i)).replace(":", "_")))
                           for i in range(n)]

            def semof(op):
                c = semval[id(op)]
                s = step[op.venue]
                lim = SEM_LIM // s
                return sems[op.venue][(c - 1) // lim], ((c - 1) % lim + 1) * s

            def run(eng, e):
                for op in self.ops[eng]:
                    for d in op.waits:
                        sm, val = semof(d)
                        e.wait_ge(sm, val)
                    if op.fn is None:
                        continue
                    ins = op.fn(e)
                    if op.inc:
                        sm, val = semof(op)
                        ins.then_inc(sm, step[op.venue])

            with nc.Block() as block:
                @block.tensor
                def _(e):
                    run("pe", e)

                @block.scalar
                def _(e):
                    run("act", e)

                @block.vector
                def _(e):
                    run("dve", e)

                @block.gpsimd
                def _(e):
                    run("pool", e)

                @block.sync
                def _(e):
                    run("sp", e)


DIL = (1, 4, 16)
REACH = (64, 256, 1024)
MC0 = tuple(r + 511 for r in REACH)
MW = tuple(2 * r + 1150 for r in REACH)
MOFF = (0, MW[0], MW[0] + MW[1])
MTOT = sum(MW)


def _host_consts():
    bf = ml_dtypes.bfloat16
    c = {}
    c["identb"] = np.eye(128, dtype=np.float32).astype(bf)
    c["identf"] = np.eye(128, dtype=np.float32)
    r1 = np.zeros((128, 128), np.float32)
    ra = np.zeros((128, 128), np.float32)
    for m in range(128):
        dm = m % 64
        base = m - dm
        if dm < 32:
            r1[base + dm + 32, m] = -1.0
        else:
            r1[base + dm - 32, m] = 1.0
        sub = dm % 32
        blk = dm - sub
        if sub < 16:
            ra[base + blk + sub + 16, m] = -1.0
        else:
            ra[base + blk + sub - 16, m] = 1.0
    c["rm1"] = r1.astype(bf)
    c["rma"] = ra.astype(bf)
    bo = np.zeros((128, 128), np.float32)
    bo[:64, :64] = 1.0
    bo[64:, 64:] = 1.0
    c["bones"] = bo.astype(bf)
    c["utri"] = np.triu(np.ones((128, 128), np.float32), 1).astype(bf)
    c["ones"] = np.ones((128, 128), np.float32).astype(bf)
    t = np.arange(S, dtype=np.float32)
    inv1 = (np.float32(10000.0) ** (-np.arange(0, 64, 2, dtype=np.float32) / np.float32(64))).astype(np.float32)
    inva = (np.float32(10000.0) ** (-np.arange(0, 32, 2, dtype=np.float32) / np.float32(32))).astype(np.float32)
    row = (np.arange(S) // 64).astype(np.float32)
    col = (np.arange(S) % 64).astype(np.float32)
    cos1 = np.zeros((128, S), np.float32)
    sin1 = np.zeros((128, S), np.float32)
    cosa = np.zeros((128, S), np.float32)
    sina = np.zeros((128, S), np.float32)
    for p in range(128):
        dm = p % 64
        ang = (t * inv1[dm % 32]).astype(np.float32)
        cos1[p] = np.cos(ang)
        sin1[p] = np.sin(ang)
        if dm < 32:
            ang = (row * inva[dm % 16]).astype(np.float32)
        else:
            ang = (col * inva[(dm - 32) % 16]).astype(np.float32)
        cosa[p] = np.cos(ang)
        sina[p] = np.sin(ang)
    c["rope"] = np.stack([cos1, sin1, cosa, sina], axis=1).astype(bf)
    strips = []
    for g in range(3):
        i = np.arange(128)[:, None]
        cc = np.arange(MW[g])[None, :]
        dlt = i - cc + MC0[g]
        ok = (np.abs(dlt) <= REACH[g]) & (dlt % DIL[g] == 0)
        strips.append(ok.astype(np.float32))
    c["mstrip"] = np.concatenate(strips, axis=1).astype(bf)
    c["eidx"] = np.tile((np.arange(NE, dtype=np.float32) * ENC + 1.0)[None, :], (128, 1)).astype(np.float32)
    c["eio"] = np.tile(np.arange(NE, dtype=np.float32)[None, :], (128, 1)).astype(np.float32)
    return c


def build(nseq=4, upto="all", dbg=()):
    nc = bass.Bass("TRN2", target_bir_lowering=False)
    NTOK = nseq * S
    NTT = nseq * NT

    def din(name, shape, dt=F32):
        return nc.dram_tensor(name, list(shape), dt, kind="ExternalInput").ap()

    x_d = din("x", [NTOK, D])
    cT_d = din("cT", [128, 8, nseq])
    w_ada = din("w_ada", [D, 6 * D])
    b_ada = din("b_ada", [1, 6 * D])
    n1g_d = din("n1g", [128, 8])
    n2g_d = din("norm2_g", [1, D])
    fg_d = din("final_g", [1, D])
    w_in = din("w_in", [D, 5888])
    bgc_d = din("bgc", [128, 16])
    qng_d = din("qng2", [128, 1])
    kng_d = din("kng2", [128, 1])
    w_o_dil = din("w_o_dil", [256, D])
    w_o_gqa = din("w_o_gqa", [D, D])
    w_out = din("w_out", [D, D])
    router_w = din("router_w", [D, NE])
    rbias_d = din("router_bias", [1, NE])
    w_eg_h = nc.dram_tensor("w_exp_gate", [NE, D, 256], F32, kind="ExternalInput")
    w_eu_h = nc.dram_tensor("w_exp_up", [NE, D, 256], F32, kind="ExternalInput")
    w_ed_h = nc.dram_tensor("w_exp_down", [NE, 256, D], F32, kind="ExternalInput")
    w_sg = din("w_sh_gate", [D, 256])
    w_su = din("w_sh_up", [D, 256])
    w_sd = din("w_sh_down", [256, D])
    identb_d = din("identb", [128, 128], BF16)
    identf_d = din("identf", [128, 128])
    rm1_d = din("rm1", [128, 128], BF16)
    rma_d = din("rma", [128, 128], BF16)
    bones_d = din("bones", [128, 128], BF16)
    utri_d = din("utri", [128, 128], BF16)
    ones_d = din("ones", [128, 128], BF16)
    rope_d = din("rope", [128, 4, S], BF16)
    mstrip_d = din("mstrip", [128, MTOT], BF16)
    eidx_d = din("eidx", [128, NE])
    out_d = nc.dram_tensor("out", [NTOK, D], F32, kind="ExternalOutput").ap()
    modd = nc.dram_tensor("modd", [nseq, 6 * D], F32, kind="Internal").ap()
    NB = NTOK * 8 // 128 + NE
    NBJ = NB // 128
    XS = nc.dram_tensor("xs", [NB * 128, D], BF16, kind="Internal").ap()
    H2S = nc.dram_tensor("h2s", [NTOK, D], BF16, kind="Internal").ap()
    eio_d = din("eio", [128, NE])
    bidx_d = din("bidx", [128, NBJ])
    PART = nc.dram_tensor("part", [NTOK, D], F32, kind="Internal").ap()

    P = Prog(nc)
    P.stream("sp_c", "sp", 4)
    P.stream("sp_x", "sp", 2)
    P.stream("sp_w", "sp", 4)
    P.stream("sp_st", "sp", 4)
    P.stream("sp_z", "sp", 4)
    P.stream("pw", "pool", 6)
    P.stream("pwg", "pool", 12)
    P.stream("psc", "pool", 8)
    P.stream("pg", "pool", 8)
    dbg_outs = {}

    with contextlib.ExitStack() as es:
        ARENA_EL = 103 * 1024 + 512
        A = es.enter_context(nc.sbuf_tensor("arena", [128, ARENA_EL], BF16))
        PS = [es.enter_context(nc.psum_tensor("ps%d" % i, [128, 512], F32)) for i in range(8)]
        cur = [0]

        def alloc(nbytes):
            o = cur[0]
            cur[0] = (o + nbytes + 63) // 64 * 64
            assert cur[0] <= ARENA_EL * 2, ("SBUF overflow", cur[0])
            return o

        def view(off, shape, dt=BF16):
            n = 1
            for s_ in shape:
                n *= s_
            nb = n * _DSIZE[dt]
            assert off % 4 == 0
            ap = A[:, off // 2: off // 2 + nb // 2]
            if dt != BF16:
                ap = ap.bitcast(dt)
            if len(shape) == 2:
                ap = ap.rearrange("p (a b) -> p a b", a=shape[0])
            elif len(shape) == 3:
                ap = ap.rearrange("p (a b c) -> p a b c", a=shape[0], b=shape[1])
            return ap

        def new(shape, dt=BF16):
            n = 1
            for s_ in shape:
                n *= s_
            return view(alloc(n * _DSIZE[dt]), shape, dt)

        def aps(*xs):
            return [a for a in xs if a is not None and not isinstance(a, (int, float))]

        def MM(out, lhsT, rhs, start, stop):
            P.add("pe", lambda e: e.matmul(out, lhsT=lhsT, rhs=rhs, start=start, stop=stop),
                  reads=[lhsT, rhs], writes=[out])

        def TR(out, in_, ident):
            P.add("pe", lambda e: e.transpose(out=out, in_=in_, identity=ident), reads=[in_, ident], writes=[out])

        def ACT(out, in_, func, bias=None, scale=None, accum=None):
            kw = {}
            if bias is not None:
                kw["bias"] = bias
            if scale is not None:
                kw["scale"] = scale
            if accum is not None:
                kw["accum_out"] = accum
            P.add("act", lambda e: e.activation(out=out, in_=in_, func=func, **kw),
                  reads=aps(in_, bias, scale), writes=aps(out, accum))

        def TT(eng, out, in0, in1, op):
            P.add(eng, lambda e: e.tensor_tensor(out=out, in0=in0, in1=in1, op=op), reads=[in0, in1], writes=[out])

        def TS(eng, out, in0, s1, s2, op0, op1=None, accum=None):
            if accum is not None:
                P.add(eng, lambda e: e.tensor_scalar(out=out, in0=in0, scalar1=s1, scalar2=s2, op0=op0, op1=op1, accum_out=accum),
                      reads=aps(in0, s1, s2), writes=[out, accum])
            elif op1 is None:
                P.add(eng, lambda e: e.tensor_scalar(out=out, in0=in0, scalar1=s1, scalar2=None, op0=op0),
                      reads=aps(in0, s1), writes=[out])
            else:
                P.add(eng, lambda e: e.tensor_scalar(out=out, in0=in0, scalar1=s1, scalar2=s2, op0=op0, op1=op1),
                      reads=aps(in0, s1, s2), writes=[out])

        def STT(out, in0, scalar, in1, op0, op1, accum=None):
            kw = {}
            if accum is not None:
                kw["accum_out"] = accum
            P.add("dve", lambda e: e.scalar_tensor_tensor(out=out, in0=in0, scalar=scalar, in1=in1, op0=op0, op1=op1, **kw),
                  reads=aps(in0, scalar, in1), writes=aps(out, accum))

        def CP(eng, out, in_):
            if eng == "act":
                P.add("act", lambda e: e.copy(out=out, in_=in_), reads=[in_], writes=[out])
            else:
                P.add(eng, lambda e: e.tensor_copy(out=out, in_=in_), reads=[in_], writes=[out])

        def MAX8(out, in_):
            P.add("dve", lambda e: e.max(out=out, in_=in_), reads=[in_], writes=[out])

        def RECIP(out, in_):
            P.add("dve", lambda e: e.reciprocal(out=out, in_=in_), reads=[in_], writes=[out])

        def MEMSET(eng, ap, val):
            P.add(eng, lambda e: e.memset(ap, val), writes=[ap])

        def LD(stream, out, in_, dr=(), **kw):
            eng = P.streams[stream]["eng"]
            P.add(eng, lambda e: e.dma_start(out=out, in_=in_, **kw), writes=[out], dr=dr, dma=stream)

        def ST(stream, out, in_, dw=(), **kw):
            eng = P.streams[stream]["eng"]
            P.add(eng, lambda e: e.dma_start(out=out, in_=in_, **kw), reads=[in_], dw=dw, dma=stream)

        def DUMP(name, ap, dt=None):
            if name not in dbg:
                return
            shp = list(ap.shape)
            d_ = nc.dram_tensor("dbg_" + name, shp, ap.dtype, kind="ExternalOutput").ap()
            dbg_outs[name] = d_
            ST("sp_st", d_, ap, dw=["dbg"])

        def rstd_from_ssq(out, ssq, n, tmp):
            ACT(tmp, ssq, AF.Ln, bias=epsc[:, 0:1], scale=1.0 / n)
            ACT(out, tmp, AF.Exp, scale=-0.5)

        identb = new([128]); rm1 = new([128]); rma = new([128]); bones = new([128])
        utri = new([128]); ones = new([128])
        identf = new([128], F32); onesf = new([128], F32)
        rope = new([4, S])
        mstrip = new([MTOT])
        n1g = new([8], F32); bgc = new([16], F32); qng = new([1], F32); kng = new([1], F32)
        epsc = new([1], F32)
        eidx = new([NE], F32); rbias = new([NE], F32); eio = new([NE], F32)
        gts = new([NTT, 8], F32); cnt = new([NE], F32)
        ef8 = new([NTT, 8], F32); pf8 = new([NTT, 8], F32)
        gs1c = new([8], F32); sh1c = new([8], F32); sc1c = new([8], F32)
        small = new([64], F32)

        o_hT = alloc(8 * S * 2)
        o_QK = alloc(12 * S * 2)
        o_U1 = alloc(16384 * 2)
        o_vb = alloc(NT * 4 * 65 * 2)
        o_oA = alloc(2 * S * 2)
        hT = view(o_hT, [8, S])
        QK = view(o_QK, [12, S])
        va = view(o_U1, [NT, 12, 65])
        oB = view(o_U1, [8, S])
        vb = view(o_vb, [NT, 4, 65])
        oA = view(o_oA, [2, S])

        xt = new([D], F32)
        xn = new([D])
        wch = [new([8, 256]) for _ in range(4)]
        PT = [new([512]) for _ in range(2)]
        tb1 = [new([512]) for _ in range(2)]
        tsq = new([512])
        o_tf = cur[0]
        tf = [new([512], F32) for _ in range(3)]
        junk = view(o_tf, [D], F32)
        zt = new([D])
        SB_END = cur[0]

        psT3 = PS[7][:, :].bitcast(BF16).rearrange("p (a b) -> p a b", a=8)
        psT3b = PS[6][:, :].bitcast(BF16).rearrange("p (a b) -> p a b", a=8)

        def stat(i):
            return small[:, (i % 4) * 16:(i % 4) * 16 + 16]

        LD("sp_c", identb, identb_d); LD("sp_c", rm1, rm1_d); LD("sp_c", rma, rma_d)
        LD("sp_c", bones, bones_d); LD("sp_c", utri, utri_d); LD("sp_c", ones, ones_d)
        LD("sp_c", identf, identf_d); LD("sp_c", rope, rope_d); LD("sp_c", mstrip, mstrip_d)
        LD("sp_c", n1g, n1g_d); LD("sp_c", bgc, bgc_d); LD("sp_c", qng, qng_d); LD("sp_c", kng, kng_d)
        LD("sp_c", eidx, eidx_d)
        LD("sp_c", eio, eio_d)
        LD("sp_c", rbias, rbias_d.partition_broadcast(128))
        MEMSET("dve", epsc, EPS)
        MEMSET("dve", cnt, 0.0)
        MEMSET("dve", onesf, 1.0)
        MEMSET("pool", zt, 0.0)
        zfill = [0]
        NZ = NB
        MEMSET("pool", vb[:, :, :, 64:65], 1.0)

        sct = view(o_QK, [8, nseq], F32)
        LD("sp_c", sct, cT_d)
        ACT(sct, sct, AF.Silu)
        wst = [view(o_hT, [8, 512], F32), view(o_hT + 16384, [8, 512], F32)]
        mrow = [view(o_U1, [512], F32), view(o_U1 + 2048, [512], F32)]
        brow = view(o_U1 + 4096, [6 * D], F32)
        LD("sp_c", brow[0:nseq, :], b_ada.partition_broadcast(nseq))
        for blk in range(12):
            wb_ = wst[blk % 2]
            LD("sp_w", wb_, w_ada[:, blk * 512:(blk + 1) * 512].rearrange("(kc p) n -> p kc n", p=128))
            pm = PS[blk % 2]
            for kc in range(8):
                MM(pm[0:nseq, :], sct[:, kc, :], wb_[:, kc, :], kc == 0, kc == 7)
            mr = mrow[blk % 2]
            TT("dve", mr[0:nseq, :], pm[0:nseq, :], brow[0:nseq, blk * 512:(blk + 1) * 512], ALU.add)
            ST("sp_st", modd[:, blk * 512:(blk + 1) * 512], mr[0:nseq, :], dw=["modd"])

        def load_wblock(buf, col0, ncols, dst0=0, src=None, kcn=8):
            src = w_in if src is None else src
            LD("pw", buf[:, 0:kcn, dst0:dst0 + ncols],
               src[:, col0:col0 + ncols].rearrange("(kc p) n -> p kc n", p=128))

        pacc_i = [0]

        def proj_fm(wbuf, c0, tb):
            pm = PS[pacc_i[0] % 2]
            pacc_i[0] += 1
            for kc in range(8):
                MM(pm[:, :], wbuf[:, kc, c0:c0 + 128], hT[:, kc, tb * 512:(tb + 1) * 512], kc == 0, kc == 7)
            return pm

        def rope_plain(pm, dst, tb, ti):
            tok = slice(tb * 512, (tb + 1) * 512)
            qg = tb1[ti % 2]
            CP("act", qg, pm[:, :])
            pr = PS[2 + ti % 2]
            MM(pr[:, :], rm1, qg, True, True)
            TT("dve", tf[0], pm[:, :], rope[:, 0, tok], ALU.mult)
            TT("dve", tf[1], pr[:, :], rope[:, 1, tok], ALU.mult)
            TT("dve", dst, tf[0], tf[1], ALU.add)

        def rope_norm(pm, dst, tb, ti, gcol):
            tok = slice(tb * 512, (tb + 1) * 512)
            ACT(tsq, pm[:, :], AF.Square)
            qg = tb1[ti % 2]
            ACT(qg, pm[:, :], AF.Copy, scale=gcol[:, 0:1])
            pq = PS[2]
            pr = PS[3]
            MM(pq[:, :], bones, tsq, True, True)
            MM(pr[:, :], rma, qg, True, True)
            ACT(tf[2], pq[:, :], AF.Ln, bias=epsc[:, 0:1], scale=1.0 / 64.0)
            ACT(tf[2], tf[2], AF.Exp, scale=-0.5)
            TT("dve", tf[0], qg, rope[:, 2, tok], ALU.mult)
            TT("dve", tf[1], pr[:, :], rope[:, 3, tok], ALU.mult)
            TT("dve", tf[0], tf[0], tf[1], ALU.add)
            TT("dve", dst, tf[0], tf[2], ALU.mult)

        def attention(branches, dst_chunk, dst_half):
            for qc in range(4):
                po = PS[4 + (qc % 2)]
                blocks = []
                for (q_fn, k_fn, v_fn, g) in branches:
                    for kb in range(NT):
                        if g is not None:
                            delta = kb * 128 - qc * 512
                            if delta + 127 < -REACH[g] or delta - 511 > REACH[g]:
                                continue
                        blocks.append((q_fn, k_fn, v_fn, g, kb))
                nb = len(blocks)
                for i, (q_fn, k_fn, v_fn, g, kb) in enumerate(blocks):
                    psc = PS[i % 3]
                    MM(psc[:, :], k_fn(kb), q_fn(qc), True, True)
                    pt = PT[i % 2]
                    ACT(pt, psc[:, :], AF.Exp, scale=0.125)
                    if g is not None:
                        c0 = MOFF[g] + MC0[g] - (kb * 128 - qc * 512)
                        TT("dve", pt, pt, mstrip[:, c0:c0 + 512], ALU.mult)
                    MM(po[0:65, :], v_fn(kb), pt, i == 0, i == nb - 1)
                rr = tf[2]
                RECIP(rr[64:65, :], po[64:65, :])
                pb_ = PS[6]
                MM(pb_[0:64, :], onesf[64:65, 0:64], rr[64:65, :], True, True)
                CP("act", tf[0][0:64, :], pb_[0:64, :])
                TT("dve", dst_chunk[dst_half * 64:(dst_half + 1) * 64, qc * 512:(qc + 1) * 512],
                   po[0:64, :], tf[0][0:64, :], ALU.mult)

        g1bc = view(o_hT, [D], F32); gs2bc = view(o_hT + 4096, [D], F32); sh2bc = view(o_hT + 8192, [D], F32)
        g2bc_t = view(o_hT + 12288, [D], F32); n2gbc = view(o_hT + 16384, [D], F32)
        x1 = view(o_hT + 20480, [D], F32); h2 = view(o_hT + 24576, [D], F32)
        h2b = view(o_hT + 28672, [D]); h2Tb = view(o_hT + 30720, [8, 128])
        wshgu = view(o_U1, [8, 512]); wshd = view(o_U1 + 8192, [2, 1024])
        routw = view(o_U1 + 12288, [8, NE], F32); h2T = view(o_U1 + 20480, [8, 128], F32)
        r_sc = view(o_U1 + 24576, [NE], F32); r_bi = view(o_U1 + 25600, [NE], F32)
        r_ma = view(o_U1 + 26624, [NE], F32); r_se = view(o_U1 + 27648, [NE], F32)
        r_ga = view(o_U1 + 28672, [NE], F32); r_D = view(o_U1 + 29696, [NE], F32)
        r_selb = view(o_U1 + 30720, [NE]); r_m8 = view(o_U1 + 31232, [8, 8], F32)
        r_gs = view(o_U1 + 31488, [8], F32); r_gs8 = view(o_U1 + 31520, [8], F32); r_gm = view(o_U1 + 31552, [8], F32)
        r_pen = view(o_U1 + 31584, [8], F32); r_t8 = view(o_U1 + 31616, [8], F32); r_s8 = view(o_U1 + 31648, [8], F32)
        r_HT = view(o_U1 + 31744, [2, 128])
        wout = view(o_QK + 8 * S * 2, [8, D])
        sgs = tf[0]

        def do_seq(s):
            tok0 = s * S
            LD("sp_c", sh1c, modd[s:s + 1, 0:D].rearrange("o (j p) -> p (o j)", p=128), dr=["modd"],
               allow_slow_non_contiguous=True)
            LD("sp_c", sc1c, modd[s:s + 1, D:2 * D].rearrange("o (j p) -> p (o j)", p=128), dr=["modd"],
               allow_slow_non_contiguous=True)
            STT(gs1c, sc1c, 1.0, n1g, ALU.add, ALU.mult)
            MEMSET("pool", va[:, :, :, 64:65], 1.0)
            for t in range(NT):
                st_ = stat(t)
                LD("sp_x", xt, x_d[tok0 + t * 128: tok0 + (t + 1) * 128, :])
                for _z in range(-(-NZ // NTT)):
                    if zfill[0] < NZ:
                        z0 = zfill[0] * 128
                        ST("sp_z", XS[z0:z0 + 128, :], zt, dw=["xsz"])
                        zfill[0] += 1
                STT(junk, xt, 1.0, xt, ALU.mult, ALU.mult, accum=st_[:, 0:1])
                ACT(st_[:, 1:2], st_[:, 0:1], AF.Ln, bias=epsc[:, 0:1], scale=1.0 / D)
                ACT(st_[:, 2:3], st_[:, 1:2], AF.Exp, scale=-0.5)
                ACT(xn, xt, AF.Copy, scale=st_[:, 2:3])
                for c in range(8):
                    TR(psT3[:, c, :], xn[:, c * 128:(c + 1) * 128], identb)
                for c in range(8):
                    TS("dve", hT[:, c, t * 128:(t + 1) * 128], psT3[:, c, :],
                       gs1c[:, c:c + 1], sh1c[:, c:c + 1], ALU.mult, ALU.add)
            DUMP("hT", hT)
            if upto == "A1":
                return
            ti = 0
            for bi in range(6):
                wb_ = wch[bi % 4]
                load_wblock(wb_, bi * 256, 256)
                for c2 in range(2):
                    ch = bi * 2 + c2
                    for tb in range(4):
                        pm = proj_fm(wb_, c2 * 128, tb)
                        rope_plain(pm, QK[:, ch, tb * 512:(tb + 1) * 512], tb, ti)
                        ti += 1
            DUMP("qaka", QK)
            if upto == "A2q":
                return
            for vbi in range(4):
                wb_ = wch[(2 + vbi) % 4]
                load_wblock(wb_, (1536 + vbi * 256) if vbi < 3 else 3584, 256)
                for t in range(NT):
                    pm = PS[pacc_i[0] % 2]
                    pacc_i[0] += 1
                    for kc in range(8):
                        MM(pm[:, 0:256], hT[:, kc, t * 128:(t + 1) * 128], wb_[:, kc, :], kc == 0, kc == 7)
                    src = pm[:, 0:256].rearrange("p (h d) -> p h d", h=4)
                    if vbi < 3:
                        CP("act" if t % 2 == 0 else "dve", va[:, t, vbi * 4:(vbi + 1) * 4, 0:64], src)
                    else:
                        CP("act" if t % 2 == 0 else "dve", vb[:, t, :, 0:64], src)
            DUMP("va", va); DUMP("vb", vb)
            if upto == "A2":
                return
            for h in range(4):
                br = []
                for g in range(3):
                    hh = g * 4 + h
                    chk, hf = hh // 2, hh % 2
                    br.append((
                        (lambda qc, chk=chk, hf=hf: QK[hf * 64:(hf + 1) * 64, chk, qc * 512:(qc + 1) * 512]),
                        (lambda kb, chk=chk, hf=hf: QK[hf * 64:(hf + 1) * 64, 6 + chk, kb * 128:(kb + 1) * 128]),
                        (lambda kb, hh=hh: va[:, kb, hh, :]),
                        g))
                attention(br, oA[:, h // 2, :], h % 2)
            DUMP("oA", oA)
            if upto == "dil":
                return
            ti = 0
            for bi in range(4):
                wb_ = wch[bi % 4]
                load_wblock(wb_, 2304 + bi * 256, 256)
                for c2 in range(2):
                    ch = bi * 2 + c2
                    for tb in range(4):
                        pm = proj_fm(wb_, c2 * 128, tb)
                        rope_norm(pm, QK[:, ch, tb * 512:(tb + 1) * 512], tb, ti, qng)
                        ti += 1
            for kv in range(4):
                wb_ = wch[kv % 4]
                load_wblock(wb_, 3328 + kv * 64, 64, dst0=0)
                load_wblock(wb_, 3328 + kv * 64, 64, dst0=64)
                for tb in range(4):
                    pm = proj_fm(wb_, 0, tb)
                    rope_norm(pm, QK[:, 8 + kv, tb * 512:(tb + 1) * 512], tb, ti, kng)
                    ti += 1
            DUMP("qbkb", QK)
            for hq in range(16):
                kv, chk, hf = hq // 4, hq // 2, hq % 2
                br = [(
                    (lambda qc, chk=chk, hf=hf: QK[hf * 64:(hf + 1) * 64, chk, qc * 512:(qc + 1) * 512]),
                    (lambda kb, kv=kv, hf=hf: QK[hf * 64:(hf + 1) * 64, 8 + kv, kb * 128:(kb + 1) * 128]),
                    (lambda kb, kv=kv: vb[:, kb, kv, :]),
                    None)]
                attention(br, oB[:, chk, :], hf)
            DUMP("oB", oB)
            if upto == "gqa":
                return
            for m in range(8):
                wg_ = wch[(m % 2) * 2]
                wo_ = wch[(m % 2) * 2 + 1]
                load_wblock(wg_, 3840 + m * 128, 128, dst0=0)
                load_wblock(wg_, 4864 + m * 128, 128, dst0=128)
                load_wblock(wo_, m * 128, 128, dst0=0, src=w_o_gqa)
                load_wblock(wo_, m * 128, 128, dst0=128, src=w_o_dil, kcn=2)
                for tb in range(4):
                    tok = slice(tb * 512, (tb + 1) * 512)
                    pa, pg, pb_, pg2 = PS[0], PS[1], PS[2], PS[3]
                    for c in range(2):
                        MM(pa[:, :], wo_[:, c, 128:256], oA[:, c, tok], c == 0, c == 1)
                    for kc in range(8):
                        MM(pg[:, :], wg_[:, kc, 0:128], hT[:, kc, tok], kc == 0, kc == 7)
                    ACT(tb1[0], pg[:, :], AF.Sigmoid, bias=bgc[:, m:m + 1])
                    TT("dve", tf[0], tb1[0], pa[:, :], ALU.mult)
                    for kc in range(8):
                        MM(pb_[:, :], wo_[:, kc, 0:128], oB[:, kc, tok], kc == 0, kc == 7)
                    for kc in range(8):
                        MM(pg2[:, :], wg_[:, kc, 128:256], hT[:, kc, tok], kc == 0, kc == 7)
                    ACT(tb1[1], pg2[:, :], AF.Sigmoid, bias=bgc[:, 8 + m:9 + m])
                    TT("dve", tf[1], tb1[1], pb_[:, :], ALU.mult)
                    TT("dve", QK[:, m, tok], tf[0], tf[1], ALU.add)
            DUMP("zT", QK)
            if upto == "A4":
                return
            LD("pw", wout, w_out.rearrange("(kc p) n -> p kc n", p=128))
            LD("pw", wshgu[:, :, 0:256], w_sg.rearrange("(kc p) n -> p kc n", p=128))
            LD("pw", wshgu[:, :, 256:512], w_su.rearrange("(kc p) n -> p kc n", p=128))
            LD("pw", wshd, w_sd.rearrange("(j p) n -> p j n", p=128))
            LD("sp_w", routw, router_w.rearrange("(kc p) n -> p kc n", p=128))
            LD("sp_c", g1bc, modd[s:s + 1, 2 * D:3 * D].partition_broadcast(128), dr=["modd"])
            LD("sp_c", sh2bc, modd[s:s + 1, 3 * D:4 * D].partition_broadcast(128), dr=["modd"])
            LD("sp_c", gs2bc, modd[s:s + 1, 4 * D:5 * D].partition_broadcast(128), dr=["modd"])
            LD("sp_c", g2bc_t, modd[s:s + 1, 5 * D:6 * D].partition_broadcast(128), dr=["modd"])
            LD("sp_c", n2gbc, n2g_d.partition_broadcast(128))
            STT(gs2bc, gs2bc, 1.0, n2gbc, ALU.add, ALU.mult)
            for t in range(NT):
                T = s * NT + t
                st_ = stat(t)
                for n in range(2):
                    for kc in range(8):
                        MM(PS[n][:, :], QK[:, kc, t * 128:(t + 1) * 128], wout[:, kc, n * 512:(n + 1) * 512], kc == 0, kc == 7)
                LD("sp_x", xt, x_d[tok0 + t * 128: tok0 + (t + 1) * 128, :])
                for n in range(2):
                    TT("dve", x1[:, n * 512:(n + 1) * 512], PS[n][:, :], g1bc[:, n * 512:(n + 1) * 512], ALU.mult)
                TT("pool", x1, x1, xt, ALU.add)
                STT(junk, x1, 1.0, x1, ALU.mult, ALU.mult, accum=st_[:, 0:1])
                ACT(st_[:, 1:2], st_[:, 0:1], AF.Ln, bias=epsc[:, 0:1], scale=1.0 / D)
                ACT(st_[:, 2:3], st_[:, 1:2], AF.Exp, scale=-0.5)
                STT(h2, x1, st_[:, 2:3], gs2bc, ALU.mult, ALU.mult)
                TT("pool", h2, h2, sh2bc, ALU.add)
                CP("act", h2b, h2)
                for c in range(8):
                    pX = PS[2 + c // 4][:, :].rearrange("p (a b) -> p a b", a=4)
                    TR(pX[:, c % 4, :], h2[:, c * 128:(c + 1) * 128], identf)
                for hf in range(2):
                    pX = PS[2 + hf][:, :].rearrange("p (a b) -> p a b", a=4)
                    CP("act", h2T[:, hf * 4:(hf + 1) * 4, :], pX)
                    CP("dve", h2Tb[:, hf * 4:(hf + 1) * 4, :], pX)
                pr = PS[4]
                for kc in range(8):
                    MM(pr[:, 0:NE], h2T[:, kc, :], routw[:, kc, :], kc == 0, kc == 7)
                ACT(r_sc, pr[:, 0:NE], AF.Sigmoid)
                TT("dve", r_bi, r_sc, rbias, ALU.add)
                for g in range(8):
                    MAX8(r_m8[:, g, :], r_bi[:, g * 32:(g + 1) * 32])
                TT("dve", r_gs, r_m8[:, :, 0], r_m8[:, :, 1], ALU.add)
                MAX8(r_gs8, r_gs)
                TS("dve", r_gm, r_gs, r_gs8[:, 3:4], None, ALU.is_ge)
                TS("dve", r_pen, r_gm, -1.0, 10.0, ALU.add, ALU.mult)
                for g in range(8):
                    TS("dve", r_ma[:, g * 32:(g + 1) * 32], r_bi[:, g * 32:(g + 1) * 32],
                       r_gm[:, g:g + 1], r_pen[:, g:g + 1], ALU.mult, ALU.add)
                MAX8(r_t8, r_ma)
                TS("dve", r_se, r_ma, r_t8[:, 7:8], None, ALU.is_ge)
                STT(r_ga, r_sc, 1.0, r_se, ALU.mult, ALU.mult, accum=st_[:, 4:5])
                RECIP(st_[:, 5:6], st_[:, 4:5])
                TS("dve", r_ga, r_ga, st_[:, 5:6], 2.5, ALU.mult, ALU.mult)
                CP("act", r_selb, r_se)
                pc = PS[5]
                MM(pc[:, 0:NE], utri, r_selb, True, True)
                MM(pc[:, NE:2 * NE], ones, r_selb, True, True)
                TT("dve", r_D, pc[:, 0:NE], cnt, ALU.add)
                TT("dve", r_D, r_D, eidx, ALU.add)
                TT("dve", r_D, r_D, r_se, ALU.mult)
                TT("dve", cnt, cnt, pc[:, NE:2 * NE], ALU.add)
                MAX8(r_s8, r_D)
                STT(r_ma, eio, 1.0, r_se, ALU.add, ALU.mult)
                MAX8(r_t8, r_ma)
                TS("dve", ef8[:, T, :], r_t8, -1.0, None, ALU.add)
                STT(pf8[:, T, :], ef8[:, T, :], -float(ENC), r_s8, ALU.mult, ALU.add)
                TS("dve", pf8[:, T, :], pf8[:, T, :], -1.0, None, ALU.add)
                for k in range(8):
                    STT(junk[:, 0:NE], r_D, r_s8[:, k:k + 1], r_ga, ALU.is_equal, ALU.mult, accum=gts[:, T, k:k + 1])
                ST("sp_st", H2S[T * 128:(T + 1) * 128, :], h2b, dw=["h2s"])
                pgu = PS[6]
                for j in range(4):
                    for kc in range(8):
                        MM(pgu[:, j * 128:(j + 1) * 128], wshgu[:, kc, j * 128:(j + 1) * 128], h2Tb[:, kc, :], kc == 0, kc == 7)
                ACT(sgs[:, 0:256], pgu[:, 0:256], AF.Silu)
                TT("dve", r_HT.rearrange("p a b -> p (a b)"), sgs[:, 0:256], pgu[:, 256:512], ALU.mult)
                for n in range(2):
                    for j in range(2):
                        MM(PS[n][:, :], r_HT[:, j, :], wshd[:, j, n * 512:(n + 1) * 512], j == 0, j == 1)
                for n in range(2):
                    TT("dve", h2[:, n * 512:(n + 1) * 512], PS[n][:, :], g2bc_t[:, n * 512:(n + 1) * 512], ALU.mult)
                TT("pool", h2, h2, x1, ALU.add)
                ST("sp_st", PART[T * 128:(T + 1) * 128, :], h2, dw=["part"])
                if t == 0:
                    DUMP("x1", x1); DUMP("rsc", r_sc); DUMP("rse", r_se); DUMP("rga", r_ga); DUMP("rD", r_D)

        for s in range(nseq):
            do_seq(s)
        DUMP("gts", gts)

        if upto in ("all", "C0", "C1"):
            oc = [o_hT]

            def cnew(shape, dt=BF16):
                n = 1
                for s_ in shape:
                    n *= s_
                o = oc[0]
                oc[0] = (o + n * _DSIZE[dt] + 63) // 64 * 64
                assert oc[0] <= SB_END
                return view(o, shape, dt)
            slots = cnew([NTT, 8], I32)
            o_after_slots = oc[0]
            nblk = cnew([NE], F32); pendb = cnew([NE], F32); onesr = cnew([NE], F32); bidx = cnew([NBJ], F32)
            pstart = cnew([NE], F32)
            t_e = cnew([NBJ], F32)
            ps8 = cnew([8], F32)
            h2r = [cnew([D]) for _ in range(2)]
            h2p = [cnew([D]) for _ in range(2)]
            LD("sp_c", bidx, bidx_d)
            MEMSET("dve", onesr, 1.0)
            TS("dve", nblk, cnt, 0.0, None, ALU.is_gt)
            for j in range(1, MAXB):
                STT(nblk, cnt, float(128 * j), nblk, ALU.is_gt, ALU.add)
            P.add("dve", lambda e: e.tensor_tensor_scan(out=pendb, data0=onesr, data1=nblk, initial=0.0,
                                                          op0=ALU.mult, op1=ALU.add),
                  reads=[onesr, nblk], writes=[pendb])
            TT("dve", pstart, pendb, nblk, ALU.subtract)
            TS("dve", pstart, pstart, 128.0, None, ALU.mult)
            for j in range(NBJ):
                TS("dve", junk[:, 0:NE], pendb, bidx[:, j:j + 1], 0.0, ALU.is_le, ALU.add, accum=t_e[:, j:j + 1])
            TS("dve", t_e, t_e, float(NE - 1), None, ALU.min)
            idxw = cnew([NB], I32)
            idxd = [cnew([NB], I32) for _ in range(2)]
            pcol = cnew([2], F32)
            dg = cnew([128], F32)
            for j in range(2):
                TS("dve", pcol[:, j:j + 1], bidx[:, 0:1], float(j * 128), None, ALU.add)
            for j in range(NBJ):
                TS("dve", dg, identf, t_e[:, j:j + 1], None, ALU.mult)
                MM(PS[j % 2][:, 0:128], onesf, dg, True, True)
                TS("dve", idxw[:, j * 128:(j + 1) * 128], PS[j % 2][:, 0:128], 128.0, bidx[:, 0:1], ALU.mult, ALU.add)
                for j2 in range(2):
                    TS("dve", idxd[j2][:, j * 128:(j + 1) * 128], PS[j % 2][:, 0:128], 256.0, pcol[:, j2:j2 + 1], ALU.mult, ALU.add)
            DUMP("t_e", t_e); DUMP("cnt", cnt); DUMP("pstart", pstart)
            for T in range(NTT):
                b_ = T % 2
                LD("sp_x", h2r[b_], H2S[T * 128:(T + 1) * 128, :], dr=["h2s"])
                for k in range(8):
                    STT(junk[:, 0:NE], eio, ef8[:, T, k:k + 1], pstart, ALU.is_equal, ALU.mult, accum=ps8[:, k:k + 1])
                TT("dve", ps8, ps8, pf8[:, T, :], ALU.add)
                CP("dve", slots[:, T, :], ps8)
                CP("act", h2p[b_].rearrange("t (k p) -> t k p", k=8), h2r[b_].rearrange("t (p k) -> t k p", k=8))
                for k in range(8):
                    P.add("pool", (lambda e, T=T, k=k, b_=b_: e.indirect_dma_start(
                        out=XS, out_offset=bass.IndirectOffsetOnAxis(ap=slots[:, T, k:k + 1], axis=0),
                        in_=h2p[b_], in_offset=None)),
                        reads=[slots[:, T, k:k + 1], h2p[b_]], dr=["xsz"], dw=["xs"], dma="psc")
            DUMP("slots", slots)

            NWB = 2
            wg32 = [cnew([8, 256], F32) for _ in range(NWB)]
            wu32 = [cnew([8, 256], F32) for _ in range(NWB)]
            wd32 = [cnew([2, D], F32) for _ in range(NWB)]
            wg = [cnew([8, 256]) for _ in range(NWB)]
            wu = [cnew([8, 256]) for _ in range(NWB)]
            wd = [cnew([2, D]) for _ in range(NWB)]
            xr = [cnew([D]) for _ in range(2)]
            XT = [cnew([8, 128]) for _ in range(2)]
            HT = [cnew([2, 128]) for _ in range(2)]
            sgt = [cnew([256], F32) for _ in range(2)]
            Yb = [cnew([D]) for _ in range(2)]
            wg_v = w_eg_h.ap().rearrange("e (p k) n -> (e p) (k n)", k=8)
            wu_v = w_eu_h.ap().rearrange("e (p k) n -> (e p) (k n)", k=8)
            wd_v = w_ed_h.ap().rearrange("e r n -> (e r) n")

            def gather_w(b, dst, srcv, idx):
                P.add("pool", (lambda e: e.indirect_dma_start(
                    out=dst, out_offset=None, in_=srcv,
                    in_offset=bass.IndirectOffsetOnAxis(ap=idx[:, b:b + 1], axis=0))),
                    reads=[idx[:, b:b + 1]], writes=[dst], dma="pwg")

            for b in range(NB if upto == "all" else (4 if upto == "C1" else 0)):
                wb_ = b % NWB
                b_ = b % 2
                gather_w(b, wg32[wb_].rearrange("p k n -> p (k n)"), wg_v, idxw)
                gather_w(b, wu32[wb_].rearrange("p k n -> p (k n)"), wu_v, idxw)
                for j2 in range(2):
                    gather_w(b, wd32[wb_][:, j2, :], wd_v, idxd[j2])
                CP("act", wg[wb_], wg32[wb_])
                CP("dve", wu[wb_], wu32[wb_])
                CP("act", wd[wb_][:, 0, :], wd32[wb_][:, 0, :])
                CP("dve", wd[wb_][:, 1, :], wd32[wb_][:, 1, :])
                LD("sp_w", xr[b_], XS[b * 128:(b + 1) * 128, :], dr=["xs"])
                pT = psT3 if b % 2 == 0 else psT3b
                for c in range(8):
                    TR(pT[:, c, :], xr[b_][:, c * 128:(c + 1) * 128], identb)
                CP("act" if b % 2 == 0 else "dve", XT[b_], pT)
                pgu = PS[b % 2]
                for j in range(4):
                    wsrc = wg[wb_] if j < 2 else wu[wb_]
                    for kc in range(8):
                        MM(pgu[:, j * 128:(j + 1) * 128], wsrc[:, kc, (j % 2) * 128:(j % 2 + 1) * 128], XT[b_][:, kc, :], kc == 0, kc == 7)
                ACT(sgt[b_], pgu[:, 0:256], AF.Silu)
                TT("dve", HT[b_].rearrange("p a b -> p (a b)"), sgt[b_], pgu[:, 256:512], ALU.mult)
                for n in range(2):
                    py = PS[2 + 2 * (b % 2) + n]
                    for j2 in range(2):
                        MM(py[:, :], HT[b_][:, j2, :], wd[wb_][:, j2, n * 512:(n + 1) * 512], j2 == 0, j2 == 1)
                    CP("act" if n == 0 else "dve", Yb[b_][:, n * 512:(n + 1) * 512], py[:, :])
                ST("sp_st", XS[b * 128:(b + 1) * 128, :], Yb[b_], dw=["ys"])
            oc[0] = o_after_slots
            ptl = [cnew([D], F32) for _ in range(2)]
            Yg = [cnew([8, D]) for _ in range(2)]
            acc = [cnew([D], F32) for _ in range(2)]
            g2all = cnew([nseq, D], F32)
            fgbc = cnew([D], F32)
            for s in range(nseq):
                LD("sp_c", g2all[:, s, :], modd[s:s + 1, 5 * D:6 * D].partition_broadcast(128), dr=["modd"])
            LD("sp_c", fgbc, fg_d.partition_broadcast(128))
            for T in range(NTT if upto == "all" else 0):
                s = T // NT
                b_ = T % 2
                st_ = stat(T)
                LD("sp_x", ptl[b_], PART[T * 128:(T + 1) * 128, :], dr=["part"])
                for k in range(8):
                    P.add("pool", (lambda e, T=T, k=k, b_=b_: e.indirect_dma_start(
                        out=Yg[b_][:, k, :], out_offset=None, in_=XS,
                        in_offset=bass.IndirectOffsetOnAxis(ap=slots[:, T, k:k + 1], axis=0))),
                        reads=[slots[:, T, k:k + 1]], writes=[Yg[b_][:, k, :]], dr=["ys"], dma="pg")
                a_ = acc[b_]
                TS("dve", a_, Yg[b_][:, 0, :], gts[:, T, 0:1], None, ALU.mult)
                for k in range(1, 8):
                    STT(a_, Yg[b_][:, k, :], gts[:, T, k:k + 1], a_, ALU.mult, ALU.add)
                TT("dve", a_, a_, g2all[:, s, :], ALU.mult)
                TT("pool", a_, a_, ptl[b_], ALU.add)
                STT(junk, a_, 1.0, a_, ALU.mult, ALU.mult, accum=st_[:, 0:1])
                ACT(st_[:, 1:2], st_[:, 0:1], AF.Ln, bias=epsc[:, 0:1], scale=1.0 / D)
                ACT(st_[:, 2:3], st_[:, 1:2], AF.Exp, scale=-0.5)
                STT(ptl[b_], a_, st_[:, 2:3], fgbc, ALU.mult, ALU.mult)
                ST("sp_st", out_d[T * 128:(T + 1) * 128, :], ptl[b_], dw=["out"])
        P.add("sp", None, dr=["out", "dbg", "part", "xs", "ys", "modd", "h2s", "xsz"])
        P.emit()
    nc._prog = P
    nc._dbg = list(dbg_outs.keys())
    return nc


def _prep_inputs(inputs, ncores, nseq):
    f = lambda a: np.ascontiguousarray(np.asarray(a, dtype=np.float32))
    cst = _host_consts()
    shared = {
        "w_ada": f(inputs["w_ada"]), "b_ada": f(inputs["b_ada"]).reshape(1, -1),
        "n1g": f(f(inputs["norm1_g"]).reshape(8, 128).T),
        "norm2_g": f(inputs["norm2_g"]).reshape(1, -1), "final_g": f(inputs["final_g"]).reshape(1, -1),
        "w_in": f(inputs["w_in"]),
        "bgc": f(f(inputs["b_gate"]).reshape(16, 128).T),
        "qng2": f(np.tile(f(inputs["qn_g"]), 2).reshape(128, 1)),
        "kng2": f(np.tile(f(inputs["kn_g"]), 2).reshape(128, 1)),
        "w_o_dil": f(inputs["w_o_dil"]), "w_o_gqa": f(inputs["w_o_gqa"]), "w_out": f(inputs["w_out"]),
        "router_w": f(inputs["router_w"]), "router_bias": f(inputs["router_bias"]).reshape(1, -1),
        "bidx": f((np.arange(((nseq * S * 8) // 128 + NE) // 128)[None, :] * 128 + np.arange(128)[:, None]).astype(np.float32)),
        "w_exp_gate": f(inputs["w_exp_gate"]), "w_exp_up": f(inputs["w_exp_up"]), "w_exp_down": f(inputs["w_exp_down"]),
        "w_sh_gate": f(inputs["w_sh_gate"]), "w_sh_up": f(inputs["w_sh_up"]), "w_sh_down": f(inputs["w_sh_down"]),
    }
    shared.update(cst)
    x = f(inputs["x"])
    c = f(inputs["c"])
    maps = []
    for i in range(ncores):
        m = dict(shared)
        m["x"] = x[i * nseq:(i + 1) * nseq].reshape(nseq * S, D)
        ci = c[i * nseq:(i + 1) * nseq]
        m["cT"] = f(ci.reshape(nseq, 8, 128).transpose(2, 1, 0))
        maps.append(m)
    return maps


def kernel(**inputs):
    ncores, nseq = 8, 4
    nc = build(nseq=nseq)
    maps = _prep_inputs(inputs, ncores, nseq)
    res = run_bass_kernel_spmd(nc, maps, core_ids=list(range(ncores)))
    out = np.concatenate([r["out"] for r in res.results], axis=0)
    return out.reshape(ncores * nseq, S, D).astype(np.float32)
```

```python
import contextlib
import numpy as np
import ml_dtypes
import concourse.bass as bass
import concourse.mybir as mybir
from concourse.bass_utils import run_bass_kernel_spmd

F32 = mybir.dt.float32
BF16 = mybir.dt.bfloat16
I32 = mybir.dt.int32
AF = mybir.ActivationFunctionType
ALU = mybir.AluOpType

_DSIZE = {F32: 4, BF16: 2, I32: 4}
GRAN = 256
SEM_LIM = 30000

D = 1024
S = 2048
NT = S // 128
NE = 256
ENC = 8192
MAXB = 64
EPS = 1e-6


class Op:
    __slots__ = ("eng", "fn", "waits", "venue", "idx", "clock", "inc")


def _region(ap):
    dsz = _DSIZE[ap.dtype]
    pairs = list(ap.ap)
    pstep = pairs[0][0]
    off = ap.offset
    if pstep > 0:
        off = off % pstep
    lo = off
    hi = off
    for (st, cnt) in pairs[1:]:
        if st >= 0:
            hi += st * (cnt - 1)
        else:
            lo += st * (cnt - 1)
    return (ap.tensor.name, lo * dsz, (hi + 1) * dsz)


class Prog:
    ENGS = ["pe", "act", "dve", "pool", "sp"]

    def __init__(self, nc):
        self.nc = nc
        self.ops = {e: [] for e in self.ENGS}
        self.know = {e: {} for e in self.ENGS}
        self.last_w = {}
        self.readers = {}
        self.dram_w = {}
        self.dram_r = {}
        self.venue_cnt = {}
        self.venue_last = {}
        self.streams = {}

    def _grans(self, ap):
        name, lo, hi = _region(ap)
        if name.startswith("ps"):
            return [(name, 0)]
        return [(name, g) for g in range(lo // GRAN, (hi - 1) // GRAN + 1)]

    def _dep(self, eng, d, waits):
        if d is None:
            return
        k = self.know[eng]
        if k.get(d.venue, 0) >= d.idx:
            return
        if d.venue == eng and eng == "pe":
            return
        waits.append(d)
        d.inc = True
        for v, n in d.clock.items():
            if k.get(v, 0) < n:
                k[v] = n

    def add(self, eng, fn, reads=(), writes=(), dr=(), dw=(), dma=None):
        op = Op()
        op.eng = eng
        op.fn = fn
        op.inc = False
        waits = []
        rg = []
        for ap in reads:
            rg += self._grans(ap)
        wg = []
        for ap in writes:
            wg += self._grans(ap)
        for g in rg:
            self._dep(eng, self.last_w.get(g), waits)
            if g[0].startswith("ps"):
                for r in self.readers.get(g, ()):
                    if r.eng != eng:
                        self._dep(eng, r, waits)
        for g in wg:
            self._dep(eng, self.last_w.get(g), waits)
            for r in self.readers.get(g, ()):
                self._dep(eng, r, waits)
        for key in dr:
            for w in self.dram_w.get(key, {}).values():
                self._dep(eng, w, waits)
        for key in dw:
            for r in self.dram_r.get(key, {}).values():
                self._dep(eng, r, waits)
        if dma is not None:
            st = self.streams[dma]
            assert st["eng"] == eng
            k = st["n"] % st["nsem"]
            st["n"] += 1
            venue = "dma:%s:%d" % (dma, k)
            self._dep(eng, self.venue_last.get(venue), waits)
            op.inc = True
        else:
            venue = eng
        op.venue = venue
        op.idx = self.venue_cnt.get(venue, 0) + 1
        self.venue_cnt[venue] = op.idx
        self.venue_last[venue] = op
        op.waits = waits
        ck = dict(self.know[eng])
        ck[venue] = op.idx
        op.clock = ck
        for g in rg:
            self.readers.setdefault(g, []).append(op)
        for g in wg:
            self.last_w[g] = op
            self.readers[g] = []
        for key in dr:
            self.dram_r.setdefault(key, {})[venue] = op
        for key in dw:
            self.dram_w.setdefault(key, {})[venue] = op
        self.ops[eng].append(op)
        return op

    def stream(self, name, eng, nsem):
        self.streams[name] = {"eng": eng, "nsem": nsem, "n": 0}

    def emit(self):
        nc = self.nc
        semval = {}
        counts = {}
        for eng in self.ENGS:
            for op in self.ops[eng]:
                if op.inc:
                    c = counts.get(op.venue, 0) + 1
                    counts[op.venue] = c
                    semval[id(op)] = c
        step = {}
        nsems = {}
        for v, c in counts.items():
            s = 16 if v.startswith("dma:") else 1
            step[v] = s
            lim = SEM_LIM // s
            nsems[v] = (c - 1) // lim + 1
        self.total_sems = sum(nsems.values())
        with contextlib.ExitStack() as es:
            sems = {}
            for v, n in nsems.items():
                sems[v] = [es.enter_context(nc.semaphore(("s_%s_%d" % (v, i)).replace(":", "_")))
                           for i in range(n)]

            def semof(op):
                c = semval[id(op)]
                s = step[op.venue]
                lim = SEM_LIM // s
                return sems[op.venue][(c - 1) // lim], ((c - 1) % lim + 1) * s

            def run(eng, e):
                for op in self.ops[eng]:
                    for d in op.waits:
                        sm, val = semof(d)
                        e.wait_ge(sm, val)
                    if op.fn is None:
                        continue
                    ins = op.fn(e)
                    if op.inc:
                        sm, val = semof(op)
                        ins.then_inc(sm, step[op.venue])

            with nc.Block() as block:
                @block.tensor
                def _(e):
                    run("pe", e)

                @block.scalar
                def _(e):
                    run("act", e)

                @block.vector
                def _(e):
                    run("dve", e)

                @block.gpsimd
                def _(e):
                    run("pool", e)

                @block.sync
                def _(e):
                    run("sp", e)


DIL = (1, 4, 16)
REACH = (64, 256, 1024)
MC0 = tuple(r + 511 for r in REACH)
MW = tuple(2 * r + 1150 for r in REACH)
MOFF = (0, MW[0], MW[0] + MW[1])
MTOT = sum(MW)


def _host_consts():
    bf = ml_dtypes.bfloat16
    c = {}
    c["identb"] = np.eye(128, dtype=np.float32).astype(bf)
    c["identf"] = np.eye(128, dtype=np.float32)
    r1 = np.zeros((128, 128), np.float32)
    ra = np.zeros((128, 128), np.float32)
    for m in range(128):
        dm = m % 64
        base = m - dm
        if dm < 32:
            r1[base + dm + 32, m] = -1.0
        else:
            r1[base + dm - 32, m] = 1.0
        sub = dm % 32
        blk = dm - sub
        if sub < 16:
            ra[base + blk + sub + 16, m] = -1.0
        else:
            ra[base + blk + sub - 16, m] = 1.0
    c["rm1"] = r1.astype(bf)
    c["rma"] = ra.astype(bf)
    bo = np.zeros((128, 128), np.float32)
    bo[:64, :64] = 1.0
    bo[64:, 64:] = 1.0
    c["bones"] = bo.astype(bf)
    c["utri"] = np.triu(np.ones((128, 128), np.float32), 1).astype(bf)
    c["ones"] = np.ones((128, 128), np.float32).astype(bf)
    t = np.arange(S, dtype=np.float32)
    inv1 = (np.float32(10000.0) ** (-np.arange(0, 64, 2, dtype=np.float32) / np.float32(64))).astype(np.float32)
    inva = (np.float32(10000.0) ** (-np.arange(0, 32, 2, dtype=np.float32) / np.float32(32))).astype(np.float32)
    row = (np.arange(S) // 64).astype(np.float32)
    col = (np.arange(S) % 64).astype(np.float32)
    cos1 = np.zeros((128, S), np.float32)
    sin1 = np.zeros((128, S), np.float32)
    cosa = np.zeros((128, S), np.float32)
    sina = np.zeros((128, S), np.float32)
    for p in range(128):
        dm = p % 64
        ang = (t * inv1[dm % 32]).astype(np.float32)
        cos1[p] = np.cos(ang)
        sin1[p] = np.sin(ang)
        if dm < 32:
            ang = (row * inva[dm % 16]).astype(np.float32)
        else:
            ang = (col * inva[(dm - 32) % 16]).astype(np.float32)
        cosa[p] = np.cos(ang)
        sina[p] = np.sin(ang)
    c["rope"] = np.stack([cos1, sin1, cosa, sina], axis=1).astype(bf)
    strips = []
    for g in range(3):
        i = np.arange(128)[:, None]
        cc = np.arange(MW[g])[None, :]
        dlt = i - cc + MC0[g]
        ok = (np.abs(dlt) <= REACH[g]) & (dlt % DIL[g] == 0)
        strips.append(ok.astype(np.float32))
    c["mstrip"] = np.concatenate(strips, axis=1).astype(bf)
    c["eidx"] = np.tile((np.arange(NE, dtype=np.float32) * ENC + 1.0)[None, :], (128, 1)).astype(np.float32)
    c["eio"] = np.tile(np.arange(NE, dtype=np.float32)[None, :], (128, 1)).astype(np.float32)
    return c


def build(nseq=4, upto="all", dbg=()):
    nc = bass.Bass("TRN2", target_bir_lowering=False)
    NTOK = nseq * S
    NTT = nseq * NT

    def din(name, shape, dt=F32):
        return nc.dram_tensor(name, list(shape), dt, kind="ExternalInput").ap()

    x_d = din("x", [NTOK, D])
    cT_d = din("cT", [128, 8, nseq])
    w_ada = din("w_ada", [D, 6 * D])
    b_ada = din("b_ada", [1, 6 * D])
    n1g_d = din("n1g", [128, 8])
    n2g_d = din("norm2_g", [1, D])
    fg_d = din("final_g", [1, D])
    w_in = din("w_in", [D, 5888])
    bgc_d = din("bgc", [128, 16])
    qng_d = din("qng2", [128, 1])
    kng_d = din("kng2", [128, 1])
    w_o_dil = din("w_o_dil", [256, D])
    w_o_gqa = din("w_o_gqa", [D, D])
    w_out = din("w_out", [D, D])
    router_w = din("router_w", [D, NE])
    rbias_d = din("router_bias", [1, NE])
    w_eg_h = nc.dram_tensor("w_exp_gate", [NE, D, 256], F32, kind="ExternalInput")
    w_eu_h = nc.dram_tensor("w_exp_up", [NE, D, 256], F32, kind="ExternalInput")
    w_ed_h = nc.dram_tensor("w_exp_down", [NE, 256, D], F32, kind="ExternalInput")
    w_sg = din("w_sh_gate", [D, 256])
    w_su = din("w_sh_up", [D, 256])
    w_sd = din("w_sh_down", [256, D])
    identb_d = din("identb", [128, 128], BF16)
    identf_d = din("identf", [128, 128])
    rm1_d = din("rm1", [128, 128], BF16)
    rma_d = din("rma", [128, 128], BF16)
    bones_d = din("bones", [128, 128], BF16)
    utri_d = din("utri", [128, 128], BF16)
    ones_d = din("ones", [128, 128], BF16)
    rope_d = din("rope", [128, 4, S], BF16)
    mstrip_d = din("mstrip", [128, MTOT], BF16)
    eidx_d = din("eidx", [128, NE])
    out_d = nc.dram_tensor("out", [NTOK, D], F32, kind="ExternalOutput").ap()
    modd = nc.dram_tensor("modd", [nseq, 6 * D], F32, kind="Internal").ap()
    NB = NTOK * 8 // 128 + NE
    NBJ = NB // 128
    XS = nc.dram_tensor("xs", [NB * 128, D], BF16, kind="Internal").ap()
    H2S = nc.dram_tensor("h2s", [NTOK, D], BF16, kind="Internal").ap()
    eio_d = din("eio", [128, NE])
    bidx_d = din("bidx", [128, NBJ])
    PART = nc.dram_tensor("part", [NTOK, D], F32, kind="Internal").ap()

    P = Prog(nc)
    P.stream("sp_c", "sp", 4)
    P.stream("sp_x", "sp", 2)
    P.stream("sp_w", "sp", 4)
    P.stream("sp_st", "sp", 4)
    P.stream("sp_z", "sp", 4)
    P.stream("pw", "pool", 6)
    P.stream("pwg", "pool", 12)
    P.stream("psc", "pool", 8)
    P.stream("pg", "pool", 8)
    dbg_outs = {}

    with contextlib.ExitStack() as es:
        ARENA_EL = 103 * 1024 + 512
        A = es.enter_context(nc.sbuf_tensor("arena", [128, ARENA_EL], BF16))
        PS = [es.enter_context(nc.psum_tensor("ps%d" % i, [128, 512], F32)) for i in range(8)]
        cur = [0]

        def alloc(nbytes):
            o = cur[0]
            cur[0] = (o + nbytes + 63) // 64 * 64
            assert cur[0] <= ARENA_EL * 2, ("SBUF overflow", cur[0])
            return o

        def view(off, shape, dt=BF16):
            n = 1
            for s_ in shape:
                n *= s_
            nb = n * _DSIZE[dt]
            assert off % 4 == 0
            ap = A[:, off // 2: off // 2 + nb // 2]
            if dt != BF16:
                ap = ap.bitcast(dt)
            if len(shape) == 2:
                ap = ap.rearrange("p (a b) -> p a b", a=shape[0])
            elif len(shape) == 3:
                ap = ap.rearrange("p (a b c) -> p a b c", a=shape[0], b=shape[1])
            return ap

        def new(shape, dt=BF16):
            n = 1
            for s_ in shape:
                n *= s_
            return view(alloc(n * _DSIZE[dt]), shape, dt)

        def aps(*xs):
            return [a for a in xs if a is not None and not isinstance(a, (int, float))]

        def MM(out, lhsT, rhs, start, stop):
            P.add("pe", lambda e: e.matmul(out, lhsT=lhsT, rhs=rhs, start=start, stop=stop),
                  reads=[lhsT, rhs], writes=[out])

        def TR(out, in_, ident):
            P.add("pe", lambda e: e.transpose(out=out, in_=in_, identity=ident), reads=[in_, ident], writes=[out])

        def ACT(out, in_, func, bias=None, scale=None, accum=None):
            kw = {}
            if bias is not None:
                kw["bias"] = bias
            if scale is not None:
                kw["scale"] = scale
            if accum is not None:
                kw["accum_out"] = accum
            P.add("act", lambda e: e.activation(out=out, in_=in_, func=func, **kw),
                  reads=aps(in_, bias, scale), writes=aps(out, accum))

        def TT(eng, out, in0, in1, op):
            P.add(eng, lambda e: e.tensor_tensor(out=out, in0=in0, in1=in1, op=op), reads=[in0, in1], writes=[out])

        def TS(eng, out, in0, s1, s2, op0, op1=None, accum=None):
            if accum is not None:
                P.add(eng, lambda e: e.tensor_scalar(out=out, in0=in0, scalar1=s1, scalar2=s2, op0=op0, op1=op1, accum_out=accum),
                      reads=aps(in0, s1, s2), writes=[out, accum])
            elif op1 is None:
                P.add(eng, lambda e: e.tensor_scalar(out=out, in0=in0, scalar1=s1, scalar2=None, op0=op0),
                      reads=aps(in0, s1), writes=[out])
            else:
                P.add(eng, lambda e: e.tensor_scalar(out=out, in0=in0, scalar1=s1, scalar2=s2, op0=op0, op1=op1),
                      reads=aps(in0, s1, s2), writes=[out])

        def STT(out, in0, scalar, in1, op0, op1, accum=None):
            kw = {}
            if accum is not None:
                kw["accum_out"] = accum
            P.add("dve", lambda e: e.scalar_tensor_tensor(out=out, in0=in0, scalar=scalar, in1=in1, op0=op0, op1=op1, **kw),
                  reads=aps(in0, scalar, in1), writes=aps(out, accum))

        def CP(eng, out, in_):
            if eng == "act":
                P.add("act", lambda e: e.copy(out=out, in_=in_), reads=[in_], writes=[out])
            else:
                P.add(eng, lambda e: e.tensor_copy(out=out, in_=in_), reads=[in_], writes=[out])

        def MAX8(out, in_):
            P.add("dve", lambda e: e.max(out=out, in_=in_), reads=[in_], writes=[out])

        def RECIP(out, in_):
            P.add("dve", lambda e: e.reciprocal(out=out, in_=in_), reads=[in_], writes=[out])

        def MEMSET(eng, ap, val):
            P.add(eng, lambda e: e.memset(ap, val), writes=[ap])

        def LD(stream, out, in_, dr=(), **kw):
            eng = P.streams[stream]["eng"]
            P.add(eng, lambda e: e.dma_start(out=out, in_=in_, **kw), writes=[out], dr=dr, dma=stream)

        def ST(stream, out, in_, dw=(), **kw):
            eng = P.streams[stream]["eng"]
            P.add(eng, lambda e: e.dma_start(out=out, in_=in_, **kw), reads=[in_], dw=dw, dma=stream)

        def DUMP(name, ap, dt=None):
            if name not in dbg:
                return
            shp = list(ap.shape)
            d_ = nc.dram_tensor("dbg_" + name, shp, ap.dtype, kind="ExternalOutput").ap()
            dbg_outs[name] = d_
            ST("sp_st", d_, ap, dw=["dbg"])

        def rstd_from_ssq(out, ssq, n, tmp):
            ACT(tmp, ssq, AF.Ln, bias=epsc[:, 0:1], scale=1.0 / n)
            ACT(out, tmp, AF.Exp, scale=-0.5)

        identb = new([128]); rm1 = new([128]); rma = new([128]); bones = new([128])
        utri = new([128]); ones = new([128])
        identf = new([128], F32); onesf = new([128], F32)
        rope = new([4, S])
        mstrip = new([MTOT])
        n1g = new([8], F32); bgc = new([16], F32); qng = new([1], F32); kng = new([1], F32)
        epsc = new([1], F32)
        eidx = new([NE], F32); rbias = new([NE], F32); eio = new([NE], F32)
        gts = new([NTT, 8], F32); cnt = new([NE], F32)
        ef8 = new([NTT, 8], F32); pf8 = new([NTT, 8], F32)
        gs1c = new([8], F32); sh1c = new([8], F32); sc1c = new([8], F32)
        small = new([64], F32)

        o_hT = alloc(8 * S * 2)
        o_QK = alloc(12 * S * 2)
        o_U1 = alloc(16384 * 2)
        o_vb = alloc(NT * 4 * 65 * 2)
        o_oA = alloc(2 * S * 2)
        hT = view(o_hT, [8, S])
        QK = view(o_QK, [12, S])
        va = view(o_U1, [NT, 12, 65])
        oB = view(o_U1, [8, S])
        vb = view(o_vb, [NT, 4, 65])
        oA = view(o_oA, [2, S])

        xt = new([D], F32)
        xn = new([D])
        wch = [new([8, 256]) for _ in range(4)]
        PT = [new([512]) for _ in range(3)]
        tb1 = [new([512]) for _ in range(2)]
        tsq = new([512])
        o_tf = cur[0]
        tf = [new([512], F32) for _ in range(3)]
        junk = view(o_tf, [D], F32)
        zt = new([D])
        SB_END = cur[0]

        psT3 = PS[7][:, :].bitcast(BF16).rearrange("p (a b) -> p a b", a=8)
        psT3b = PS[6][:, :].bitcast(BF16).rearrange("p (a b) -> p a b", a=8)

        def stat(i):
            return small[:, (i % 4) * 16:(i % 4) * 16 + 16]

        LD("sp_c", identb, identb_d); LD("sp_c", rm1, rm1_d); LD("sp_c", rma, rma_d)
        LD("sp_c", bones, bones_d); LD("sp_c", utri, utri_d); LD("sp_c", ones, ones_d)
        LD("sp_c", identf, identf_d); LD("sp_c", rope, rope_d); LD("sp_c", mstrip, mstrip_d)
        LD("sp_c", n1g, n1g_d); LD("sp_c", bgc, bgc_d); LD("sp_c", qng, qng_d); LD("sp_c", kng, kng_d)
        LD("sp_c", eidx, eidx_d)
        LD("sp_c", eio, eio_d)
        LD("sp_c", rbias, rbias_d.partition_broadcast(128))
        MEMSET("dve", epsc, EPS)
        MEMSET("dve", cnt, 0.0)
        MEMSET("dve", onesf, 1.0)
        MEMSET("pool", zt, 0.0)
        zfill = [0]
        NZ = NB
        MEMSET("pool", vb[:, :, :, 64:65], 1.0)

        sct = view(o_QK, [8, nseq], F32)
        LD("sp_c", sct, cT_d)
        ACT(sct, sct, AF.Silu)
        wst = [view(o_hT, [8, 512], F32), view(o_hT + 16384, [8, 512], F32)]
        mrow = [view(o_U1, [512], F32), view(o_U1 + 2048, [512], F32)]
        brow = view(o_U1 + 4096, [6 * D], F32)
        LD("sp_c", brow[0:nseq, :], b_ada.partition_broadcast(nseq))
        for blk in range(12):
            wb_ = wst[blk % 2]
            LD("sp_w", wb_, w_ada[:, blk * 512:(blk + 1) * 512].rearrange("(kc p) n -> p kc n", p=128))
            pm = PS[blk % 2]
            for kc in range(8):
                MM(pm[0:nseq, :], sct[:, kc, :], wb_[:, kc, :], kc == 0, kc == 7)
            mr = mrow[blk % 2]
            TT("dve", mr[0:nseq, :], pm[0:nseq, :], brow[0:nseq, blk * 512:(blk + 1) * 512], ALU.add)
            ST("sp_st", modd[:, blk * 512:(blk + 1) * 512], mr[0:nseq, :], dw=["modd"])

        def load_wblock(buf, col0, ncols, dst0=0, src=None, kcn=8):
            src = w_in if src is None else src
            LD("pw", buf[:, 0:kcn, dst0:dst0 + ncols],
               src[:, col0:col0 + ncols].rearrange("(kc p) n -> p kc n", p=128))

        pacc_i = [0]

        def proj_fm(wbuf, c0, tb):
            pm = PS[pacc_i[0] % 2]
            pacc_i[0] += 1
            for kc in range(8):
                MM(pm[:, :], wbuf[:, kc, c0:c0 + 128], hT[:, kc, tb * 512:(tb + 1) * 512], kc == 0, kc == 7)
            return pm

        def rope_plain(pm, dst, tb, ti):
            tok = slice(tb * 512, (tb + 1) * 512)
            qg = tb1[ti % 2]
            CP("act", qg, pm[:, :])
            pr = PS[2 + ti % 2]
            MM(pr[:, :], rm1, qg, True, True)
            TT("dve", tf[0], pm[:, :], rope[:, 0, tok], ALU.mult)
            TT("dve", tf[1], pr[:, :], rope[:, 1, tok], ALU.mult)
            TT("dve", dst, tf[0], tf[1], ALU.add)

        def rope_norm(pm, dst, tb, ti, gcol):
            tok = slice(tb * 512, (tb + 1) * 512)
            ACT(tsq, pm[:, :], AF.Square)
            qg = tb1[ti % 2]
            ACT(qg, pm[:, :], AF.Copy, scale=gcol[:, 0:1])
            pq = PS[2]
            pr = PS[3]
            MM(pq[:, :], bones, tsq, True, True)
            MM(pr[:, :], rma, qg, True, True)
            ACT(tf[2], pq[:, :], AF.Ln, bias=epsc[:, 0:1], scale=1.0 / 64.0)
            ACT(tf[2], tf[2], AF.Exp, scale=-0.5)
            TT("dve", tf[0], qg, rope[:, 2, tok], ALU.mult)
            TT("dve", tf[1], pr[:, :], rope[:, 3, tok], ALU.mult)
            TT("dve", tf[0], tf[0], tf[1], ALU.add)
            TT("dve", dst, tf[0], tf[2], ALU.mult)

        pend_norm = []

        def flush_norm():
            while pend_norm:
                po, dst = pend_norm.pop(0)
                rr = tf[2]
                RECIP(rr[64:65, :], po[64:65, :])
                pb_ = PS[6]
                MM(pb_[0:64, :], onesf[64:65, 0:64], rr[64:65, :], True, True)
                CP("act", tf[0][0:64, :], pb_[0:64, :])
                TT("dve", dst, po[0:64, :], tf[0][0:64, :], ALU.mult)

        def attention(branches, dst_chunk, dst_half):
            for qc in range(4):
                po = PS[4 + (qc % 2)]
                blocks = []
                for (q_fn, k_fn, v_fn, g) in branches:
                    for kb in range(NT):
                        if g is not None:
                            delta = kb * 128 - qc * 512
                            if delta + 127 < -REACH[g] or delta - 511 > REACH[g]:
                                continue
                        blocks.append((q_fn, k_fn, v_fn, g, kb))
                nb = len(blocks)

                def qk(i):
                    q_fn, k_fn, v_fn, g, kb = blocks[i]
                    psc = PS[i % 3]
                    MM(psc[:, :], k_fn(kb), q_fn(qc), True, True)
                    pt = PT[i % 3]
                    ACT(pt, psc[:, :], AF.Exp, scale=0.125)
                    if g is not None:
                        c0 = MOFF[g] + MC0[g] - (kb * 128 - qc * 512)
                        TT("dve", pt, pt, mstrip[:, c0:c0 + 512], ALU.mult)

                def pv(i):
                    q_fn, k_fn, v_fn, g, kb = blocks[i]
                    MM(po[0:65, :], v_fn(kb), PT[i % 3], i == 0, i == nb - 1)

                LA = 2
                for i in range(min(LA, nb)):
                    qk(i)
                flush_norm()
                for i in range(nb):
                    if i + LA < nb:
                        qk(i + LA)
                    pv(i)
                pend_norm.append((po, dst_chunk[dst_half * 64:(dst_half + 1) * 64, qc * 512:(qc + 1) * 512]))

        g1bc = view(o_hT, [D], F32); gs2bc = view(o_hT + 4096, [D], F32); sh2bc = view(o_hT + 8192, [D], F32)
        g2bc_t = view(o_hT + 12288, [D], F32); n2gbc = view(o_hT + 16384, [D], F32)
        x1 = view(o_hT + 20480, [D], F32); h2 = view(o_hT + 24576, [D], F32)
        h2b = view(o_hT + 28672, [D]); h2Tb = view(o_hT + 30720, [8, 128])
        wshgu = view(o_U1, [8, 512]); wshd = view(o_U1 + 8192, [2, 1024])
        routw = view(o_U1 + 12288, [8, NE], F32); h2T = view(o_U1 + 20480, [8, 128], F32)
        r_sc = view(o_U1 + 24576, [NE], F32); r_bi = view(o_U1 + 25600, [NE], F32)
        r_ma = view(o_U1 + 26624, [NE], F32); r_se = view(o_U1 + 27648, [NE], F32)
        r_ga = view(o_U1 + 28672, [NE], F32); r_D = view(o_U1 + 29696, [NE], F32)
        r_selb = view(o_U1 + 30720, [NE]); r_m8 = view(o_U1 + 31232, [8, 8], F32)
        r_gs = view(o_U1 + 31488, [8], F32); r_gs8 = view(o_U1 + 31520, [8], F32); r_gm = view(o_U1 + 31552, [8], F32)
        r_pen = view(o_U1 + 31584, [8], F32); r_t8 = view(o_U1 + 31616, [8], F32); r_s8 = view(o_U1 + 31648, [8], F32)
        r_HT = view(o_U1 + 31744, [2, 128])
        wout = view(o_QK + 8 * S * 2, [8, D])
        sgs = tf[0]

        def do_seq(s):
            tok0 = s * S
            LD("sp_c", sh1c, modd[s:s + 1, 0:D].rearrange("o (j p) -> p (o j)", p=128), dr=["modd"],
               allow_slow_non_contiguous=True)
            LD("sp_c", sc1c, modd[s:s + 1, D:2 * D].rearrange("o (j p) -> p (o j)", p=128), dr=["modd"],
               allow_slow_non_contiguous=True)
            STT(gs1c, sc1c, 1.0, n1g, ALU.add, ALU.mult)
            MEMSET("pool", va[:, :, :, 64:65], 1.0)
            for t in range(NT):
                st_ = stat(t)
                LD("sp_x", xt, x_d[tok0 + t * 128: tok0 + (t + 1) * 128, :])
                for _z in range(-(-NZ // NTT)):
                    if zfill[0] < NZ:
                        z0 = zfill[0] * 128
                        ST("sp_z", XS[z0:z0 + 128, :], zt, dw=["xsz"])
                        zfill[0] += 1
                STT(junk, xt, 1.0, xt, ALU.mult, ALU.mult, accum=st_[:, 0:1])
                ACT(st_[:, 1:2], st_[:, 0:1], AF.Ln, bias=epsc[:, 0:1], scale=1.0 / D)
                ACT(st_[:, 2:3], st_[:, 1:2], AF.Exp, scale=-0.5)
                ACT(xn, xt, AF.Copy, scale=st_[:, 2:3])
                for c in range(8):
                    TR(psT3[:, c, :], xn[:, c * 128:(c + 1) * 128], identb)
                for c in range(8):
                    TS("dve", hT[:, c, t * 128:(t + 1) * 128], psT3[:, c, :],
                       gs1c[:, c:c + 1], sh1c[:, c:c + 1], ALU.mult, ALU.add)
            DUMP("hT", hT)
            if upto == "A1":
                return
            ti = 0
            for bi in range(6):
                wb_ = wch[bi % 4]
                load_wblock(wb_, bi * 256, 256)
                for c2 in range(2):
                    ch = bi * 2 + c2
                    for tb in range(4):
                        pm = proj_fm(wb_, c2 * 128, tb)
                        rope_plain(pm, QK[:, ch, tb * 512:(tb + 1) * 512], tb, ti)
                        ti += 1
            DUMP("qaka", QK)
            if upto == "A2q":
                return
            for vbi in range(4):
                wb_ = wch[(2 + vbi) % 4]
                load_wblock(wb_, (1536 + vbi * 256) if vbi < 3 else 3584, 256)
                for t in range(NT):
                    pm = PS[pacc_i[0] % 2]
                    pacc_i[0] += 1
                    for kc in range(8):
                        MM(pm[:, 0:256], hT[:, kc, t * 128:(t + 1) * 128], wb_[:, kc, :], kc == 0, kc == 7)
                    src = pm[:, 0:256].rearrange("p (h d) -> p h d", h=4)
                    if vbi < 3:
                        CP("act" if t % 2 == 0 else "dve", va[:, t, vbi * 4:(vbi + 1) * 4, 0:64], src)
                    else:
                        CP("act" if t % 2 == 0 else "dve", vb[:, t, :, 0:64], src)
            DUMP("va", va); DUMP("vb", vb)
            if upto == "A2":
                return
            for h in range(4):
                br = []
                for g in range(3):
                    hh = g * 4 + h
                    chk, hf = hh // 2, hh % 2
                    br.append((
                        (lambda qc, chk=chk, hf=hf: QK[hf * 64:(hf + 1) * 64, chk, qc * 512:(qc + 1) * 512]),
                        (lambda kb, chk=chk, hf=hf: QK[hf * 64:(hf + 1) * 64, 6 + chk, kb * 128:(kb + 1) * 128]),
                        (lambda kb, hh=hh: va[:, kb, hh, :]),
                        g))
                attention(br, oA[:, h // 2, :], h % 2)
            flush_norm()
            DUMP("oA", oA)
            if upto == "dil":
                return
            ti = 0
            for bi in range(4):
                wb_ = wch[bi % 4]
                load_wblock(wb_, 2304 + bi * 256, 256)
                for c2 in range(2):
                    ch = bi * 2 + c2
                    for tb in range(4):
                        pm = proj_fm(wb_, c2 * 128, tb)
                        rope_norm(pm, QK[:, ch, tb * 512:(tb + 1) * 512], tb, ti, qng)
                        ti += 1
            for kv in range(4):
                wb_ = wch[kv % 4]
                load_wblock(wb_, 3328 + kv * 64, 64, dst0=0)
                load_wblock(wb_, 3328 + kv * 64, 64, dst0=64)
                for tb in range(4):
                    pm = proj_fm(wb_, 0, tb)
                    rope_norm(pm, QK[:, 8 + kv, tb * 512:(tb + 1) * 512], tb, ti, kng)
                    ti += 1
            DUMP("qbkb", QK)
            for hq in range(16):
                kv, chk, hf = hq // 4, hq // 2, hq % 2
                br = [(
                    (lambda qc, chk=chk, hf=hf: QK[hf * 64:(hf + 1) * 64, chk, qc * 512:(qc + 1) * 512]),
                    (lambda kb, kv=kv, hf=hf: QK[hf * 64:(hf + 1) * 64, 8 + kv, kb * 128:(kb + 1) * 128]),
                    (lambda kb, kv=kv: vb[:, kb, kv, :]),
                    None)]
                attention(br, oB[:, chk, :], hf)
            flush_norm()
            DUMP("oB", oB)
            if upto == "gqa":
                return
            for m in range(8):
                wg_ = wch[(m % 2) * 2]
                wo_ = wch[(m % 2) * 2 + 1]
                load_wblock(wg_, 3840 + m * 128, 128, dst0=0)
                load_wblock(wg_, 4864 + m * 128, 128, dst0=128)
                load_wblock(wo_, m * 128, 128, dst0=0, src=w_o_gqa)
                load_wblock(wo_, m * 128, 128, dst0=128, src=w_o_dil, kcn=2)
                for tb in range(4):
                    tok = slice(tb * 512, (tb + 1) * 512)
                    pa, pg, pb_, pg2 = PS[0], PS[1], PS[2], PS[3]
                    for c in range(2):
                        MM(pa[:, :], wo_[:, c, 128:256], oA[:, c, tok], c == 0, c == 1)
                    for kc in range(8):
                        MM(pg[:, :], wg_[:, kc, 0:128], hT[:, kc, tok], kc == 0, kc == 7)
                    ACT(tb1[0], pg[:, :], AF.Sigmoid, bias=bgc[:, m:m + 1])
                    TT("dve", tf[0], tb1[0], pa[:, :], ALU.mult)
                    for kc in range(8):
                        MM(pb_[:, :], wo_[:, kc, 0:128], oB[:, kc, tok], kc == 0, kc == 7)
                    for kc in range(8):
                        MM(pg2[:, :], wg_[:, kc, 128:256], hT[:, kc, tok], kc == 0, kc == 7)
                    ACT(tb1[1], pg2[:, :], AF.Sigmoid, bias=bgc[:, 8 + m:9 + m])
                    TT("dve", tf[1], tb1[1], pb_[:, :], ALU.mult)
                    TT("dve", QK[:, m, tok], tf[0], tf[1], ALU.add)
            DUMP("zT", QK)
            if upto == "A4":
                return
            LD("pw", wout, w_out.rearrange("(kc p) n -> p kc n", p=128))
            LD("pw", wshgu[:, :, 0:256], w_sg.rearrange("(kc p) n -> p kc n", p=128))
            LD("pw", wshgu[:, :, 256:512], w_su.rearrange("(kc p) n -> p kc n", p=128))
            LD("pw", wshd, w_sd.rearrange("(j p) n -> p j n", p=128))
            LD("sp_w", routw, router_w.rearrange("(kc p) n -> p kc n", p=128))
            LD("sp_c", g1bc, modd[s:s + 1, 2 * D:3 * D].partition_broadcast(128), dr=["modd"])
            LD("sp_c", sh2bc, modd[s:s + 1, 3 * D:4 * D].partition_broadcast(128), dr=["modd"])
            LD("sp_c", gs2bc, modd[s:s + 1, 4 * D:5 * D].partition_broadcast(128), dr=["modd"])
            LD("sp_c", g2bc_t, modd[s:s + 1, 5 * D:6 * D].partition_broadcast(128), dr=["modd"])
            LD("sp_c", n2gbc, n2g_d.partition_broadcast(128))
            STT(gs2bc, gs2bc, 1.0, n2gbc, ALU.add, ALU.mult)
            for t in range(NT):
                T = s * NT + t
                st_ = stat(t)
                for n in range(2):
                    for kc in range(8):
                        MM(PS[n][:, :], QK[:, kc, t * 128:(t + 1) * 128], wout[:, kc, n * 512:(n + 1) * 512], kc == 0, kc == 7)
                LD("sp_x", xt, x_d[tok0 + t * 128: tok0 + (t + 1) * 128, :])
                for n in range(2):
                    TT("dve", x1[:, n * 512:(n + 1) * 512], PS[n][:, :], g1bc[:, n * 512:(n + 1) * 512], ALU.mult)
                TT("pool", x1, x1, xt, ALU.add)
                STT(junk, x1, 1.0, x1, ALU.mult, ALU.mult, accum=st_[:, 0:1])
                ACT(st_[:, 1:2], st_[:, 0:1], AF.Ln, bias=epsc[:, 0:1], scale=1.0 / D)
                ACT(st_[:, 2:3], st_[:, 1:2], AF.Exp, scale=-0.5)
                STT(h2, x1, st_[:, 2:3], gs2bc, ALU.mult, ALU.mult)
                TT("pool", h2, h2, sh2bc, ALU.add)
                CP("act", h2b, h2)
                for c in range(8):
                    pX = PS[2 + c // 4][:, :].rearrange("p (a b) -> p a b", a=4)
                    TR(pX[:, c % 4, :], h2[:, c * 128:(c + 1) * 128], identf)
                for hf in range(2):
                    pX = PS[2 + hf][:, :].rearrange("p (a b) -> p a b", a=4)
                    CP("act", h2T[:, hf * 4:(hf + 1) * 4, :], pX)
                    CP("dve", h2Tb[:, hf * 4:(hf + 1) * 4, :], pX)
                pr = PS[4]
                for kc in range(8):
                    MM(pr[:, 0:NE], h2T[:, kc, :], routw[:, kc, :], kc == 0, kc == 7)
                ACT(r_sc, pr[:, 0:NE], AF.Sigmoid)
                TT("dve", r_bi, r_sc, rbias, ALU.add)
                for g in range(8):
                    MAX8(r_m8[:, g, :], r_bi[:, g * 32:(g + 1) * 32])
                TT("dve", r_gs, r_m8[:, :, 0], r_m8[:, :, 1], ALU.add)
                MAX8(r_gs8, r_gs)
                TS("dve", r_gm, r_gs, r_gs8[:, 3:4], None, ALU.is_ge)
                TS("dve", r_pen, r_gm, -1.0, 10.0, ALU.add, ALU.mult)
                for g in range(8):
                    TS("dve", r_ma[:, g * 32:(g + 1) * 32], r_bi[:, g * 32:(g + 1) * 32],
                       r_gm[:, g:g + 1], r_pen[:, g:g + 1], ALU.mult, ALU.add)
                MAX8(r_t8, r_ma)
                TS("dve", r_se, r_ma, r_t8[:, 7:8], None, ALU.is_ge)
                STT(r_ga, r_sc, 1.0, r_se, ALU.mult, ALU.mult, accum=st_[:, 4:5])
                RECIP(st_[:, 5:6], st_[:, 4:5])
                TS("dve", r_ga, r_ga, st_[:, 5:6], 2.5, ALU.mult, ALU.mult)
                CP("act", r_selb, r_se)
                pc = PS[5]
                MM(pc[:, 0:NE], utri, r_selb, True, True)
                MM(pc[:, NE:2 * NE], ones, r_selb, True, True)
                TT("dve", r_D, pc[:, 0:NE], cnt, ALU.add)
                TT("dve", r_D, r_D, eidx, ALU.add)
                TT("dve", r_D, r_D, r_se, ALU.mult)
                TT("dve", cnt, cnt, pc[:, NE:2 * NE], ALU.add)
                MAX8(r_s8, r_D)
                STT(r_ma, eio, 1.0, r_se, ALU.add, ALU.mult)
                MAX8(r_t8, r_ma)
                TS("dve", ef8[:, T, :], r_t8, -1.0, None, ALU.add)
                STT(pf8[:, T, :], ef8[:, T, :], -float(ENC), r_s8, ALU.mult, ALU.add)
                TS("dve", pf8[:, T, :], pf8[:, T, :], -1.0, None, ALU.add)
                for k in range(8):
                    STT(junk[:, 0:NE], r_D, r_s8[:, k:k + 1], r_ga, ALU.is_equal, ALU.mult, accum=gts[:, T, k:k + 1])
                ST("sp_st", H2S[T * 128:(T + 1) * 128, :], h2b, dw=["h2s"])
                pgu = PS[6]
                for j in range(4):
                    for kc in range(8):
                        MM(pgu[:, j * 128:(j + 1) * 128], wshgu[:, kc, j * 128:(j + 1) * 128], h2Tb[:, kc, :], kc == 0, kc == 7)
                ACT(sgs[:, 0:256], pgu[:, 0:256], AF.Silu)
                TT("dve", r_HT.rearrange("p a b -> p (a b)"), sgs[:, 0:256], pgu[:, 256:512], ALU.mult)
                for n in range(2):
                    for j in range(2):
                        MM(PS[n][:, :], r_HT[:, j, :], wshd[:, j, n * 512:(n + 1) * 512], j == 0, j == 1)
                for n in range(2):
                    TT("dve", h2[:, n * 512:(n + 1) * 512], PS[n][:, :], g2bc_t[:, n * 512:(n + 1) * 512], ALU.mult)
                TT("pool", h2, h2, x1, ALU.add)
                ST("sp_st", PART[T * 128:(T + 1) * 128, :], h2, dw=["part"])
                if t == 0:
                    DUMP("x1", x1); DUMP("rsc", r_sc); DUMP("rse", r_se); DUMP("rga", r_ga); DUMP("rD", r_D)

        for s in range(nseq):
            do_seq(s)
        DUMP("gts", gts)

        if upto in ("all", "C0", "C1"):
            oc = [o_hT]

            def cnew(shape, dt=BF16):
                n = 1
                for s_ in shape:
                    n *= s_
                o = oc[0]
                oc[0] = (o + n * _DSIZE[dt] + 63) // 64 * 64
                assert oc[0] <= SB_END
                return view(o, shape, dt)
            slots = cnew([NTT, 8], I32)
            o_after_slots = oc[0]
            nblk = cnew([NE], F32); pendb = cnew([NE], F32); onesr = cnew([NE], F32); bidx = cnew([NBJ], F32)
            pstart = cnew([NE], F32)
            t_e = cnew([NBJ], F32)
            ps8 = cnew([8], F32)
            h2r = [cnew([D]) for _ in range(2)]
            h2p = [cnew([D]) for _ in range(2)]
            LD("sp_c", bidx, bidx_d)
            MEMSET("dve", onesr, 1.0)
            TS("dve", nblk, cnt, 0.0, None, ALU.is_gt)
            for j in range(1, MAXB):
                STT(nblk, cnt, float(128 * j), nblk, ALU.is_gt, ALU.add)
            P.add("dve", lambda e: e.tensor_tensor_scan(out=pendb, data0=onesr, data1=nblk, initial=0.0,
                                                          op0=ALU.mult, op1=ALU.add),
                  reads=[onesr, nblk], writes=[pendb])
            TT("dve", pstart, pendb, nblk, ALU.subtract)
            TS("dve", pstart, pstart, 128.0, None, ALU.mult)
            for j in range(NBJ):
                TS("dve", junk[:, 0:NE], pendb, bidx[:, j:j + 1], 0.0, ALU.is_le, ALU.add, accum=t_e[:, j:j + 1])
            TS("dve", t_e, t_e, float(NE - 1), None, ALU.min)
            idxw = cnew([NB], I32)
            idxd = [cnew([NB], I32) for _ in range(2)]
            pcol = cnew([2], F32)
            dg = cnew([128], F32)
            for j in range(2):
                TS("dve", pcol[:, j:j + 1], bidx[:, 0:1], float(j * 128), None, ALU.add)
            for j in range(NBJ):
                TS("dve", dg, identf, t_e[:, j:j + 1], None, ALU.mult)
                MM(PS[j % 2][:, 0:128], onesf, dg, True, True)
                TS("dve", idxw[:, j * 128:(j + 1) * 128], PS[j % 2][:, 0:128], 128.0, bidx[:, 0:1], ALU.mult, ALU.add)
                for j2 in range(2):
                    TS("dve", idxd[j2][:, j * 128:(j + 1) * 128], PS[j % 2][:, 0:128], 256.0, pcol[:, j2:j2 + 1], ALU.mult, ALU.add)
            DUMP("t_e", t_e); DUMP("cnt", cnt); DUMP("pstart", pstart)
            for T in range(NTT):
                b_ = T % 2
                LD("sp_x", h2r[b_], H2S[T * 128:(T + 1) * 128, :], dr=["h2s"])
                for k in range(8):
                    STT(junk[:, 0:NE], eio, ef8[:, T, k:k + 1], pstart, ALU.is_equal, ALU.mult, accum=ps8[:, k:k + 1])
                TT("dve", ps8, ps8, pf8[:, T, :], ALU.add)
                CP("dve", slots[:, T, :], ps8)
                CP("act", h2p[b_].rearrange("t (k p) -> t k p", k=8), h2r[b_].rearrange("t (p k) -> t k p", k=8))
                for k in range(8):
                    P.add("pool", (lambda e, T=T, k=k, b_=b_: e.indirect_dma_start(
                        out=XS, out_offset=bass.IndirectOffsetOnAxis(ap=slots[:, T, k:k + 1], axis=0),
                        in_=h2p[b_], in_offset=None)),
                        reads=[slots[:, T, k:k + 1], h2p[b_]], dr=["xsz"], dw=["xs"], dma="psc")
            DUMP("slots", slots)

            NWB = 2
            NW32 = 4
            wg32 = [cnew([8, 256], F32) for _ in range(NW32)]
            wu32 = [cnew([8, 256], F32) for _ in range(NW32)]
            wd32 = [cnew([2, D], F32) for _ in range(NW32)]
            wg = [cnew([8, 256]) for _ in range(NWB)]
            wu = [cnew([8, 256]) for _ in range(NWB)]
            wd = [cnew([2, D]) for _ in range(NWB)]
            xr = [cnew([D]) for _ in range(2)]
            XT = [cnew([8, 128]) for _ in range(2)]
            HT = [cnew([2, 128]) for _ in range(2)]
            sgt = [cnew([256], F32) for _ in range(2)]
            Yb = [cnew([D]) for _ in range(2)]
            wg_v = w_eg_h.ap().rearrange("e (p k) n -> (e p) (k n)", k=8)
            wu_v = w_eu_h.ap().rearrange("e (p k) n -> (e p) (k n)", k=8)
            wd_v = w_ed_h.ap().rearrange("e r n -> (e r) n")

            def gather_w(b, dst, srcv, idx):
                P.add("pool", (lambda e: e.indirect_dma_start(
                    out=dst, out_offset=None, in_=srcv,
                    in_offset=bass.IndirectOffsetOnAxis(ap=idx[:, b:b + 1], axis=0))),
                    reads=[idx[:, b:b + 1]], writes=[dst], dma="pwg")

            for b in range(NB if upto == "all" else (4 if upto == "C1" else 0)):
                wb_ = b % NWB
                b_ = b % 2
                w3_ = b % NW32
                gather_w(b, wg32[w3_].rearrange("p k n -> p (k n)"), wg_v, idxw)
                gather_w(b, wu32[w3_].rearrange("p k n -> p (k n)"), wu_v, idxw)
                for j2 in range(2):
                    gather_w(b, wd32[w3_][:, j2, :], wd_v, idxd[j2])
                CP("act", wg[wb_], wg32[w3_])
                CP("dve", wu[wb_], wu32[w3_])
                CP("act", wd[wb_][:, 0, :], wd32[w3_][:, 0, :])
                CP("dve", wd[wb_][:, 1, :], wd32[w3_][:, 1, :])
                LD("sp_w", xr[b_], XS[b * 128:(b + 1) * 128, :], dr=["xs"])
                pT = psT3 if b % 2 == 0 else psT3b
                for c in range(8):
                    TR(pT[:, c, :], xr[b_][:, c * 128:(c + 1) * 128], identb)
                CP("act" if b % 2 == 0 else "dve", XT[b_], pT)
                pgu = PS[b % 2]
                for j in range(4):
                    wsrc = wg[wb_] if j < 2 else wu[wb_]
                    for kc in range(8):
                        MM(pgu[:, j * 128:(j + 1) * 128], wsrc[:, kc, (j % 2) * 128:(j % 2 + 1) * 128], XT[b_][:, kc, :], kc == 0, kc == 7)
                ACT(sgt[b_], pgu[:, 0:256], AF.Silu)
                TT("dve", HT[b_].rearrange("p a b -> p (a b)"), sgt[b_], pgu[:, 256:512], ALU.mult)
                for n in range(2):
                    py = PS[2 + 2 * (b % 2) + n]
                    for j2 in range(2):
                        MM(py[:, :], HT[b_][:, j2, :], wd[wb_][:, j2, n * 512:(n + 1) * 512], j2 == 0, j2 == 1)
                    CP("act" if n == 0 else "dve", Yb[b_][:, n * 512:(n + 1) * 512], py[:, :])
                ST("sp_st", XS[b * 128:(b + 1) * 128, :], Yb[b_], dw=["ys"])
            oc[0] = o_after_slots
            ptl = [cnew([D], F32) for _ in range(2)]
            Yg = [cnew([8, D]) for _ in range(2)]
            acc = [cnew([D], F32) for _ in range(2)]
            g2all = cnew([nseq, D], F32)
            fgbc = cnew([D], F32)
            for s in range(nseq):
                LD("sp_c", g2all[:, s, :], modd[s:s + 1, 5 * D:6 * D].partition_broadcast(128), dr=["modd"])
            LD("sp_c", fgbc, fg_d.partition_broadcast(128))
            for T in range(NTT if upto == "all" else 0):
                s = T // NT
                b_ = T % 2
                st_ = stat(T)
                LD("sp_x", ptl[b_], PART[T * 128:(T + 1) * 128, :], dr=["part"])
                for k in range(8):
                    P.add("pool", (lambda e, T=T, k=k, b_=b_: e.indirect_dma_start(
                        out=Yg[b_][:, k, :], out_offset=None, in_=XS,
                        in_offset=bass.IndirectOffsetOnAxis(ap=slots[:, T, k:k + 1], axis=0))),
                        reads=[slots[:, T, k:k + 1]], writes=[Yg[b_][:, k, :]], dr=["ys"], dma="pg")
                a_ = acc[b_]
                TS("dve", a_, Yg[b_][:, 0, :], gts[:, T, 0:1], None, ALU.mult)
                for k in range(1, 8):
                    STT(a_, Yg[b_][:, k, :], gts[:, T, k:k + 1], a_, ALU.mult, ALU.add)
                TT("dve", a_, a_, g2all[:, s, :], ALU.mult)
                TT("pool", a_, a_, ptl[b_], ALU.add)
                STT(junk, a_, 1.0, a_, ALU.mult, ALU.mult, accum=st_[:, 0:1])
                ACT(st_[:, 1:2], st_[:, 0:1], AF.Ln, bias=epsc[:, 0:1], scale=1.0 / D)
                ACT(st_[:, 2:3], st_[:, 1:2], AF.Exp, scale=-0.5)
                STT(ptl[b_], a_, st_[:, 2:3], fgbc, ALU.mult, ALU.mult)
                ST("sp_st", out_d[T * 128:(T + 1) * 128, :], ptl[b_], dw=["out"])
        P.add("sp", None, dr=["out", "dbg", "part", "xs", "ys", "modd", "h2s", "xsz"])
        P.emit()
    nc._prog = P
    nc._dbg = list(dbg_outs.keys())
    return nc


def _prep_inputs(inputs, ncores, nseq):
    f = lambda a: np.ascontiguousarray(np.asarray(a, dtype=np.float32))
    cst = _host_consts()
    shared = {
        "w_ada": f(inputs["w_ada"]), "b_ada": f(inputs["b_ada"]).reshape(1, -1),
        "n1g": f(f(inputs["norm1_g"]).reshape(8, 128).T),
        "norm2_g": f(inputs["norm2_g"]).reshape(1, -1), "final_g": f(inputs["final_g"]).reshape(1, -1),
        "w_in": f(inputs["w_in"]),
        "bgc": f(f(inputs["b_gate"]).reshape(16, 128).T),
        "qng2": f(np.tile(f(inputs["qn_g"]), 2).reshape(128, 1)),
        "kng2": f(np.tile(f(inputs["kn_g"]), 2).reshape(128, 1)),
        "w_o_dil": f(inputs["w_o_dil"]), "w_o_gqa": f(inputs["w_o_gqa"]), "w_out": f(inputs["w_out"]),
        "router_w": f(inputs["router_w"]), "router_bias": f(inputs["router_bias"]).reshape(1, -1),
        "bidx": f((np.arange(((nseq * S * 8) // 128 + NE) // 128)[None, :] * 128 + np.arange(128)[:, None]).astype(np.float32)),
        "w_exp_gate": f(inputs["w_exp_gate"]), "w_exp_up": f(inputs["w_exp_up"]), "w_exp_down": f(inputs["w_exp_down"]),
        "w_sh_gate": f(inputs["w_sh_gate"]), "w_sh_up": f(inputs["w_sh_up"]), "w_sh_down": f(inputs["w_sh_down"]),
    }
    shared.update(cst)
    x = f(inputs["x"])
    c = f(inputs["c"])
    maps = []
    for i in range(ncores):
        m = dict(shared)
        m["x"] = x[i * nseq:(i + 1) * nseq].reshape(nseq * S, D)
        ci = c[i * nseq:(i + 1) * nseq]
        m["cT"] = f(ci.reshape(nseq, 8, 128).transpose(2, 1, 0))
        maps.append(m)
    return maps


def kernel(**inputs):
    ncores, nseq = 8, 4
    nc = build(nseq=nseq)
    maps = _prep_inputs(inputs, ncores, nseq)
    res = run_bass_kernel_spmd(nc, maps, core_ids=list(range(ncores)))
    out = np.concatenate([r["out"] for r in res.results], axis=0)
    return out.reshape(ncores * nseq, S, D).astype(np.float32)
```

```python
import contextlib
import numpy as np
import ml_dtypes
import concourse.bass as bass
import concourse.mybir as mybir
from concourse.bass_utils import run_bass_kernel_spmd

F32 = mybir.dt.float32
BF16 = mybir.dt.bfloat16
I32 = mybir.dt.int32
AF = mybir.ActivationFunctionType
ALU = mybir.AluOpType

_DSIZE = {F32: 4, BF16: 2, I32: 4}
GRAN = 256
SEM_LIM = 30000

D = 1024
S = 2048
NT = S // 128
NE = 256
ENC = 8192
BLK = 256
MAXB = 8192 // BLK
EPS = 1e-6


class Op:
    __slots__ = ("eng", "fn", "waits", "venue", "idx", "clock", "inc")


def _region(ap):
    dsz = _DSIZE[ap.dtype]
    pairs = list(ap.ap)
    pstep = pairs[0][0]
    off = ap.offset
    if pstep > 0:
        off = off % pstep
    lo = off
    hi = off
    for (st, cnt) in pairs[1:]:
        if st >= 0:
            hi += st * (cnt - 1)
        else:
            lo += st * (cnt - 1)
    return (ap.tensor.name, lo * dsz, (hi + 1) * dsz)


class Prog:
    ENGS = ["pe", "act", "dve", "pool", "sp"]

    def __init__(self, nc):
        self.nc = nc
        self.ops = {e: [] for e in self.ENGS}
        self.know = {e: {} for e in self.ENGS}
        self.last_w = {}
        self.readers = {}
        self.dram_w = {}
        self.dram_r = {}
        self.venue_cnt = {}
        self.venue_last = {}
        self.streams = {}

    def _grans(self, ap):
        name, lo, hi = _region(ap)
        if name.startswith("ps"):
            return [(name, 0)]
        return [(name, g) for g in range(lo // GRAN, (hi - 1) // GRAN + 1)]

    def _dep(self, eng, d, waits):
        if d is None:
            return
        k = self.know[eng]
        if k.get(d.venue, 0) >= d.idx:
            return
        if d.venue == eng and eng == "pe":
            return
        waits.append(d)
        d.inc = True
        for v, n in d.clock.items():
            if k.get(v, 0) < n:
                k[v] = n

    def add(self, eng, fn, reads=(), writes=(), dr=(), dw=(), dma=None):
        op = Op()
        op.eng = eng
        op.fn = fn
        op.inc = False
        waits = []
        rg = []
        for ap in reads:
            rg += self._grans(ap)
        wg = []
        for ap in writes:
            wg += self._grans(ap)
        for g in rg:
            self._dep(eng, self.last_w.get(g), waits)
            if g[0].startswith("ps"):
                for r in self.readers.get(g, ()):
                    if r.eng != eng:
                        self._dep(eng, r, waits)
        for g in wg:
            self._dep(eng, self.last_w.get(g), waits)
            for r in self.readers.get(g, ()):
                self._dep(eng, r, waits)
        for key in dr:
            for w in self.dram_w.get(key, {}).values():
                self._dep(eng, w, waits)
        for key in dw:
            for r in self.dram_r.get(key, {}).values():
                self._dep(eng, r, waits)
        if dma is not None:
            st = self.streams[dma]
            assert st["eng"] == eng
            k = st["n"] % st["nsem"]
            st["n"] += 1
            venue = "dma:%s:%d" % (dma, k)
            self._dep(eng, self.venue_last.get(venue), waits)
            op.inc = True
        else:
            venue = eng
        op.venue = venue
        op.idx = self.venue_cnt.get(venue, 0) + 1
        self.venue_cnt[venue] = op.idx
        self.venue_last[venue] = op
        op.waits = waits
        ck = dict(self.know[eng])
        ck[venue] = op.idx
        op.clock = ck
        for g in rg:
            self.readers.setdefault(g, []).append(op)
        for g in wg:
            self.last_w[g] = op
            self.readers[g] = []
        for key in dr:
            self.dram_r.setdefault(key, {})[venue] = op
        for key in dw:
            self.dram_w.setdefault(key, {})[venue] = op
        self.ops[eng].append(op)
        return op

    def stream(self, name, eng, nsem):
        self.streams[name] = {"eng": eng, "nsem": nsem, "n": 0}

    def emit(self):
        nc = self.nc
        semval = {}
        counts = {}
        for eng in self.ENGS:
            for op in self.ops[eng]:
                if op.inc:
                    c = counts.get(op.venue, 0) + 1
                    counts[op.venue] = c
                    semval[id(op)] = c
        step = {}
        nsems = {}
        for v, c in counts.items():
            s = 16 if v.startswith("dma:") else 1
            step[v] = s
            lim = SEM_LIM // s
            nsems[v] = (c - 1) // lim + 1
        self.total_sems = sum(nsems.values())
        with contextlib.ExitStack() as es:
            sems = {}
            for v, n in nsems.items():
                sems[v] = [es.enter_context(nc.semaphore(("s_%s_%d" % (v, i)).replace(":", "_")))
                           for i in range(n)]

            def semof(op):
                c = semval[id(op)]
                s = step[op.venue]
                lim = SEM_LIM // s
                return sems[op.venue][(c - 1) // lim], ((c - 1) % lim + 1) * s

            def run(eng, e):
                for op in self.ops[eng]:
                    for d in op.waits:
                        sm, val = semof(d)
                        e.wait_ge(sm, val)
                    if op.fn is None:
                        continue
                    ins = op.fn(e)
                    if op.inc:
                        sm, val = semof(op)
                        ins.then_inc(sm, step[op.venue])

            with nc.Block() as block:
                @block.tensor
                def _(e):
                    run("pe", e)

                @block.scalar
                def _(e):
                    run("act", e)

                @block.vector
                def _(e):
                    run("dve", e)

                @block.gpsimd
                def _(e):
                    run("pool", e)

                @block.sync
                def _(e):
                    run("sp", e)


DIL = (1, 4, 16)
REACH = (64, 256, 1024)
MC0 = tuple(r + 511 for r in REACH)
MW = tuple(2 * r + 1150 for r in REACH)
MOFF = (0, MW[0], MW[0] + MW[1])
MTOT = sum(MW)


def _host_consts():
    bf = ml_dtypes.bfloat16
    c = {}
    c["identb"] = np.eye(128, dtype=np.float32).astype(bf)
    c["identf"] = np.eye(128, dtype=np.float32)
    r1 = np.zeros((128, 128), np.float32)
    ra = np.zeros((128, 128), np.float32)
    for m in range(128):
        dm = m % 64
        base = m - dm
        if dm < 32:
            r1[base + dm + 32, m] = -1.0
        else:
            r1[base + dm - 32, m] = 1.0
        sub = dm % 32
        blk = dm - sub
        if sub < 16:
            ra[base + blk + sub + 16, m] = -1.0
        else:
            ra[base + blk + sub - 16, m] = 1.0
    c["rm1"] = r1.astype(bf)
    c["rma"] = ra.astype(bf)
    bo = np.zeros((128, 128), np.float32)
    bo[:64, :64] = 1.0
    bo[64:, 64:] = 1.0
    c["bones"] = bo.astype(bf)
    c["utri"] = np.triu(np.ones((128, 128), np.float32), 1).astype(bf)
    c["ones"] = np.ones((128, 128), np.float32).astype(bf)
    t = np.arange(S, dtype=np.float32)
    inv1 = (np.float32(10000.0) ** (-np.arange(0, 64, 2, dtype=np.float32) / np.float32(64))).astype(np.float32)
    inva = (np.float32(10000.0) ** (-np.arange(0, 32, 2, dtype=np.float32) / np.float32(32))).astype(np.float32)
    row = (np.arange(S) // 64).astype(np.float32)
    col = (np.arange(S) % 64).astype(np.float32)
    cos1 = np.zeros((128, S), np.float32)
    sin1 = np.zeros((128, S), np.float32)
    cosa = np.zeros((128, S), np.float32)
    sina = np.zeros((128, S), np.float32)
    for p in range(128):
        dm = p % 64
        ang = (t * inv1[dm % 32]).astype(np.float32)
        cos1[p] = np.cos(ang)
        sin1[p] = np.sin(ang)
        if dm < 32:
            ang = (row * inva[dm % 16]).astype(np.float32)
        else:
            ang = (col * inva[(dm - 32) % 16]).astype(np.float32)
        cosa[p] = np.cos(ang)
        sina[p] = np.sin(ang)
    c["rope"] = np.stack([cos1, sin1, cosa, sina], axis=1).astype(bf)
    strips = []
    for g in range(3):
        i = np.arange(128)[:, None]
        cc = np.arange(MW[g])[None, :]
        dlt = i - cc + MC0[g]
        ok = (np.abs(dlt) <= REACH[g]) & (dlt % DIL[g] == 0)
        strips.append(ok.astype(np.float32))
    c["mstrip"] = np.concatenate(strips, axis=1).astype(bf)
    c["eidx"] = np.tile((np.arange(NE, dtype=np.float32) * ENC + 1.0)[None, :], (128, 1)).astype(np.float32)
    c["eio"] = np.tile(np.arange(NE, dtype=np.float32)[None, :], (128, 1)).astype(np.float32)
    return c


def build(nseq=4, upto="all", dbg=()):
    nc = bass.Bass("TRN2", target_bir_lowering=False)
    NTOK = nseq * S
    NTT = nseq * NT

    def din(name, shape, dt=F32):
        return nc.dram_tensor(name, list(shape), dt, kind="ExternalInput").ap()

    x_d = din("x", [NTOK, D])
    cT_d = din("cT", [128, 8, nseq])
    w_ada = din("w_ada", [D, 6 * D])
    b_ada = din("b_ada", [1, 6 * D])
    n1g_d = din("n1g", [128, 8])
    n2g_d = din("norm2_g", [1, D])
    fg_d = din("final_g", [1, D])
    w_in = din("w_in", [D, 5888])
    bgc_d = din("bgc", [128, 16])
    qng_d = din("qng2", [128, 1])
    kng_d = din("kng2", [128, 1])
    w_o_dil = din("w_o_dil", [256, D])
    w_o_gqa = din("w_o_gqa", [D, D])
    w_out = din("w_out", [D, D])
    router_w = din("router_w", [D, NE])
    rbias_d = din("router_bias", [1, NE])
    w_eg_h = nc.dram_tensor("w_exp_gate", [NE, D, 256], F32, kind="ExternalInput")
    w_eu_h = nc.dram_tensor("w_exp_up", [NE, D, 256], F32, kind="ExternalInput")
    w_ed_h = nc.dram_tensor("w_exp_down", [NE, 256, D], F32, kind="ExternalInput")
    w_sg = din("w_sh_gate", [D, 256])
    w_su = din("w_sh_up", [D, 256])
    w_sd = din("w_sh_down", [256, D])
    identb_d = din("identb", [128, 128], BF16)
    identf_d = din("identf", [128, 128])
    rm1_d = din("rm1", [128, 128], BF16)
    rma_d = din("rma", [128, 128], BF16)
    bones_d = din("bones", [128, 128], BF16)
    utri_d = din("utri", [128, 128], BF16)
    ones_d = din("ones", [128, 128], BF16)
    rope_d = din("rope", [128, 4, S], BF16)
    mstrip_d = din("mstrip", [128, MTOT], BF16)
    eidx_d = din("eidx", [128, NE])
    out_d = nc.dram_tensor("out", [NTOK, D], F32, kind="ExternalOutput").ap()
    modd = nc.dram_tensor("modd", [nseq, 6 * D], F32, kind="Internal").ap()
    NB = -(-(NTOK * 8 // BLK + NE) // 128) * 128
    NBJ = NB // 128
    XS = nc.dram_tensor("xs", [NB * BLK, D], BF16, kind="Internal").ap()
    H2S = nc.dram_tensor("h2s", [NTOK, D], BF16, kind="Internal").ap()
    eio_d = din("eio", [128, NE])
    bidx_d = din("bidx", [128, NBJ])
    PART = nc.dram_tensor("part", [NTOK, D], F32, kind="Internal").ap()

    P = Prog(nc)
    P.stream("sp_c", "sp", 4)
    P.stream("sp_x", "sp", 2)
    P.stream("sp_w", "sp", 4)
    P.stream("sp_st", "sp", 4)
    P.stream("sp_z", "sp", 4)
    P.stream("pw", "pool", 6)
    P.stream("pwg", "pool", 12)
    P.stream("psc", "pool", 8)
    P.stream("pg", "pool", 8)
    dbg_outs = {}

    with contextlib.ExitStack() as es:
        ARENA_EL = 103 * 1024 + 512
        A = es.enter_context(nc.sbuf_tensor("arena", [128, ARENA_EL], BF16))
        PS = [es.enter_context(nc.psum_tensor("ps%d" % i, [128, 512], F32)) for i in range(8)]
        cur = [0]

        def alloc(nbytes):
            o = cur[0]
            cur[0] = (o + nbytes + 63) // 64 * 64
            assert cur[0] <= ARENA_EL * 2, ("SBUF overflow", cur[0])
            return o

        def view(off, shape, dt=BF16):
            n = 1
            for s_ in shape:
                n *= s_
            nb = n * _DSIZE[dt]
            assert off % 4 == 0
            ap = A[:, off // 2: off // 2 + nb // 2]
            if dt != BF16:
                ap = ap.bitcast(dt)
            if len(shape) == 2:
                ap = ap.rearrange("p (a b) -> p a b", a=shape[0])
            elif len(shape) == 3:
                ap = ap.rearrange("p (a b c) -> p a b c", a=shape[0], b=shape[1])
            return ap

        def new(shape, dt=BF16):
            n = 1
            for s_ in shape:
                n *= s_
            return view(alloc(n * _DSIZE[dt]), shape, dt)

        def aps(*xs):
            return [a for a in xs if a is not None and not isinstance(a, (int, float))]

        def MM(out, lhsT, rhs, start, stop):
            P.add("pe", lambda e: e.matmul(out, lhsT=lhsT, rhs=rhs, start=start, stop=stop),
                  reads=[lhsT, rhs], writes=[out])

        def TR(out, in_, ident):
            P.add("pe", lambda e: e.transpose(out=out, in_=in_, identity=ident), reads=[in_, ident], writes=[out])

        def ACT(out, in_, func, bias=None, scale=None, accum=None):
            kw = {}
            if bias is not None:
                kw["bias"] = bias
            if scale is not None:
                kw["scale"] = scale
            if accum is not None:
                kw["accum_out"] = accum
            P.add("act", lambda e: e.activation(out=out, in_=in_, func=func, **kw),
                  reads=aps(in_, bias, scale), writes=aps(out, accum))

        def TT(eng, out, in0, in1, op):
            P.add(eng, lambda e: e.tensor_tensor(out=out, in0=in0, in1=in1, op=op), reads=[in0, in1], writes=[out])

        def TS(eng, out, in0, s1, s2, op0, op1=None, accum=None):
            if accum is not None:
                P.add(eng, lambda e: e.tensor_scalar(out=out, in0=in0, scalar1=s1, scalar2=s2, op0=op0, op1=op1, accum_out=accum),
                      reads=aps(in0, s1, s2), writes=[out, accum])
            elif op1 is None:
                P.add(eng, lambda e: e.tensor_scalar(out=out, in0=in0, scalar1=s1, scalar2=None, op0=op0),
                      reads=aps(in0, s1), writes=[out])
            else:
                P.add(eng, lambda e: e.tensor_scalar(out=out, in0=in0, scalar1=s1, scalar2=s2, op0=op0, op1=op1),
                      reads=aps(in0, s1, s2), writes=[out])

        def STT(out, in0, scalar, in1, op0, op1, accum=None):
            kw = {}
            if accum is not None:
                kw["accum_out"] = accum
            P.add("dve", lambda e: e.scalar_tensor_tensor(out=out, in0=in0, scalar=scalar, in1=in1, op0=op0, op1=op1, **kw),
                  reads=aps(in0, scalar, in1), writes=aps(out, accum))

        def CP(eng, out, in_):
            if eng == "act":
                P.add("act", lambda e: e.copy(out=out, in_=in_), reads=[in_], writes=[out])
            else:
                P.add(eng, lambda e: e.tensor_copy(out=out, in_=in_), reads=[in_], writes=[out])

        def MAX8(out, in_):
            P.add("dve", lambda e: e.max(out=out, in_=in_), reads=[in_], writes=[out])

        def RECIP(out, in_):
            P.add("dve", lambda e: e.reciprocal(out=out, in_=in_), reads=[in_], writes=[out])

        def MEMSET(eng, ap, val):
            P.add(eng, lambda e: e.memset(ap, val), writes=[ap])

        def LD(stream, out, in_, dr=(), **kw):
            eng = P.streams[stream]["eng"]
            P.add(eng, lambda e: e.dma_start(out=out, in_=in_, **kw), writes=[out], dr=dr, dma=stream)

        def ST(stream, out, in_, dw=(), **kw):
            eng = P.streams[stream]["eng"]
            P.add(eng, lambda e: e.dma_start(out=out, in_=in_, **kw), reads=[in_], dw=dw, dma=stream)

        def DUMP(name, ap, dt=None):
            if name not in dbg:
                return
            shp = list(ap.shape)
            d_ = nc.dram_tensor("dbg_" + name, shp, ap.dtype, kind="ExternalOutput").ap()
            dbg_outs[name] = d_
            ST("sp_st", d_, ap, dw=["dbg"])

        def rstd_from_ssq(out, ssq, n, tmp):
            ACT(tmp, ssq, AF.Ln, bias=epsc[:, 0:1], scale=1.0 / n)
            ACT(out, tmp, AF.Exp, scale=-0.5)

        identb = new([128]); rm1 = new([128]); rma = new([128]); bones = new([128])
        utri = new([128]); ones = new([128])
        identf = new([128], F32); onesf = new([128], F32)
        rope = new([4, S])
        mstrip = new([MTOT])
        n1g = new([8], F32); bgc = new([16], F32); qng = new([1], F32); kng = new([1], F32)
        epsc = new([1], F32)
        eidx = new([NE], F32); rbias = new([NE], F32); eio = new([NE], F32)
        gts = new([NTT, 8], F32); cnt = new([NE], F32)
        ef8 = new([NTT, 8], F32); pf8 = new([NTT, 8], F32)
        gs1c = new([8], F32); sh1c = new([8], F32); sc1c = new([8], F32)
        small = new([64], F32)

        o_hT = alloc(8 * S * 2)
        o_QK = alloc(12 * S * 2)
        o_U1 = alloc(16384 * 2)
        o_vb = alloc(NT * 4 * 65 * 2)
        o_oA = alloc(2 * S * 2)
        hT = view(o_hT, [8, S])
        QK = view(o_QK, [12, S])
        va = view(o_U1, [NT, 12, 65])
        oB = view(o_U1, [8, S])
        vb = view(o_vb, [NT, 4, 65])
        oA = view(o_oA, [2, S])

        xt = new([D], F32)
        xn = new([D])
        wch = [new([8, 256]) for _ in range(4)]
        PT = [new([512]) for _ in range(3)]
        tb1 = [new([512]) for _ in range(2)]
        tsq = new([512])
        o_tf = cur[0]
        tf = [new([512], F32) for _ in range(3)]
        junk = view(o_tf, [D], F32)
        zt = new([D])
        SB_END = cur[0]

        psT3 = PS[7][:, :].bitcast(BF16).rearrange("p (a b) -> p a b", a=8)
        psT3b = PS[6][:, :].bitcast(BF16).rearrange("p (a b) -> p a b", a=8)

        def stat(i):
            return small[:, (i % 4) * 16:(i % 4) * 16 + 16]

        LD("sp_c", identb, identb_d); LD("sp_c", rm1, rm1_d); LD("sp_c", rma, rma_d)
        LD("sp_c", bones, bones_d); LD("sp_c", utri, utri_d); LD("sp_c", ones, ones_d)
        LD("sp_c", identf, identf_d); LD("sp_c", rope, rope_d); LD("sp_c", mstrip, mstrip_d)
        LD("sp_c", n1g, n1g_d); LD("sp_c", bgc, bgc_d); LD("sp_c", qng, qng_d); LD("sp_c", kng, kng_d)
        LD("sp_c", eidx, eidx_d)
        LD("sp_c", eio, eio_d)
        LD("sp_c", rbias, rbias_d.partition_broadcast(128))
        MEMSET("dve", epsc, EPS)
        MEMSET("dve", cnt, 0.0)
        MEMSET("dve", onesf, 1.0)
        MEMSET("pool", zt, 0.0)
        zfill = [0]
        NZ = NB * BLK // 128
        MEMSET("pool", vb[:, :, :, 64:65], 1.0)

        sct = view(o_QK, [8, nseq], F32)
        LD("sp_c", sct, cT_d)
        ACT(sct, sct, AF.Silu)
        wst = [view(o_hT, [8, 512], F32), view(o_hT + 16384, [8, 512], F32)]
        mrow = [view(o_U1, [512], F32), view(o_U1 + 2048, [512], F32)]
        brow = view(o_U1 + 4096, [6 * D], F32)
        LD("sp_c", brow[0:nseq, :], b_ada.partition_broadcast(nseq))
        for blk in range(12):
            wb_ = wst[blk % 2]
            LD("sp_w", wb_, w_ada[:, blk * 512:(blk + 1) * 512].rearrange("(kc p) n -> p kc n", p=128))
            pm = PS[blk % 2]
            for kc in range(8):
                MM(pm[0:nseq, :], sct[:, kc, :], wb_[:, kc, :], kc == 0, kc == 7)
            mr = mrow[blk % 2]
            TT("dve", mr[0:nseq, :], pm[0:nseq, :], brow[0:nseq, blk * 512:(blk + 1) * 512], ALU.add)
            ST("sp_st", modd[:, blk * 512:(blk + 1) * 512], mr[0:nseq, :], dw=["modd"])

        def load_wblock(buf, col0, ncols, dst0=0, src=None, kcn=8):
            src = w_in if src is None else src
            LD("pw", buf[:, 0:kcn, dst0:dst0 + ncols],
               src[:, col0:col0 + ncols].rearrange("(kc p) n -> p kc n", p=128))

        pacc_i = [0]

        def proj_fm(wbuf, c0, tb):
            pm = PS[pacc_i[0] % 2]
            pacc_i[0] += 1
            for kc in range(8):
                MM(pm[:, :], wbuf[:, kc, c0:c0 + 128], hT[:, kc, tb * 512:(tb + 1) * 512], kc == 0, kc == 7)
            return pm

        def rope_plain(pm, dst, tb, ti):
            tok = slice(tb * 512, (tb + 1) * 512)
            qg = tb1[ti % 2]
            CP("act", qg, pm[:, :])
            pr = PS[2 + ti % 2]
            MM(pr[:, :], rm1, qg, True, True)
            TT("dve", tf[0], pm[:, :], rope[:, 0, tok], ALU.mult)
            TT("dve", tf[1], pr[:, :], rope[:, 1, tok], ALU.mult)
            TT("dve", dst, tf[0], tf[1], ALU.add)

        def rope_norm(pm, dst, tb, ti, gcol):
            tok = slice(tb * 512, (tb + 1) * 512)
            ACT(tsq, pm[:, :], AF.Square)
            qg = tb1[ti % 2]
            ACT(qg, pm[:, :], AF.Copy, scale=gcol[:, 0:1])
            pq = PS[2]
            pr = PS[3]
            MM(pq[:, :], bones, tsq, True, True)
            MM(pr[:, :], rma, qg, True, True)
            ACT(tf[2], pq[:, :], AF.Ln, bias=epsc[:, 0:1], scale=1.0 / 64.0)
            ACT(tf[2], tf[2], AF.Exp, scale=-0.5)
            TT("dve", tf[0], qg, rope[:, 2, tok], ALU.mult)
            TT("dve", tf[1], pr[:, :], rope[:, 3, tok], ALU.mult)
            TT("dve", tf[0], tf[0], tf[1], ALU.add)
            TT("dve", dst, tf[0], tf[2], ALU.mult)

        pend_norm = []

        def flush_norm():
            while pend_norm:
                po, dst = pend_norm.pop(0)
                rr = tf[2]
                RECIP(rr[64:65, :], po[64:65, :])
                pb_ = PS[6]
                MM(pb_[0:64, :], onesf[64:65, 0:64], rr[64:65, :], True, True)
                CP("act", tf[0][0:64, :], pb_[0:64, :])
                TT("dve", dst, po[0:64, :], tf[0][0:64, :], ALU.mult)

        def attention(branches, dst_chunk, dst_half):
            for qc in range(4):
                po = PS[4 + (qc % 2)]
                blocks = []
                for (q_fn, k_fn, v_fn, g) in branches:
                    for kb in range(NT):
                        if g is not None:
                            delta = kb * 128 - qc * 512
                            if delta + 127 < -REACH[g] or delta - 511 > REACH[g]:
                                continue
                        blocks.append((q_fn, k_fn, v_fn, g, kb))
                nb = len(blocks)

                def qk(i):
                    q_fn, k_fn, v_fn, g, kb = blocks[i]
                    psc = PS[i % 3]
                    MM(psc[:, :], k_fn(kb), q_fn(qc), True, True)
                    pt = PT[i % 3]
                    ACT(pt, psc[:, :], AF.Exp, scale=0.125)
                    if g is not None:
                        c0 = MOFF[g] + MC0[g] - (kb * 128 - qc * 512)
                        TT("dve", pt, pt, mstrip[:, c0:c0 + 512], ALU.mult)

                def pv(i):
                    q_fn, k_fn, v_fn, g, kb = blocks[i]
                    MM(po[0:65, :], v_fn(kb), PT[i % 3], i == 0, i == nb - 1)

                LA = 2
                for i in range(min(LA, nb)):
                    qk(i)
                flush_norm()
                for i in range(nb):
                    if i + LA < nb:
                        qk(i + LA)
                    pv(i)
                pend_norm.append((po, dst_chunk[dst_half * 64:(dst_half + 1) * 64, qc * 512:(qc + 1) * 512]))

        g1bc = view(o_hT, [D], F32); gs2bc = view(o_hT + 4096, [D], F32); sh2bc = view(o_hT + 8192, [D], F32)
        g2bc_t = view(o_hT + 12288, [D], F32); n2gbc = view(o_hT + 16384, [D], F32)
        x1 = view(o_hT + 20480, [D], F32); h2 = view(o_hT + 24576, [D], F32)
        h2b = view(o_hT + 28672, [D]); h2Tb = view(o_hT + 30720, [8, 128])
        wshgu = view(o_U1, [8, 512]); wshd = view(o_U1 + 8192, [2, 1024])
        routw = view(o_U1 + 12288, [8, NE], F32); h2T = view(o_U1 + 20480, [8, 128], F32)
        r_sc = view(o_U1 + 24576, [NE], F32); r_bi = view(o_U1 + 25600, [NE], F32)
        r_ma = view(o_U1 + 26624, [NE], F32); r_se = view(o_U1 + 27648, [NE], F32)
        r_ga = view(o_U1 + 28672, [NE], F32); r_D = view(o_U1 + 29696, [NE], F32)
        r_selb = view(o_U1 + 30720, [NE]); r_m8 = view(o_U1 + 31232, [8, 8], F32)
        r_gs = view(o_U1 + 31488, [8], F32); r_gs8 = view(o_U1 + 31520, [8], F32); r_gm = view(o_U1 + 31552, [8], F32)
        r_pen = view(o_U1 + 31584, [8], F32); r_t8 = view(o_U1 + 31616, [8], F32); r_s8 = view(o_U1 + 31648, [8], F32)
        r_HT = view(o_U1 + 31744, [2, 128])
        wout = view(o_QK + 8 * S * 2, [8, D])
        sgs = tf[0]

        def do_seq(s):
            tok0 = s * S
            LD("sp_c", sh1c, modd[s:s + 1, 0:D].rearrange("o (j p) -> p (o j)", p=128), dr=["modd"],
               allow_slow_non_contiguous=True)
            LD("sp_c", sc1c, modd[s:s + 1, D:2 * D].rearrange("o (j p) -> p (o j)", p=128), dr=["modd"],
               allow_slow_non_contiguous=True)
            STT(gs1c, sc1c, 1.0, n1g, ALU.add, ALU.mult)
            MEMSET("pool", va[:, :, :, 64:65], 1.0)
            for t in range(NT):
                st_ = stat(t)
                LD("sp_x", xt, x_d[tok0 + t * 128: tok0 + (t + 1) * 128, :])
                for _z in range(-(-NZ // NTT)):
                    if zfill[0] < NZ:
                        z0 = zfill[0] * 128
                        ST("sp_z", XS[z0:z0 + 128, :], zt, dw=["xsz"])
                        zfill[0] += 1
                STT(junk, xt, 1.0, xt, ALU.mult, ALU.mult, accum=st_[:, 0:1])
                ACT(st_[:, 1:2], st_[:, 0:1], AF.Ln, bias=epsc[:, 0:1], scale=1.0 / D)
                ACT(st_[:, 2:3], st_[:, 1:2], AF.Exp, scale=-0.5)
                ACT(xn, xt, AF.Copy, scale=st_[:, 2:3])
                for c in range(8):
                    TR(psT3[:, c, :], xn[:, c * 128:(c + 1) * 128], identb)
                for c in range(8):
                    TS("dve", hT[:, c, t * 128:(t + 1) * 128], psT3[:, c, :],
                       gs1c[:, c:c + 1], sh1c[:, c:c + 1], ALU.mult, ALU.add)
            DUMP("hT", hT)
            if upto == "A1":
                return
            ti = 0
            for bi in range(6):
                wb_ = wch[bi % 4]
                load_wblock(wb_, bi * 256, 256)
                for c2 in range(2):
                    ch = bi * 2 + c2
                    for tb in range(4):
                        pm = proj_fm(wb_, c2 * 128, tb)
                        rope_plain(pm, QK[:, ch, tb * 512:(tb + 1) * 512], tb, ti)
                        ti += 1
            DUMP("qaka", QK)
            if upto == "A2q":
                return
            for vbi in range(4):
                wb_ = wch[(2 + vbi) % 4]
                load_wblock(wb_, (1536 + vbi * 256) if vbi < 3 else 3584, 256)
                for t in range(NT):
                    pm = PS[pacc_i[0] % 2]
                    pacc_i[0] += 1
                    for kc in range(8):
                        MM(pm[:, 0:256], hT[:, kc, t * 128:(t + 1) * 128], wb_[:, kc, :], kc == 0, kc == 7)
                    src = pm[:, 0:256].rearrange("p (h d) -> p h d", h=4)
                    if vbi < 3:
                        CP("act" if t % 2 == 0 else "dve", va[:, t, vbi * 4:(vbi + 1) * 4, 0:64], src)
                    else:
                        CP("act" if t % 2 == 0 else "dve", vb[:, t, :, 0:64], src)
            DUMP("va", va); DUMP("vb", vb)
            if upto == "A2":
                return
            for h in range(4):
                br = []
                for g in range(3):
                    hh = g * 4 + h
                    chk, hf = hh // 2, hh % 2
                    br.append((
                        (lambda qc, chk=chk, hf=hf: QK[hf * 64:(hf + 1) * 64, chk, qc * 512:(qc + 1) * 512]),
                        (lambda kb, chk=chk, hf=hf: QK[hf * 64:(hf + 1) * 64, 6 + chk, kb * 128:(kb + 1) * 128]),
                        (lambda kb, hh=hh: va[:, kb, hh, :]),
                        g))
                attention(br, oA[:, h // 2, :], h % 2)
            flush_norm()
            DUMP("oA", oA)
            if upto == "dil":
                return
            ti = 0
            for bi in range(4):
                wb_ = wch[bi % 4]
                load_wblock(wb_, 2304 + bi * 256, 256)
                for c2 in range(2):
                    ch = bi * 2 + c2
                    for tb in range(4):
                        pm = proj_fm(wb_, c2 * 128, tb)
                        rope_norm(pm, QK[:, ch, tb * 512:(tb + 1) * 512], tb, ti, qng)
                        ti += 1
            for kv in range(4):
                wb_ = wch[kv % 4]
                load_wblock(wb_, 3328 + kv * 64, 64, dst0=0)
                load_wblock(wb_, 3328 + kv * 64, 64, dst0=64)
                for tb in range(4):
                    pm = proj_fm(wb_, 0, tb)
                    rope_norm(pm, QK[:, 8 + kv, tb * 512:(tb + 1) * 512], tb, ti, kng)
                    ti += 1
            DUMP("qbkb", QK)
            for hq in range(16):
                kv, chk, hf = hq // 4, hq // 2, hq % 2
                br = [(
                    (lambda qc, chk=chk, hf=hf: QK[hf * 64:(hf + 1) * 64, chk, qc * 512:(qc + 1) * 512]),
                    (lambda kb, kv=kv, hf=hf: QK[hf * 64:(hf + 1) * 64, 8 + kv, kb * 128:(kb + 1) * 128]),
                    (lambda kb, kv=kv: vb[:, kb, kv, :]),
                    None)]
                attention(br, oB[:, chk, :], hf)
            flush_norm()
            DUMP("oB", oB)
            if upto == "gqa":
                return
            for m in range(8):
                wg_ = wch[(m % 2) * 2]
                wo_ = wch[(m % 2) * 2 + 1]
                load_wblock(wg_, 3840 + m * 128, 128, dst0=0)
                load_wblock(wg_, 4864 + m * 128, 128, dst0=128)
                load_wblock(wo_, m * 128, 128, dst0=0, src=w_o_gqa)
                load_wblock(wo_, m * 128, 128, dst0=128, src=w_o_dil, kcn=2)
                for tb in range(4):
                    tok = slice(tb * 512, (tb + 1) * 512)
                    pa, pg, pb_, pg2 = PS[0], PS[1], PS[2], PS[3]
                    for c in range(2):
                        MM(pa[:, :], wo_[:, c, 128:256], oA[:, c, tok], c == 0, c == 1)
                    for kc in range(8):
                        MM(pg[:, :], wg_[:, kc, 0:128], hT[:, kc, tok], kc == 0, kc == 7)
                    ACT(tb1[0], pg[:, :], AF.Sigmoid, bias=bgc[:, m:m + 1])
                    TT("dve", tf[0], tb1[0], pa[:, :], ALU.mult)
                    for kc in range(8):
                        MM(pb_[:, :], wo_[:, kc, 0:128], oB[:, kc, tok], kc == 0, kc == 7)
                    for kc in range(8):
                        MM(pg2[:, :], wg_[:, kc, 128:256], hT[:, kc, tok], kc == 0, kc == 7)
                    ACT(tb1[1], pg2[:, :], AF.Sigmoid, bias=bgc[:, 8 + m:9 + m])
                    TT("dve", tf[1], tb1[1], pb_[:, :], ALU.mult)
                    TT("dve", QK[:, m, tok], tf[0], tf[1], ALU.add)
            DUMP("zT", QK)
            if upto == "A4":
                return
            LD("pw", wout, w_out.rearrange("(kc p) n -> p kc n", p=128))
            LD("pw", wshgu[:, :, 0:256], w_sg.rearrange("(kc p) n -> p kc n", p=128))
            LD("pw", wshgu[:, :, 256:512], w_su.rearrange("(kc p) n -> p kc n", p=128))
            LD("pw", wshd, w_sd.rearrange("(j p) n -> p j n", p=128))
            LD("sp_w", routw, router_w.rearrange("(kc p) n -> p kc n", p=128))
            LD("sp_c", g1bc, modd[s:s + 1, 2 * D:3 * D].partition_broadcast(128), dr=["modd"])
            LD("sp_c", sh2bc, modd[s:s + 1, 3 * D:4 * D].partition_broadcast(128), dr=["modd"])
            LD("sp_c", gs2bc, modd[s:s + 1, 4 * D:5 * D].partition_broadcast(128), dr=["modd"])
            LD("sp_c", g2bc_t, modd[s:s + 1, 5 * D:6 * D].partition_broadcast(128), dr=["modd"])
            LD("sp_c", n2gbc, n2g_d.partition_broadcast(128))
            STT(gs2bc, gs2bc, 1.0, n2gbc, ALU.add, ALU.mult)
            for t in range(NT):
                T = s * NT + t
                st_ = stat(t)
                for n in range(2):
                    for kc in range(8):
                        MM(PS[n][:, :], QK[:, kc, t * 128:(t + 1) * 128], wout[:, kc, n * 512:(n + 1) * 512], kc == 0, kc == 7)
                LD("sp_x", xt, x_d[tok0 + t * 128: tok0 + (t + 1) * 128, :])
                for n in range(2):
                    TT("dve", x1[:, n * 512:(n + 1) * 512], PS[n][:, :], g1bc[:, n * 512:(n + 1) * 512], ALU.mult)
                TT("pool", x1, x1, xt, ALU.add)
                STT(junk, x1, 1.0, x1, ALU.mult, ALU.mult, accum=st_[:, 0:1])
                ACT(st_[:, 1:2], st_[:, 0:1], AF.Ln, bias=epsc[:, 0:1], scale=1.0 / D)
                ACT(st_[:, 2:3], st_[:, 1:2], AF.Exp, scale=-0.5)
                STT(h2, x1, st_[:, 2:3], gs2bc, ALU.mult, ALU.mult)
                TT("pool", h2, h2, sh2bc, ALU.add)
                CP("act", h2b, h2)
                for c in range(8):
                    pX = PS[2 + c // 4][:, :].rearrange("p (a b) -> p a b", a=4)
                    TR(pX[:, c % 4, :], h2[:, c * 128:(c + 1) * 128], identf)
                for hf in range(2):
                    pX = PS[2 + hf][:, :].rearrange("p (a b) -> p a b", a=4)
                    CP("act", h2T[:, hf * 4:(hf + 1) * 4, :], pX)
                    CP("dve", h2Tb[:, hf * 4:(hf + 1) * 4, :], pX)
                pr = PS[4]
                for kc in range(8):
                    MM(pr[:, 0:NE], h2T[:, kc, :], routw[:, kc, :], kc == 0, kc == 7)
                ACT(r_sc, pr[:, 0:NE], AF.Sigmoid)
                TT("dve", r_bi, r_sc, rbias, ALU.add)
                for g in range(8):
                    MAX8(r_m8[:, g, :], r_bi[:, g * 32:(g + 1) * 32])
                TT("dve", r_gs, r_m8[:, :, 0], r_m8[:, :, 1], ALU.add)
                MAX8(r_gs8, r_gs)
                TS("dve", r_gm, r_gs, r_gs8[:, 3:4], None, ALU.is_ge)
                TS("dve", r_pen, r_gm, -1.0, 10.0, ALU.add, ALU.mult)
                for g in range(8):
                    TS("dve", r_ma[:, g * 32:(g + 1) * 32], r_bi[:, g * 32:(g + 1) * 32],
                       r_gm[:, g:g + 1], r_pen[:, g:g + 1], ALU.mult, ALU.add)
                MAX8(r_t8, r_ma)
                TS("dve", r_se, r_ma, r_t8[:, 7:8], None, ALU.is_ge)
                STT(r_ga, r_sc, 1.0, r_se, ALU.mult, ALU.mult, accum=st_[:, 4:5])
                RECIP(st_[:, 5:6], st_[:, 4:5])
                TS("dve", r_ga, r_ga, st_[:, 5:6], 2.5, ALU.mult, ALU.mult)
                CP("act", r_selb, r_se)
                pc = PS[5]
                MM(pc[:, 0:NE], utri, r_selb, True, True)
                MM(pc[:, NE:2 * NE], ones, r_selb, True, True)
                TT("dve", r_D, pc[:, 0:NE], cnt, ALU.add)
                TT("dve", r_D, r_D, eidx, ALU.add)
                TT("dve", r_D, r_D, r_se, ALU.mult)
                TT("dve", cnt, cnt, pc[:, NE:2 * NE], ALU.add)
                MAX8(r_s8, r_D)
                STT(r_ma, eio, 1.0, r_se, ALU.add, ALU.mult)
                MAX8(r_t8, r_ma)
                TS("dve", ef8[:, T, :], r_t8, -1.0, None, ALU.add)
                STT(pf8[:, T, :], ef8[:, T, :], -float(ENC), r_s8, ALU.mult, ALU.add)
                TS("dve", pf8[:, T, :], pf8[:, T, :], -1.0, None, ALU.add)
                for k in range(8):
                    STT(junk[:, 0:NE], r_D, r_s8[:, k:k + 1], r_ga, ALU.is_equal, ALU.mult, accum=gts[:, T, k:k + 1])
                ST("sp_st", H2S[T * 128:(T + 1) * 128, :], h2b, dw=["h2s"])
                pgu = PS[6]
                for j in range(4):
                    for kc in range(8):
                        MM(pgu[:, j * 128:(j + 1) * 128], wshgu[:, kc, j * 128:(j + 1) * 128], h2Tb[:, kc, :], kc == 0, kc == 7)
                ACT(sgs[:, 0:256], pgu[:, 0:256], AF.Silu)
                TT("dve", r_HT.rearrange("p a b -> p (a b)"), sgs[:, 0:256], pgu[:, 256:512], ALU.mult)
                for n in range(2):
                    for j in range(2):
                        MM(PS[n][:, :], r_HT[:, j, :], wshd[:, j, n * 512:(n + 1) * 512], j == 0, j == 1)
                for n in range(2):
                    TT("dve", h2[:, n * 512:(n + 1) * 512], PS[n][:, :], g2bc_t[:, n * 512:(n + 1) * 512], ALU.mult)
                TT("pool", h2, h2, x1, ALU.add)
                ST("sp_st", PART[T * 128:(T + 1) * 128, :], h2, dw=["part"])
                if t == 0:
                    DUMP("x1", x1); DUMP("rsc", r_sc); DUMP("rse", r_se); DUMP("rga", r_ga); DUMP("rD", r_D)

        for s in range(nseq):
            do_seq(s)
        DUMP("gts", gts)

        if upto in ("all", "C0", "C1"):
            oc = [o_hT]

            def cnew(shape, dt=BF16):
                n = 1
                for s_ in shape:
                    n *= s_
                o = oc[0]
                oc[0] = (o + n * _DSIZE[dt] + 63) // 64 * 64
                assert oc[0] <= SB_END
                return view(o, shape, dt)
            slots = cnew([NTT, 8], I32)
            o_after_slots = oc[0]
            nblk = cnew([NE], F32); pendb = cnew([NE], F32); onesr = cnew([NE], F32); bidx = cnew([NBJ], F32)
            pstart = cnew([NE], F32)
            t_e = cnew([NBJ], F32)
            ps8 = cnew([8], F32)
            h2r = [cnew([D]) for _ in range(2)]
            h2p = [cnew([D]) for _ in range(2)]
            LD("sp_c", bidx, bidx_d)
            MEMSET("dve", onesr, 1.0)
            TS("dve", nblk, cnt, 0.0, None, ALU.is_gt)
            for j in range(1, MAXB):
                STT(nblk, cnt, float(BLK * j), nblk, ALU.is_gt, ALU.add)
            P.add("dve", lambda e: e.tensor_tensor_scan(out=pendb, data0=onesr, data1=nblk, initial=0.0,
                                                          op0=ALU.mult, op1=ALU.add),
                  reads=[onesr, nblk], writes=[pendb])
            TT("dve", pstart, pendb, nblk, ALU.subtract)
            TS("dve", pstart, pstart, float(BLK), None, ALU.mult)
            for j in range(NBJ):
                TS("dve", junk[:, 0:NE], pendb, bidx[:, j:j + 1], 0.0, ALU.is_le, ALU.add, accum=t_e[:, j:j + 1])
            idxw = cnew([NB], I32)
            idxd = [cnew([NB], I32) for _ in range(2)]
            pcol = cnew([2], F32)
            dg = cnew([128], F32)
            for j in range(2):
                TS("dve", pcol[:, j:j + 1], bidx[:, 0:1], float(j * 128), None, ALU.add)
            for j in range(NBJ):
                TS("dve", dg, identf, t_e[:, j:j + 1], None, ALU.mult)
                MM(PS[j % 2][:, 0:128], onesf, dg, True, True)
                TS("dve", idxw[:, j * 128:(j + 1) * 128], PS[j % 2][:, 0:128], 128.0, bidx[:, 0:1], ALU.mult, ALU.add)
                for j2 in range(2):
                    TS("dve", idxd[j2][:, j * 128:(j + 1) * 128], PS[j % 2][:, 0:128], 256.0, pcol[:, j2:j2 + 1], ALU.mult, ALU.add)
            DUMP("t_e", t_e); DUMP("cnt", cnt); DUMP("pstart", pstart)
            for T in range(NTT):
                b_ = T % 2
                LD("sp_x", h2r[b_], H2S[T * 128:(T + 1) * 128, :], dr=["h2s"])
                for k in range(8):
                    STT(junk[:, 0:NE], eio, ef8[:, T, k:k + 1], pstart, ALU.is_equal, ALU.mult, accum=ps8[:, k:k + 1])
                TT("dve", ps8, ps8, pf8[:, T, :], ALU.add)
                CP("dve", slots[:, T, :], ps8)
                CP("act", h2p[b_].rearrange("t (k p) -> t k p", k=8), h2r[b_].rearrange("t (p k) -> t k p", k=8))
                for k in range(8):
                    P.add("pool", (lambda e, T=T, k=k, b_=b_: e.indirect_dma_start(
                        out=XS, out_offset=bass.IndirectOffsetOnAxis(ap=slots[:, T, k:k + 1], axis=0),
                        in_=h2p[b_], in_offset=None)),
                        reads=[slots[:, T, k:k + 1], h2p[b_]], dr=["xsz"], dw=["xs"], dma="psc")
            DUMP("slots", slots)

            NWB = 2
            NW32 = 3
            wg32 = [cnew([8, 256], F32) for _ in range(NW32)]
            wu32 = [cnew([8, 256], F32) for _ in range(NW32)]
            wd32 = [cnew([2, D], F32) for _ in range(NW32)]
            wg = [cnew([8, 256]) for _ in range(NWB)]
            wu = [cnew([8, 256]) for _ in range(NWB)]
            wd = [cnew([2, D]) for _ in range(NWB)]
            xr = [cnew([2, D]) for _ in range(2)]
            XT = [cnew([8, BLK]) for _ in range(2)]
            HT = [cnew([2, BLK]) for _ in range(2)]
            sgt = [cnew([2 * BLK], F32) for _ in range(2)]
            Yb = [cnew([D]) for _ in range(2)]
            wg_v = w_eg_h.ap().rearrange("e (p k) n -> (e p) (k n)", k=8)
            wu_v = w_eu_h.ap().rearrange("e (p k) n -> (e p) (k n)", k=8)
            wd_v = w_ed_h.ap().rearrange("e r n -> (e r) n")

            bc_regs = {}

            def gather_w(b, dst, srcv, idx):
                def fn(e):
                    lim = int(srcv.shape[0]) - 1
                    if lim not in bc_regs:
                        bc_regs[lim] = e.to_reg(lim)
                    return e.indirect_dma_start(
                        out=dst, out_offset=None, in_=srcv,
                        in_offset=bass.IndirectOffsetOnAxis(ap=idx[:, b:b + 1], axis=0),
                        bounds_check=bc_regs[lim], oob_is_err=False)
                P.add("pool", fn,
                    reads=[idx[:, b:b + 1]], writes=[dst], dma="pwg")

            NBR = NB if upto == "all" else (4 if upto == "C1" else 0)

            def stage_a(b):
                wb_ = b % NWB
                b_ = b % 2
                w3_ = b % NW32
                gather_w(b, wg32[w3_].rearrange("p k n -> p (k n)"), wg_v, idxw)
                gather_w(b, wu32[w3_].rearrange("p k n -> p (k n)"), wu_v, idxw)
                for j2 in range(2):
                    gather_w(b, wd32[w3_][:, j2, :], wd_v, idxd[j2])
                CP("act", wg[wb_], wg32[w3_])
                CP("dve", wu[wb_], wu32[w3_])
                CP("act", wd[wb_][:, 0, :], wd32[w3_][:, 0, :])
                CP("dve", wd[wb_][:, 1, :], wd32[w3_][:, 1, :])
                LD("sp_w", xr[b_], XS[b * BLK:(b + 1) * BLK, :].rearrange("(r p) n -> p r n", p=128), dr=["xs"])
                for r in range(2):
                    pT = psT3 if r == 0 else psT3b
                    for c in range(8):
                        TR(pT[:, c, :], xr[b_][:, r, c * 128:(c + 1) * 128], identb)
                    CP("act" if r == 0 else "dve", XT[b_][:, :, r * 128:(r + 1) * 128], pT)

            def stage_b(b):
                wb_ = b % NWB
                b_ = b % 2
                pg_ = PS[2 * (b % 2)]
                pu_ = PS[2 * (b % 2) + 1]
                for j in range(4):
                    wsrc = wg[wb_] if j < 2 else wu[wb_]
                    pdst = pg_ if j < 2 else pu_
                    for kc in range(8):
                        MM(pdst[:, (j % 2) * BLK:(j % 2 + 1) * BLK], wsrc[:, kc, (j % 2) * 128:(j % 2 + 1) * 128],
                           XT[b_][:, kc, :], kc == 0, kc == 7)
                ACT(sgt[b_], pg_[:, :], AF.Silu)
                TT("dve", HT[b_].rearrange("p a b -> p (a b)"), sgt[b_], pu_[:, :], ALU.mult)

            def stage_c(b):
                wb_ = b % NWB
                b_ = b % 2
                for r in range(2):
                    yb = Yb[r]
                    for n in range(2):
                        py = PS[4 + n]
                        for j2 in range(2):
                            MM(py[:, :], HT[b_][:, j2, r * 128:(r + 1) * 128], wd[wb_][:, j2, n * 512:(n + 1) * 512], j2 == 0, j2 == 1)
                        CP("act" if n == 0 else "dve", yb[:, n * 512:(n + 1) * 512], py[:, :])
                    ST("sp_st", XS[b * BLK + r * 128: b * BLK + (r + 1) * 128, :], yb, dw=["ys"])

            if NBR > 0:
                stage_a(0)
                stage_b(0)
            for b in range(NBR):
                if b + 1 < NBR:
                    stage_a(b + 1)
                stage_c(b)
                if b + 1 < NBR:
                    stage_b(b + 1)
            oc[0] = o_after_slots
            ptl = [cnew([D], F32) for _ in range(2)]
            Yg = [cnew([8, D]) for _ in range(2)]
            acc = [cnew([D], F32) for _ in range(2)]
            g2all = cnew([nseq, D], F32)
            fgbc = cnew([D], F32)
            for s in range(nseq):
                LD("sp_c", g2all[:, s, :], modd[s:s + 1, 5 * D:6 * D].partition_broadcast(128), dr=["modd"])
            LD("sp_c", fgbc, fg_d.partition_broadcast(128))
            for T in range(NTT if upto == "all" else 0):
                s = T // NT
                b_ = T % 2
                st_ = stat(T)
                LD("sp_x", ptl[b_], PART[T * 128:(T + 1) * 128, :], dr=["part"])
                for k in range(8):
                    P.add("pool", (lambda e, T=T, k=k, b_=b_: e.indirect_dma_start(
                        out=Yg[b_][:, k, :], out_offset=None, in_=XS,
                        in_offset=bass.IndirectOffsetOnAxis(ap=slots[:, T, k:k + 1], axis=0))),
                        reads=[slots[:, T, k:k + 1]], writes=[Yg[b_][:, k, :]], dr=["ys"], dma="pg")
                a_ = acc[b_]
                TS("dve", a_, Yg[b_][:, 0, :], gts[:, T, 0:1], None, ALU.mult)
                for k in range(1, 8):
                    STT(a_, Yg[b_][:, k, :], gts[:, T, k:k + 1], a_, ALU.mult, ALU.add)
                TT("dve", a_, a_, g2all[:, s, :], ALU.mult)
                TT("pool", a_, a_, ptl[b_], ALU.add)
                STT(junk, a_, 1.0, a_, ALU.mult, ALU.mult, accum=st_[:, 0:1])
                ACT(st_[:, 1:2], st_[:, 0:1], AF.Ln, bias=epsc[:, 0:1], scale=1.0 / D)
                ACT(st_[:, 2:3], st_[:, 1:2], AF.Exp, scale=-0.5)
                STT(ptl[b_], a_, st_[:, 2:3], fgbc, ALU.mult, ALU.mult)
                ST("sp_st", out_d[T * 128:(T + 1) * 128, :], ptl[b_], dw=["out"])
        P.add("sp", None, dr=["out", "dbg", "part", "xs", "ys", "modd", "h2s", "xsz"])
        P.emit()
    nc._prog = P
    nc._dbg = list(dbg_outs.keys())
    return nc


def _prep_inputs(inputs, ncores, nseq):
    f = lambda a: np.ascontiguousarray(np.asarray(a, dtype=np.float32))
    cst = _host_consts()
    shared = {
        "w_ada": f(inputs["w_ada"]), "b_ada": f(inputs["b_ada"]).reshape(1, -1),
        "n1g": f(f(inputs["norm1_g"]).reshape(8, 128).T),
        "norm2_g": f(inputs["norm2_g"]).reshape(1, -1), "final_g": f(inputs["final_g"]).reshape(1, -1),
        "w_in": f(inputs["w_in"]),
        "bgc": f(f(inputs["b_gate"]).reshape(16, 128).T),
        "qng2": f(np.tile(f(inputs["qn_g"]), 2).reshape(128, 1)),
        "kng2": f(np.tile(f(inputs["kn_g"]), 2).reshape(128, 1)),
        "w_o_dil": f(inputs["w_o_dil"]), "w_o_gqa": f(inputs["w_o_gqa"]), "w_out": f(inputs["w_out"]),
        "router_w": f(inputs["router_w"]), "router_bias": f(inputs["router_bias"]).reshape(1, -1),
        "bidx": f((np.arange(-(-((nseq * S * 8) // BLK + NE) // 128))[None, :] * 128 + np.arange(128)[:, None]).astype(np.float32)),
        "w_exp_gate": f(inputs["w_exp_gate"]), "w_exp_up": f(inputs["w_exp_up"]), "w_exp_down": f(inputs["w_exp_down"]),
        "w_sh_gate": f(inputs["w_sh_gate"]), "w_sh_up": f(inputs["w_sh_up"]), "w_sh_down": f(inputs["w_sh_down"]),
    }
    shared.update(cst)
    x = f(inputs["x"])
    c = f(inputs["c"])
    maps = []
    for i in range(ncores):
        m = dict(shared)
        m["x"] = x[i * nseq:(i + 1) * nseq].reshape(nseq * S, D)
        ci = c[i * nseq:(i + 1) * nseq]
        m["cT"] = f(ci.reshape(nseq, 8, 128).transpose(2, 1, 0))
        maps.append(m)
    return maps


def kernel(**inputs):
    ncores, nseq = 8, 4
    nc = build(nseq=nseq)
    maps = _prep_inputs(inputs, ncores, nseq)
    res = run_bass_kernel_spmd(nc, maps, core_ids=list(range(ncores)))
    out = np.concatenate([r["out"] for r in res.results], axis=0)
    return out.reshape(ncores * nseq, S, D).astype(np.float32)
```
